# Optimizing a Trainium2 kernel written in Bass

```python
import math
import jax, jax.numpy as jnp
from jax import lax
import numpy as np

D_MODEL = 4096
BATCH = 4
SEQ = 4096
DEPTH = 2

HEAD_DIM = 128
N_HEADS = D_MODEL // HEAD_DIM
DILATED_PATTERNS = ((128, 1), (512, 4), (2048, 16))
N_DIL = len(DILATED_PATTERNS)
BLOCK = 128
NUM_BUCKETS = 32
MAX_DISTANCE = 2048
POOL_WINDOWS = (2, 4, 8, 16)
POOL_GROUP = D_MODEL // len(POOL_WINDOWS)
D_FF_DENSE = 11008 * D_MODEL // 4096
D_FF_EXPERT = 3584 * D_MODEL // 4096
N_EXPERTS = 8
TOP_K = 2
N_A = DEPTH // 2
N_B = DEPTH - N_A
N_DENSE = (DEPTH + 1) // 2
N_MOE = DEPTH // 2
EPS = 1e-6
NEG_INF = -1e30

kernel_name = 'hybrid_pool_dilated_moe_yoco'


def rmsnorm(x, g):
    x32 = x.astype(jnp.float32)
    y = x32 * lax.rsqrt(jnp.mean(x32 * x32, axis=-1, keepdims=True) + EPS)
    return y.astype(x.dtype) * g


def head_rms(t, g):
    t32 = t.astype(jnp.float32)
    y = t32 * lax.rsqrt(jnp.mean(t32 * t32, axis=-1, keepdims=True) + EPS)
    return y.astype(t.dtype) * g


def modulate(h, shift, scale):
    return h * (1 + scale[:, None, :]) + shift[:, None, :]


def t5_bucket(n):
    max_exact = NUM_BUCKETS // 2
    nf = jnp.maximum(n, 1).astype(jnp.float32)
    large = max_exact + (jnp.log(nf / max_exact) / math.log(MAX_DISTANCE / max_exact)
                         * (NUM_BUCKETS - max_exact)).astype(jnp.int32)
    return jnp.where(n < max_exact, n, jnp.minimum(large, NUM_BUCKETS - 1))


def band_geometry(window, dilation):
    n_taps = window // dilation
    i = jnp.arange(BLOCK)[:, None]
    kk = jnp.arange(2 * BLOCK)[None, :]
    rel = i + BLOCK - kk
    valid = (rel >= 0) & (rel <= n_taps)
    bucket = t5_bucket(jnp.maximum(rel, 0) * dilation)
    return valid, bucket


def pool_mixer(h, w, scale):
    B, S, D = h.shape
    h32 = h.astype(jnp.float32)
    cs = jnp.cumsum(h32, axis=1)
    t = jnp.arange(S)
    parts = []
    for g, k in enumerate(POOL_WINDOWS):
        sl = slice(g * POOL_GROUP, (g + 1) * POOL_GROUP)
        c_g = cs[..., sl]
        lagged = jnp.pad(c_g, ((0, 0), (k, 0), (0, 0)))[:, :S]
        cnt = jnp.minimum(t + 1, k).astype(jnp.float32)[None, :, None]
        parts.append((c_g - lagged) / cnt - h32[..., sl])
    p = jnp.stack(parts, axis=2).astype(h.dtype)
    y = jnp.einsum('bsgc,gcd->bsgd', p, w).reshape(B, S, D)
    return y * scale


def dilated_branch(q, k, v, bias, valid, dilation):
    B, S, H, E = q.shape
    L = S // dilation
    nb = -(-L // BLOCK)
    Lp = nb * BLOCK

    def to_blocks(t):
        t = t.reshape(B, L, dilation, H, E).transpose(0, 2, 1, 3, 4)
        t = jnp.pad(t, ((0, 0), (0, 0), (0, Lp - L), (0, 0), (0, 0)))
        return t.reshape(B, dilation, nb, BLOCK, H, E)

    def band(t):
        prev = jnp.pad(t, ((0, 0), (0, 0), (1, 0), (0, 0), (0, 0), (0, 0)))[:, :, :nb]
        return jnp.concatenate([prev, t], axis=3)

    qb = to_blocks(q)
    kb = band(to_blocks(k))
    vb = band(to_blocks(v))
    s = jnp.einsum('brnqhe,brnkhe->brnhqk', qb, kb,
                   preferred_element_type=jnp.float32) * (E ** -0.5) + bias
    key_idx = jnp.arange(nb)[:, None] * BLOCK - BLOCK + jnp.arange(2 * BLOCK)[None, :]
    mask = valid[None] & (key_idx >= 0)[:, None, :]
    s = jnp.where(mask[None, None, :, None], s, NEG_INF)
    m = jnp.max(s, axis=-1, keepdims=True)
    p = jnp.exp(s - m)
    l = jnp.sum(p, axis=-1)
    o = jnp.einsum('brnhqk,brnkhe->brnqhe', p.astype(v.dtype), vb,
                   preferred_element_type=jnp.float32)
    o = o / jnp.transpose(l, (0, 1, 2, 4, 3))[..., None]
    lse = jnp.transpose(m[..., 0] + jnp.log(l), (0, 1, 2, 4, 3))
    o = o.reshape(B, dilation, Lp, H, E)[:, :, :L].transpose(0, 2, 1, 3, 4).reshape(B, S, H, E)
    lse = lse.reshape(B, dilation, Lp, H)[:, :, :L].transpose(0, 2, 1, 3).reshape(B, S, H)
    return o, lse


def dilated_attention(h, k, v, w_q, q_g, w_o, rel_bias):
    B, S, _ = h.shape
    q = head_rms((h @ w_q).reshape(B, S, N_DIL, N_HEADS, HEAD_DIM), q_g)
    outs, lses = [], []
    for g, (window, dilation) in enumerate(DILATED_PATTERNS):
        valid, bucket = band_geometry(window, dilation)
        bias = jnp.transpose(rel_bias[bucket, g], (2, 0, 1)).astype(jnp.float32)
        o, lse = dilated_branch(q[:, :, g], k, v, bias, valid, dilation)
        outs.append(o)
        lses.append(lse)
    wts = jax.nn.softmax(jnp.stack(lses), axis=0)
    o = jnp.einsum('gbsh,gbshe->bshe', wts, jnp.stack(outs))
    return o.reshape(B, S, N_HEADS * HEAD_DIM).astype(h.dtype) @ w_o


def swiglu(h, w1, w3, w2):
    return (jax.nn.silu(h @ w1) * (h @ w3)) @ w2


def moe_swiglu(h, router_w, w1, w3, w2):
    B, S, D = h.shape
    t = h.reshape(B * S, D)
    logits = (t @ router_w).astype(jnp.float32)
    top_v, top_i = lax.top_k(logits, TOP_K)
    gates = jax.nn.softmax(top_v, axis=-1)
    combine = jnp.sum(jax.nn.one_hot(top_i, N_EXPERTS, dtype=jnp.float32) * gates[..., None], axis=1)
    combine = combine.astype(t.dtype)
    out = jnp.zeros_like(t)
    for e in range(N_EXPERTS):
        out = out + combine[:, e:e + 1] * swiglu(t, w1[e], w3[e], w2[e])
    return out.reshape(B, S, D)


def setup_inputs(seed: int = 0) -> dict:
    key = jax.random.key(seed)
    ks = jax.random.split(key, 24)
    f32 = jnp.float32
    D, H, E = D_MODEL, N_HEADS, HEAD_DIM
    nrm = lambda k, shape, s: jax.random.normal(k, shape, f32) * s
    return {
        'x': nrm(ks[0], (BATCH, SEQ, D), 1.0),
        'c': nrm(ks[1], (BATCH, D), 1.0),
        'ada_w': nrm(ks[2], (DEPTH, 2, D, 3 * D), 0.5 * D ** -0.5),
        'ada_b': nrm(ks[3], (DEPTH, 2, 3 * D), 0.02),
        'norm_g': 1.0 + nrm(ks[4], (DEPTH, 2, D), 0.1),
        'pool_w': nrm(ks[5], (N_A, len(POOL_WINDOWS), POOL_GROUP, POOL_GROUP), POOL_GROUP ** -0.5),
        'pool_scale': 1.0 + nrm(ks[6], (N_A, D), 0.1),
        'kv_ada_w': nrm(ks[7], (D, 2 * D), 0.5 * D ** -0.5),
        'kv_ada_b': nrm(ks[8], (2 * D,), 0.02),
        'kv_norm_g': 1.0 + nrm(ks[9], (D,), 0.1),
        'w_k': nrm(ks[10], (D, H * E), D ** -0.5),
        'w_v': nrm(ks[11], (D, H * E), D ** -0.5),
        'k_norm_g': 1.0 + nrm(ks[12], (E,), 0.1),
        'w_q': nrm(ks[13], (N_B, D, N_DIL * H * E), D ** -0.5),
        'q_norm_g': 1.0 + nrm(ks[14], (N_B, E), 0.1),
        'w_o': nrm(ks[15], (N_B, H * E, D), (H * E) ** -0.5),
        'rel_bias': nrm(ks[16], (NUM_BUCKETS, N_DIL, H), 0.5),
        'ffn_w1': nrm(ks[17], (N_DENSE, D, D_FF_DENSE), D ** -0.5),
        'ffn_w3': nrm(ks[18], (N_DENSE, D, D_FF_DENSE), D ** -0.5),
        'ffn_w2': nrm(ks[19], (N_DENSE, D_FF_DENSE, D), D_FF_DENSE ** -0.5),
        'router_w': nrm(ks[20], (N_MOE, D, N_EXPERTS), D ** -0.5),
        'moe_w1': nrm(ks[21], (N_MOE, N_EXPERTS, D, D_FF_EXPERT), D ** -0.5),
        'moe_w3': nrm(ks[22], (N_MOE, N_EXPERTS, D, D_FF_EXPERT), D ** -0.5),
        'moe_w2': nrm(ks[23], (N_MOE, N_EXPERTS, D_FF_EXPERT, D), D_FF_EXPERT ** -0.5),
    }


def reference(x, c, ada_w, ada_b, norm_g, pool_w, pool_scale, kv_ada_w, kv_ada_b, kv_norm_g,
              w_k, w_v, k_norm_g, w_q, q_norm_g, w_o, rel_bias, ffn_w1, ffn_w3, ffn_w2,
              router_w, moe_w1, moe_w3, moe_w2):
    B, S, D = x.shape
    cond = jax.nn.silu(c)
    k = None
    v = None
    for i in range(DEPTH):
        if i == N_A:
            kv_shift, kv_scale = jnp.split(cond @ kv_ada_w + kv_ada_b, 2, axis=-1)
            kvh = modulate(rmsnorm(x, kv_norm_g), kv_shift, kv_scale)
            k = head_rms((kvh @ w_k).reshape(B, S, N_HEADS, HEAD_DIM), k_norm_g)
            v = (kvh @ w_v).reshape(B, S, N_HEADS, HEAD_DIM)
        shift, scale, gate = jnp.split(cond @ ada_w[i, 0] + ada_b[i, 0], 3, axis=-1)
        h = modulate(rmsnorm(x, norm_g[i, 0]), shift, scale)
        if i < N_A:
            y = pool_mixer(h, pool_w[i], pool_scale[i])
        else:
            j = i - N_A
            y = dilated_attention(h, k, v, w_q[j], q_norm_g[j], w_o[j], rel_bias)
        x = x + gate[:, None, :] * y
        shift, scale, gate = jnp.split(cond @ ada_w[i, 1] + ada_b[i, 1], 3, axis=-1)
        h = modulate(rmsnorm(x, norm_g[i, 1]), shift, scale)
        if i % 2 == 0:
            y = swiglu(h, ffn_w1[i // 2], ffn_w3[i // 2], ffn_w2[i // 2])
        else:
            y = moe_swiglu(h, router_w[i // 2], moe_w1[i // 2], moe_w3[i // 2], moe_w2[i // 2])
        x = x + gate[:, None, :] * y
    return x
```

```python
import numpy as np
import ml_dtypes
from contextlib import ExitStack
import concourse.bass as bass
import concourse.mybir as mybir
from concourse.bass_utils import run_bass_kernel_spmd

F32 = mybir.dt.float32
BF16 = mybir.dt.bfloat16
AF = mybir.ActivationFunctionType
ALU = mybir.AluOpType
NEG = -1.0e30
EPS = 1e-6
HALO = 16
PATTERNS = ((128, 1), (512, 4), (2048, 16))


class Cfg:
    def __init__(self, D=4096, DFF=11008, DFE=3584, NE=8, SEQ=4096, B=4):
        self.D = D
        self.KD = D // 128
        self.H = D // 128
        self.DFF = DFF
        self.DFE = DFE
        self.NE = NE
        self.SEQ = SEQ
        self.B = B
        self.T = SEQ // 2
        self.G = D // 4


class Sem:
    def __init__(self, h):
        self.h = h
        self.n = 0


class Prog:
    def __init__(self):
        self.q = {k: [] for k in ("sp", "act", "dve", "pool", "pe")}

    def op(self, eng, fn, waits=(), inc=None, k=1):
        if inc is not None:
            inc.n += k
        self.q[eng].append((tuple(waits), fn, inc, k))

    def dma(self, eng, out, in_, waits=(), inc=None):
        self.op(eng, lambda e, o=out, i=in_: e.dma_start(out=o, in_=i), waits, inc, 16)


class Chain:
    def __init__(self, P, cs, ds):
        self.P, self.cs, self.ds = P, cs, ds
        self.last = None

    def op(self, eng, fn, extra=()):
        w = list(extra)
        if self.last:
            w.append(self.last)
        self.P.op(eng, fn, w, self.cs, 1)
        self.last = (self.cs, self.cs.n)

    def dma(self, eng, out, in_, extra=()):
        w = list(extra)
        if self.last:
            w.append(self.last)
        self.P.dma(eng, out, in_, w, self.ds)
        self.last = (self.ds, self.ds.n)


def _run(e, ops):
    seen = {}
    for waits, fn, inc, k in ops:
        for (s, v) in waits:
            if v <= 0:
                continue
            key = id(s)
            if seen.get(key, 0) >= v:
                continue
            seen[key] = v
            e.wait_ge(s.h, v)
        ins = fn(e)
        if inc is not None:
            ins.then_inc(inc.h, k)


def emit(nc, P):
    with nc.Block() as block:
        if P.q["sp"]:
            @block.sync
            def _(e):
                _run(e, P.q["sp"])
        if P.q["act"]:
            @block.scalar
            def _(e):
                _run(e, P.q["act"])
        if P.q["dve"]:
            @block.vector
            def _(e):
                _run(e, P.q["dve"])
        if P.q["pool"]:
            @block.gpsimd
            def _(e):
                _run(e, P.q["pool"])
        if P.q["pe"]:
            @block.tensor
            def _(e):
                _run(e, P.q["pe"])


class Ctx:
    def __init__(self, nc, cfg, es):
        self.nc = nc
        self.cfg = cfg
        self.ps = [es.enter_context(nc.psum_tensor(f"psb{i}", [128, 512], F32)) for i in range(8)]
        self.IN = None
        self.scr = es.enter_context(nc.sbuf_tensor("scr", [128, 16], F32))
        self.epsD = es.enter_context(nc.sbuf_tensor("epsD", [128, 1], F32))
        self.epsE = es.enter_context(nc.sbuf_tensor("epsE", [128, 1], F32))
        self.uid = 0
        self._phase_sems = {}

    def alloc_in(self, es):
        self.uid += 1
        self.IN = es.enter_context(self.nc.sbuf_tensor(f"IN_{self.uid}", [128, 32, self.cfg.T], BF16))

    def sb(self, es, name, shape, dt):
        self.uid += 1
        return es.enter_context(self.nc.sbuf_tensor(f"{name}_{self.uid}", shape, dt))

    def sem(self, es, name):
        self.uid += 1
        h = self.nc.alloc_semaphore(name=f"{name}_{self.uid}")
        lst = getattr(es, "_sem_list", None)
        if lst is None:
            lst = []
            es._sem_list = lst

            def cleanup(lst=lst):
                self.nc.clear_and_free_semaphores(lst)
                self.nc.all_engine_barrier()
            es.callback(cleanup)
        lst.append(h)
        return Sem(h)


def phase_loads(cx, pairs, post=None):
    with ExitStack() as es:
        cs, ds = cx.sem(es, "lc"), cx.sem(es, "ld")
        P = Prog()
        for (o, i) in pairs:
            P.dma("sp", o, i, inc=ds)
        ch = Chain(P, cs, ds)
        ch.last = (ds, ds.n)
        if post is not None:
            post(ch)
        else:
            ch.op("dve", lambda e: e.memset(cx.scr[0:1, 0:1], 0.0))
        emit(cx.nc, P)


def phase_ada(cx, w_ap, bT_sb, condS, mod_sb, NJ):
    nc, cfg = cx.nc, cx.cfg
    KD = cfg.KD
    with ExitStack() as es:
        NBUF = 2
        AW = [cx.sb(es, "aw", [128, KD, 256], F32) for i in range(NBUF)]
        ld = [cx.sem(es, "awld") for i in range(NBUF)]
        s_mm = cx.sem(es, "awmm")
        s_fin = cx.sem(es, "awfin")
        P = Prog()
        wv = w_ap.rearrange("(kc p) n -> p kc n", p=128)
        ntile = NJ // 2
        ps = cx.ps[0]
        for t in range(ntile):
            b = t % NBUF
            P.dma("sp", AW[b][:], wv[:, :, t * 256:(t + 1) * 256],
                  waits=[(s_mm, t - NBUF + 1)], inc=ld[b])
            for jj in range(2):
                j = t * 2 + jj
                for kc in range(KD):
                    last = (jj == 1 and kc == KD - 1)
                    P.op("pe",
                         lambda e, b=b, jj=jj, kc=kc, j=j: e.matmul(
                             ps[:, j:j + 1], lhsT=AW[b][:, kc, jj * 128:(jj + 1) * 128],
                             rhs=condS[:, kc:kc + 1], start=(kc == 0), stop=(kc == KD - 1)),
                         waits=[(ld[b], 16 * (t // NBUF + 1))],
                         inc=s_mm if last else None)
        P.op("dve", lambda e: e.tensor_tensor(out=mod_sb[:, 0:NJ], in0=ps[:, 0:NJ], in1=bT_sb[:, 0:NJ], op=ALU.add),
             waits=[(s_mm, ntile)], inc=s_fin)
        emit(nc, P)


def phase_transpose_in(cx, x_ap, xT_ap, ident, ntok_total):
    nc, cfg = cx.nc, cx.cfg
    KD, D = cfg.KD, cfg.D
    xTv = xT_ap.rearrange("c p t -> p c t")
    tiles = []
    t0 = 0
    rem = ntok_total % 128
    if rem:
        tiles.append((0, rem))
        t0 = rem
    while t0 < ntok_total:
        tiles.append((t0, 128))
        t0 += 128
    with ExitStack() as es:
        XI = cx.sb(es, "xi", [128, D], F32)
        XO = cx.sb(es, "xo", [128, KD, 128], F32)
        cs, ds = cx.sem(es, "tc"), cx.sem(es, "td")
        P = Prog()
        ch = Chain(P, cs, ds)
        for (t0, n) in tiles:
            ch.dma("sp", XI[0:n, :], x_ap[t0:t0 + n, :])
            for g0 in range(0, KD, 32):
                gcs = list(range(g0, min(KD, g0 + 32)))

                def tr(e, gcs=gcs, g0=g0, n=n):
                    ins = None
                    for c in gcs:
                        bank = cx.ps[((c - g0) // 4) % 8]
                        cc = (c - g0) % 4
                        ins = e.transpose(bank[:, cc * 128:cc * 128 + n], XI[0:n, c * 128:(c + 1) * 128],
                                          ident[0:n, 0:n])
                    return ins
                ch.op("pe", tr)
                nb = (len(gcs) + 3) // 4
                for bk in range(nb):
                    c0 = g0 + bk * 4
                    ncz = min(4, KD - c0)
                    src = cx.ps[bk][:, 0:ncz * 128].rearrange("p (c t) -> p c t", t=128)[:, :, 0:n]
                    dst = XO[:, c0:c0 + ncz, 0:n]
                    if bk % 2 == 0:
                        ch.op("act", lambda e, s=src, d=dst: e.copy(out=d, in_=s))
                    else:
                        ch.op("dve", lambda e, s=src, d=dst: e.tensor_copy(out=d, in_=s))
            ch.dma("sp", xTv[:, :, t0:t0 + n], XO[:, :, 0:n])
        ch.op("dve", lambda e: e.memset(XO[0:1, 0, 0:1], 0.0))
        emit(nc, P)


def phase_transpose_out(cx, xT_ap, col0, out_ap, ident):
    nc, cfg = cx.nc, cx.cfg
    KD, D, T = cfg.KD, cfg.D, cfg.T
    xTv = xT_ap.rearrange("c p t -> p c t")
    with ExitStack() as es:
        XI = cx.sb(es, "yi", [128, KD, 128], F32)
        XO = cx.sb(es, "yo", [128, D], F32)
        cs, ds = cx.sem(es, "tc"), cx.sem(es, "td")
        P = Prog()
        ch = Chain(P, cs, ds)
        for t0 in range(0, T, 128):
            ch.dma("sp", XI[:, :, :], xTv[:, :, col0 + t0:col0 + t0 + 128])
            for g0 in range(0, KD, 32):
                gcs = list(range(g0, min(KD, g0 + 32)))

                def tr(e, gcs=gcs, g0=g0):
                    ins = None
                    for c in gcs:
                        bank = cx.ps[((c - g0) // 4) % 8]
                        cc = (c - g0) % 4
                        ins = e.transpose(bank[:, cc * 128:(cc + 1) * 128], XI[:, c, :], ident[:, :])
                    return ins
                ch.op("pe", tr)
                nb = (len(gcs) + 3) // 4
                for bk in range(nb):
                    c0 = g0 + bk * 4
                    ncz = min(4, KD - c0)
                    src = cx.ps[bk][:, 0:ncz * 128]
                    dst = XO[:, c0 * 128:(c0 + ncz) * 128]
                    if bk % 2 == 0:
                        ch.op("act", lambda e, s=src, d=dst: e.copy(out=d, in_=s))
                    else:
                        ch.op("dve", lambda e, s=src, d=dst: e.tensor_copy(out=d, in_=s))
            ch.dma("sp", out_ap[t0:t0 + 128, :], XO[:, :])
        ch.op("dve", lambda e: e.memset(XO[0:1, 0:1], 0.0))
        emit(nc, P)


def phase_norm(cx, xT_ap, col0, A_sb, B_sb, ones_bf, mode="plain", pool=None, router=None):
    nc, cfg = cx.nc, cx.cfg
    KD, D, T = cfg.KD, cfg.D, cfg.T
    xTv = xT_ap.rearrange("c p t -> p c t")
    IN = cx.IN
    blocks = [(t, 128) for t in range(0, T, 128)]
    if mode == "pool":
        blocks = [(-HALO, HALO)] + blocks
    with ExitStack() as es:
        XB = cx.sb(es, "nxb", [128, KD, 128], F32)
        SQ = cx.sb(es, "nsq", [128, KD, 128], BF16)
        TMP = cx.sb(es, "ntmp", [128, KD, 128], F32)
        RS = cx.sb(es, "nrs", [128, 128], F32)
        cs, ds = cx.sem(es, "nc"), cx.sem(es, "nd")
        if mode == "pool":
            HB = cx.sb(es, "nhb", [128, KD, 128 + HALO], F32)
            S1 = cx.sb(es, "ns1", [128, KD // 4, 128 + HALO], F32)
            S2 = cx.sb(es, "ns2", [128, KD // 4, 128 + HALO], F32)
        if mode == "router":
            NE = cfg.NE
            HF = cx.sb(es, "nhf", [128, KD, 128], F32)
            LG = cx.sb(es, "nlg", [128, 16], F32)
            M8 = cx.sb(es, "nm8", [128, 16], F32)
            CB = cx.sb(es, "ncb", [128, 16], F32)
            DG = cx.sb(es, "ndg", [128, NE, 128], F32)
            CO = cx.sb(es, "nco", [128, NE, 128], F32)
            combv = router["comb"].rearrange("e p t -> p e t")
        P = Prog()
        ch = Chain(P, cs, ds)
        psS = cx.ps[0]
        for (t0, n) in blocks:
            ch.dma("sp", XB[:, :, 0:n], xTv[:, :, col0 + t0:col0 + t0 + n])
            ch.op("act", lambda e, n=n: e.activation(out=SQ[:, :, 0:n], in_=XB[:, :, 0:n], func=AF.Square))

            def ssmm(e, n=n):
                ins = None
                for c in range(KD):
                    ins = e.matmul(psS[:, 0:n], lhsT=ones_bf[:, :], rhs=SQ[:, c, 0:n],
                                   start=(c == 0), stop=(c == KD - 1))
                return ins
            ch.op("pe", ssmm)

            ch.op("act", lambda e, n=n: e.activation(out=RS[:, 0:n], in_=psS[:, 0:n], func=AF.Sqrt,
                                                     bias=cx.epsD[:, 0:1], scale=1.0))

            ch.op("dve", lambda e, n=n: e.reciprocal(out=RS[:, 0:n], in_=RS[:, 0:n]))

            def nrm(e, n=n):
                ins = None
                for c in range(KD):
                    ins = e.scalar_tensor_tensor(out=TMP[:, c, 0:n], in0=XB[:, c, 0:n], scalar=A_sb[:, c:c + 1],
                                                 in1=RS[:, 0:n], op0=ALU.mult, op1=ALU.mult)
                return ins
            ch.op("dve", nrm)

            def shf(e, n=n, t0=t0):
                ins = None
                for c in range(KD):
                    if mode == "pool":
                        dst = HB[:, c, HALO:HALO + n] if t0 >= 0 else HB[:, c, 0:HALO]
                    elif mode == "router":
                        dst = HF[:, c, 0:n]
                    else:
                        dst = IN[:, c, t0:t0 + n]
                    ins = e.activation(out=dst, in_=TMP[:, c, 0:n], func=AF.Identity, bias=B_sb[:, c:c + 1])
                return ins
            ch.op("act", shf)
            if mode == "pool":
                _pool_block(ch, cx, pool, HB, S1, S2, t0)
            if mode == "router":
                _router_block(ch, cx, router, HF, LG, M8, CB, DG, CO, combv, t0)
        if mode != "pool":
            ch.op("dve", lambda e: e.memset(RS[0:1, 0:1], 0.0))
        emit(nc, P)


def _pool_block(ch, cx, pool, HB, S1, S2, t0):
    cfg = cx.cfg
    KG = cfg.KD // 4
    IN = cx.IN
    W = 128 + HALO
    if t0 < 0:
        ch.op("dve", lambda e: e.tensor_scalar(out=HB[:, :, 0:HALO], in0=HB[:, :, 0:HALO],
                                               scalar1=pool["hmask"][:, 0:1], scalar2=None, op0=ALU.mult))
        return
    first = (t0 == 0)
    for g, k in enumerate((2, 4, 8, 16)):
        cs_ = slice(g * KG, (g + 1) * KG)
        cur, cur_cs, lo, step, it = HB, cs_, 0, 1, 0
        bufs = [S1, S2]
        while step < k:
            dst = bufs[it % 2]
            nlo = lo + step
            ch.op("dve", lambda e, dst=dst, cur=cur, cur_cs=cur_cs, nlo=nlo, step=step: e.tensor_tensor(
                out=dst[:, :, nlo:W], in0=cur[:, cur_cs, nlo:W], in1=cur[:, cur_cs, nlo - step:W - step], op=ALU.add))
            cur, cur_cs, lo = dst, slice(0, KG), nlo
            step *= 2
            it += 1
        ch.op("dve", lambda e, cur=cur, cs_=cs_, k=k: e.scalar_tensor_tensor(
            out=IN[:, cs_, t0:t0 + 128], in0=cur[:, :, HALO:W], scalar=1.0 / k, in1=HB[:, cs_, HALO:W],
            op0=ALU.mult, op1=ALU.subtract))
        if first:
            def fix(e, cur=cur, g=g):
                ins = None
                for c in range(KG):
                    ins = e.tensor_tensor(out=cur[:, c, HALO:2 * HALO], in0=cur[:, c, HALO:2 * HALO],
                                          in1=pool["invc"][:, g, :], op=ALU.mult)
                return ins
            ch.op("dve", fix)
            ch.op("dve", lambda e, cur=cur, cs_=cs_: e.tensor_tensor(
                out=IN[:, cs_, 0:HALO], in0=cur[:, :, HALO:2 * HALO], in1=HB[:, cs_, HALO:2 * HALO], op=ALU.subtract))
    ch.op("dve", lambda e: e.tensor_copy(out=HB[:, :, 0:HALO], in_=HB[:, :, 128:W]))


def _router_block(ch, cx, R, HF, LG, M8, CB, DG, CO, combv, t0):
    cfg = cx.cfg
    KD, NE = cfg.KD, cfg.NE
    IN = cx.IN
    psR = cx.ps[1]
    ch.op("pool", lambda e: e.tensor_copy(out=IN[:, 0:KD, t0:t0 + 128], in_=HF[:, :, :]))

    def rmm(e):
        ins = None
        for c in range(KD):
            ins = e.matmul(psR[:, 0:NE], lhsT=HF[:, c, :], rhs=R["rw"][:, c, :], start=(c == 0), stop=(c == KD - 1))
        return ins
    ch.op("pe", rmm)

    ch.op("dve", lambda e: e.tensor_copy(out=LG[:, 0:NE], in_=psR[:, 0:NE]))
    ch.op("dve", lambda e: e.max(out=M8[:, 0:8], in_=LG[:, 0:NE]))
    ch.op("dve", lambda e: e.tensor_tensor(out=M8[:, 8:9], in0=M8[:, 1:2], in1=M8[:, 0:1], op=ALU.subtract))
    ch.op("act", lambda e: e.activation(out=M8[:, 9:10], in_=M8[:, 8:9], func=AF.Sigmoid))

    ch.op("dve", lambda e: e.tensor_scalar(out=M8[:, 10:11], in0=M8[:, 9:10], scalar1=-1.0, scalar2=1.0,
                                           op0=ALU.mult, op1=ALU.add))

    def cm2(e):
        e.tensor_scalar(out=CB[:, 0:NE], in0=LG[:, 0:NE], scalar1=M8[:, 0:1], scalar2=M8[:, 10:11],
                        op0=ALU.is_equal, op1=ALU.mult)
        return e.tensor_scalar(out=CB[:, NE:2 * NE], in0=LG[:, 0:NE], scalar1=M8[:, 1:2], scalar2=M8[:, 9:10],
                               op0=ALU.is_equal, op1=ALU.mult)
    ch.op("dve", cm2)
    ch.op("dve", lambda e: e.tensor_tensor(out=CB[:, 0:NE], in0=CB[:, 0:NE], in1=CB[:, NE:2 * NE], op=ALU.add))

    def cmb(e):
        ins = None
        for ex in range(NE):
            ins = e.tensor_scalar(out=DG[:, ex, :], in0=R["ident"][:, :], scalar1=CB[:, ex:ex + 1], scalar2=None,
                                  op0=ALU.mult)
        return ins
    ch.op("dve", cmb)

    def bmm(e):
        ins = None
        for ex in range(NE):
            bank = cx.ps[2 + ex // 4]
            ins = e.matmul(bank[:, (ex % 4) * 128:(ex % 4 + 1) * 128], lhsT=R["ones32"][:, :], rhs=DG[:, ex, :],
                           start=True, stop=True)
        return ins
    ch.op("pe", bmm)
    for hb in range(NE // 4):
        ch.op("act", lambda e, hb=hb: e.copy(out=CO[:, hb * 4:(hb + 1) * 4, :],
                                             in_=cx.ps[2 + hb][:, :].rearrange("p (a t) -> p a t", t=128)))
    ch.dma("sp", combv[:, :, t0:t0 + 128], CO[:, :, :])


def phase_gemm(cx, name, KC, kc0, tiles, epi, in_src=None, mode="fm", WCOLS=256):
    nc, cfg = cx.nc, cx.cfg
    T = cfg.T
    IN = cx.IN
    NB = T // 512
    with ExitStack() as es:
        NBUF = 3
        WT = [cx.sb(es, "wt", [128, KC, WCOLS], BF16) for _ in range(NBUF)]
        wld = [cx.sem(es, "wld") for _ in range(NBUF)]
        s_mm = cx.sem(es, "gmm")
        s_free = cx.sem(es, "gfree")
        s_in = cx.sem(es, "gin")
        P = Prog()
        epi.setup(cx, es, P, s_mm, s_free)
        in_wait = []
        if in_src is not None:
            for kc in range(KC):
                P.dma("sp", IN[:, kc0 + kc, :], in_src[kc, :, :], inc=s_in)
            in_wait = [(s_in, 16 * KC)]
        G = 0
        tile_lastG = []
        for ti, tl in enumerate(tiles):
            b = ti % NBUF
            wv = tl["ap"].rearrange("(kc p) n -> p kc n", p=128)
            wfree = [(s_mm, tile_lastG[ti - NBUF])] if ti >= NBUF else []
            P.dma("pool", WT[b][:, :, :], wv, waits=wfree, inc=wld[b])
            wready = [(wld[b], 16 * (ti // NBUF + 1))]
            if mode == "fm":
                for ji, meta in enumerate(tl["jobs"]):
                    for tb in range(NB):
                        bank = cx.ps[G % 8]
                        for kc in range(KC):
                            P.op("pe", lambda e, bank=bank, b=b, kc=kc, ji=ji, tb=tb: e.matmul(
                                bank[:, :], lhsT=WT[b][:, kc, ji * 128:(ji + 1) * 128],
                                rhs=IN[:, kc0 + kc, tb * 512:(tb + 1) * 512],
                                start=(kc == 0), stop=(kc == KC - 1)),
                                waits=wready + in_wait + [(s_free, epi.need_free(G))],
                                inc=s_mm if kc == KC - 1 else None)
                        epi.group(G, meta, tb, bank)
                        G += 1
            else:
                for tt in range(T // 128):
                    bank = cx.ps[G % 8]
                    for kc in range(KC):
                        P.op("pe", lambda e, bank=bank, b=b, kc=kc, tt=tt: e.matmul(
                            bank[:, 0:WCOLS], lhsT=IN[:, kc0 + kc, tt * 128:(tt + 1) * 128],
                            rhs=WT[b][:, kc, :], start=(kc == 0), stop=(kc == KC - 1)),
                            waits=wready + in_wait + [(s_free, epi.need_free(G))],
                            inc=s_mm if kc == KC - 1 else None)
                    epi.group(G, tl["jobs"][0], tt, bank)
                    G += 1
            tile_lastG.append(G)
        epi.finish()
        emit(nc, P)


class EpiResid:
    NS = 4

    def __init__(self, xT_ap, col0, gate_sb):
        self.xT, self.col0, self.gate = xT_ap, col0, gate_sb

    def setup(self, cx, es, P, s_mm, s_free):
        self.cx, self.P, self.s_mm, self.s_free = cx, P, s_mm, s_free
        self.XR = [cx.sb(es, "xr", [128, 512], F32) for _ in range(self.NS)]
        self.ld = [cx.sem(es, "xrld") for _ in range(self.NS)]
        self.st = [cx.sem(es, "xrst") for _ in range(self.NS)]

    def need_free(self, G):
        return G - 7

    def group(self, G, meta, tb, bank):
        P, s = self.P, G % self.NS
        r = G // self.NS
        chunk = meta
        c0 = self.col0 + tb * 512
        XR = self.XR[s]
        P.dma("sp", XR[:, :], self.xT[chunk, :, c0:c0 + 512], waits=[(self.st[s], 16 * r)], inc=self.ld[s])
        P.op("dve", lambda e: e.scalar_tensor_tensor(out=XR[:, :], in0=bank[:, :], scalar=self.gate[:, chunk:chunk + 1],
                                                     in1=XR[:, :], op0=ALU.mult, op1=ALU.add),
             waits=[(self.s_mm, G + 1), (self.ld[s], 16 * (r + 1))], inc=self.s_free)
        P.dma("act", self.xT[chunk, :, c0:c0 + 512], XR[:, :], waits=[(self.s_free, G + 1)], inc=self.st[s])

    def finish(self):
        self.P.op("dve", lambda e: e.memset(self.cx.scr[0:1, 0:1], 0.0), waits=[(s, s.n) for s in self.st])


class EpiSwiglu:
    NS = 2

    def __init__(self, aT_ap, comb_ap=None):
        self.aT, self.comb = aT_ap, comb_ap

    def setup(self, cx, es, P, s_mm, s_free):
        self.cx, self.P, self.s_mm, self.s_free = cx, P, s_mm, s_free
        self.SG = [cx.sb(es, "sg", [128, 512], F32) for _ in range(self.NS)]
        self.AO = [cx.sb(es, "ao", [128, 512], BF16) for _ in range(self.NS)]
        self.st = [cx.sem(es, "aost") for _ in range(self.NS)]
        self.s_sg = cx.sem(es, "sgs")
        self.s_d1 = cx.sem(es, "sd1")
        self.E = 0
        self.bank1 = {}
        self.cur_e = -1
        self.last_E_of = {}
        if self.comb is not None:
            self.CBT = [cx.sb(es, "cbt", [128, cx.cfg.T], F32) for _ in range(2)]
            self.cbld = [cx.sem(es, "cbld") for _ in range(2)]

    def need_free(self, G):
        Gp = G - 8
        if Gp < 0:
            return 0
        m, r = divmod(Gp, 8)
        return 4 * m + (r % 4) + 1

    def group(self, G, meta, tb, bank):
        which, fchunk, ex = meta
        if which == 0:
            self.bank1[tb] = bank
            return
        P = self.P
        E = self.E
        self.E += 1
        s = E % self.NS
        r = E // self.NS
        b1, b3 = self.bank1[tb], bank
        SG, AO = self.SG[s], self.AO[s]
        P.op("act", lambda e: e.activation(out=SG[:, :], in_=b1[:, :], func=AF.Silu),
             waits=[(self.s_mm, G + 1), (self.s_free, E - self.NS + 1)], inc=self.s_sg)
        if self.comb is None:
            P.op("dve", lambda e: e.tensor_tensor(out=AO[:, :], in0=SG[:, :], in1=b3[:, :], op=ALU.mult),
                 waits=[(self.s_sg, E + 1), (self.st[s], 16 * r)], inc=self.s_free)
        else:
            cb = ex % 2
            if ex != self.cur_e:
                P.dma("sp", self.CBT[cb][:, :], self.comb[ex, :, :],
                      waits=[(self.s_free, self.last_E_of.get(ex - 2, 0))], inc=self.cbld[cb])
                self.cur_e = ex
            CBT = self.CBT[cb]
            P.op("dve", lambda e: e.tensor_tensor(out=SG[:, :], in0=SG[:, :], in1=b3[:, :], op=ALU.mult),
                 waits=[(self.s_sg, E + 1), (self.st[s], 16 * r)], inc=self.s_d1)
            P.op("dve", lambda e: e.tensor_tensor(out=AO[:, :], in0=SG[:, :], in1=CBT[:, tb * 512:(tb + 1) * 512],
                                                  op=ALU.mult),
                 waits=[(self.cbld[cb], 16 * (ex // 2 + 1)), (self.s_d1, E + 1)], inc=self.s_free)
            self.last_E_of[ex] = E + 1
        P.dma("sp", self.aT[fchunk, :, tb * 512:(tb + 1) * 512], AO[:, :], waits=[(self.s_free, E + 1)],
              inc=self.st[s])

    def finish(self):
        self.P.op("dve", lambda e: e.memset(self.cx.scr[0:1, 0:1], 0.0), waits=[(s, s.n) for s in self.st])


class EpiRaw:
    NS = 4

    def __init__(self, dst_fn, dt, width=512):
        self.dst_fn, self.dt, self.width = dst_fn, dt, width

    def setup(self, cx, es, P, s_mm, s_free):
        self.cx, self.P, self.s_mm, self.s_free = cx, P, s_mm, s_free
        self.RO = [cx.sb(es, "ro", [128, self.width], self.dt) for _ in range(self.NS)]
        self.st = [cx.sem(es, "rost") for _ in range(self.NS)]

    def need_free(self, G):
        return G - 7

    def group(self, G, meta, tb, bank):
        P, s = self.P, G % self.NS
        r = G // self.NS
        RO = self.RO[s]
        w = self.width
        if G % 2 == 0:
            P.op("act", lambda e: e.copy(out=RO[:, :], in_=bank[:, 0:w]),
                 waits=[(self.s_mm, G + 1), (self.st[s], 16 * r), (self.s_free, G)], inc=self.s_free)
        else:
            P.op("dve", lambda e: e.tensor_copy(out=RO[:, :], in_=bank[:, 0:w]),
                 waits=[(self.s_mm, G + 1), (self.st[s], 16 * r), (self.s_free, G)], inc=self.s_free)
        P.dma("sp", self.dst_fn(meta, tb), RO[:, :], waits=[(self.s_free, G + 1)], inc=self.st[s])

    def finish(self):
        self.P.op("dve", lambda e: e.memset(self.cx.scr[0:1, 0:1], 0.0), waits=[(s, s.n) for s in self.st])


def phase_headnorm(cx, raw_ap, out_ap, nchunk, gvec_sb, ones_bf):
    nc, cfg = cx.nc, cx.cfg
    T = cfg.T
    NB = T // 512
    with ExitStack() as es:
        RW = cx.sb(es, "hraw", [128, T], F32)
        SQ = cx.sb(es, "hsq", [128, T], BF16)
        RS = cx.sb(es, "hrs", [128, T], F32)
        QN = cx.sb(es, "hqn", [128, T], BF16)
        cs, ds = cx.sem(es, "hc"), cx.sem(es, "hd")
        P = Prog()
        ch = Chain(P, cs, ds)
        for ci in range(nchunk):
            ch.dma("sp", RW[:, :], raw_ap[ci, :, :])
            ch.op("act", lambda e: e.activation(out=SQ[:, :], in_=RW[:, :], func=AF.Square))

            def mm(e):
                ins = None
                for tb in range(NB):
                    ins = e.matmul(cx.ps[tb][:, :], lhsT=ones_bf[:, :], rhs=SQ[:, tb * 512:(tb + 1) * 512],
                                   start=True, stop=True)
                return ins
            ch.op("pe", mm)

            def sq(e):
                ins = None
                for tb in range(NB):
                    ins = e.activation(out=RS[:, tb * 512:(tb + 1) * 512], in_=cx.ps[tb][:, :], func=AF.Sqrt,
                                       bias=cx.epsE[:, 0:1], scale=1.0)
                return ins
            ch.op("act", sq)

            ch.op("dve", lambda e: e.reciprocal(out=RS[:, :], in_=RS[:, :]))

            def fin(e):
                return e.scalar_tensor_tensor(out=QN[:, :], in0=RW[:, :], scalar=gvec_sb[:, 0:1], in1=RS[:, :],
                                              op0=ALU.mult, op1=ALU.mult)
            ch.op("dve", fin)
            ch.dma("sp", out_ap[ci, :, :], QN[:, :])
        ch.op("dve", lambda e: e.memset(RS[0:1, 0:1], 0.0))
        emit(nc, P)


def _din(nc, name, shape, dt=F32):
    return nc.dram_tensor(name, list(shape), dt, kind="ExternalInput").ap()


def _dout(nc, name, shape, dt=F32):
    return nc.dram_tensor(name, list(shape), dt, kind="ExternalOutput").ap()


def _dint(nc, name, shape, dt=F32):
    return nc.dram_tensor(name, list(shape), dt, kind="Internal").ap()


def _derive_AB(ch, A, MOD, g_sb, KD, D):
    ch.op("dve", lambda e: e.scalar_tensor_tensor(out=A[:, 0:KD], in0=MOD[:, KD:2 * KD], scalar=1.0, in1=g_sb[:, 0:KD],
                                                  op0=ALU.add, op1=ALU.mult))
    ch.op("dve", lambda e: e.tensor_scalar(out=A[:, 0:KD], in0=A[:, 0:KD], scalar1=float(np.sqrt(D)), scalar2=None,
                                           op0=ALU.mult))


def build_l0(cfg, stage=99):
    nc = bass.Bass("TRN2", target_bir_lowering=False)
    D, KD, T, H, G, DFF = cfg.D, cfg.KD, cfg.T, cfg.H, cfg.G, cfg.DFF
    KG = KD // 4
    xin = _din(nc, "xin", [HALO + T, D])
    condT_d = _din(nc, "condT", [128, KD])
    adaw00 = _din(nc, "adaw00", [D, 3 * D])
    adab00 = _din(nc, "adab00T", [128, 3 * KD])
    adaw01 = _din(nc, "adaw01", [D, 3 * D])
    adab01 = _din(nc, "adab01T", [128, 3 * KD])
    kvadaw = _din(nc, "kvadaw", [D, 2 * D])
    kvadab = _din(nc, "kvadabT", [128, 2 * KD])
    gvecs_d = _din(nc, "gvecs", [128, 4, KD])
    kg_d = _din(nc, "kgT", [128, 1])
    poolw = _din(nc, "poolw", [4, G, G])
    w1 = _din(nc, "w1", [D, DFF])
    w3 = _din(nc, "w3", [D, DFF])
    w2 = _din(nc, "w2", [DFF, D])
    wk = _din(nc, "wk", [D, D])
    wv = _din(nc, "wv", [D, D])
    ident_d = _din(nc, "ident", [128, 128])
    onesbf_d = _din(nc, "onesbf", [128, 128], BF16)
    hmask_d = _din(nc, "hmask", [128, 2])
    invc_d = _din(nc, "invc", [128, 4, HALO])
    xT = _dout(nc, "xT", [KD, 128, HALO + T])
    knT = _dout(nc, "knT", [H, 128, T], BF16)
    v_o = _dout(nc, "v", [T, D], BF16)
    aT = _dint(nc, "aT", [DFF // 128, 128, T], BF16)
    kraw = _dint(nc, "kraw", [H, 128, T])

    with ExitStack() as es:
        cx = Ctx(nc, cfg, es)
        sb = lambda name, shape, dt=F32: es.enter_context(nc.sbuf_tensor("s_" + name, shape, dt))
        ident = sb("ident", [128, 128])
        onesbf = sb("onesbf", [128, 128], BF16)
        condS = sb("condS", [128, KD])
        b00, b01, bkv = sb("b00", [128, 3 * KD]), sb("b01", [128, 3 * KD]), sb("bkv", [128, 2 * KD])
        M00, M01, MKV = sb("M00", [128, 3 * KD]), sb("M01", [128, 3 * KD]), sb("MKV", [128, 2 * KD])
        gv = sb("gv", [128, 4, KD])
        kg = sb("kg", [128, 1])
        hmask = sb("hmask", [128, 2])
        invc = sb("invc", [128, 4, HALO])
        A00, A01, AKV, GT0 = sb("A00", [128, KD]), sb("A01", [128, KD]), sb("AKV", [128, KD]), sb("GT0", [128, KD])

        def post(ch):
            ch.op("dve", lambda e: e.memset(cx.epsD[:, :], float(D * EPS)))
            ch.op("dve", lambda e: e.memset(cx.epsE[:, :], float(128 * EPS)))
            ch.op("act", lambda e: e.activation(out=condS[:, :], in_=condS[:, :], func=AF.Silu))
            ch.op("dve", lambda e: e.tensor_scalar(out=kg[:, :], in0=kg[:, :], scalar1=float(np.sqrt(128.0)),
                                                   scalar2=None, op0=ALU.mult))
        phase_loads(cx, [(ident[:, :], ident_d), (onesbf[:, :], onesbf_d), (condS[:, :], condT_d),
                         (b00[:, :], adab00), (b01[:, :], adab01), (bkv[:, :], kvadab), (gv[:, :, :], gvecs_d),
                         (kg[:, :], kg_d), (hmask[:, :], hmask_d), (invc[:, :, :], invc_d)], post)
        phase_ada(cx, adaw00, b00, condS, M00, 3 * KD)
        phase_ada(cx, adaw01, b01, condS, M01, 3 * KD)
        phase_ada(cx, kvadaw, bkv, condS, MKV, 2 * KD)

        def derive(ch):
            _derive_AB(ch, A00, M00, gv[:, 0, :], KD, D)
            _derive_AB(ch, A01, M01, gv[:, 1, :], KD, D)
            _derive_AB(ch, AKV, MKV, gv[:, 2, :], KD, D)
            ch.op("dve", lambda e: e.tensor_tensor(out=GT0[:, :], in0=M00[:, 2 * KD:3 * KD], in1=gv[:, 3, :],
                                                   op=ALU.mult))
        phase_loads(cx, [], derive)
        phase_transpose_in(cx, xin, xT, ident, HALO + T)
        seg = ExitStack()
        cx.alloc_in(seg)
        if stage >= 1:
            phase_norm(cx, xT, HALO, A00, M00[:, 0:KD], onesbf, mode="pool", pool={"hmask": hmask, "invc": invc})
        if stage >= 2:
            for g in range(4):
                tiles = []
                for cb in range(G // 256):
                    tiles.append({"ap": poolw[g, :, cb * 256:(cb + 1) * 256],
                                  "jobs": [g * KG + cb * 2, g * KG + cb * 2 + 1]})
                phase_gemm(cx, f"pool{g}", KG, g * KG, tiles, EpiResid(xT, HALO, GT0))
        if stage >= 3:
            phase_norm(cx, xT, HALO, A01, M01[:, 0:KD], onesbf)
            tiles = []
            for j in range(DFF // 128):
                tiles.append({"ap": w1[:, j * 128:(j + 1) * 128], "jobs": [(0, j, 0)]})
                tiles.append({"ap": w3[:, j * 128:(j + 1) * 128], "jobs": [(1, j, 0)]})
            phase_gemm(cx, "ffn1", KD, 0, tiles, EpiSwiglu(aT), WCOLS=128)
        if stage >= 4:
            nfc = DFF // 128
            for c0 in range(0, nfc, 32):
                kc = min(32, nfc - c0)
                tiles = [{"ap": w2[c0 * 128:(c0 + kc) * 128, cb * 256:(cb + 1) * 256], "jobs": [cb * 2, cb * 2 + 1]}
                         for cb in range(D // 256)]
                phase_gemm(cx, "ffn2", kc, 0, tiles, EpiResid(xT, HALO, M01[:, 2 * KD:3 * KD]),
                           in_src=aT[c0:c0 + kc, :, :])
        if stage >= 5:
            phase_norm(cx, xT, HALO, AKV, MKV[:, 0:KD], onesbf)
            tiles = [{"ap": wk[:, cb * 256:(cb + 1) * 256], "jobs": [cb * 2, cb * 2 + 1]} for cb in range(D // 256)]
            phase_gemm(cx, "kproj", KD, 0, tiles,
                       EpiRaw(lambda ch_, tb: kraw[ch_, :, tb * 512:(tb + 1) * 512], F32, 512))
            phase_headnorm(cx, kraw, knT, H, kg, onesbf)
            tiles = [{"ap": wv[:, cb * 256:(cb + 1) * 256], "jobs": [cb]} for cb in range(D // 256)]
            phase_gemm(cx, "vproj", KD, 0, tiles,
                       EpiRaw(lambda cb, tt: v_o[tt * 128:(tt + 1) * 128, cb * 256:(cb + 1) * 256], BF16, 256),
                       mode="tm")
        seg.close()
    return nc


def _colT(vec, KD):
    return np.ascontiguousarray(np.asarray(vec, np.float32).reshape(KD, 128).T)


def prep_l0(inp, cfg, core):
    b, half = divmod(core, 2)
    D, KD, T = cfg.D, cfg.KD, cfg.T
    t0 = half * T
    x = inp["x"]
    xin = np.zeros((HALO + T, D), np.float32)
    xin[HALO:] = x[b, t0:t0 + T]
    if half == 1:
        xin[:HALO] = x[b, t0 - HALO:t0]
    hmask = np.full((128, 2), float(half), np.float32)
    invc = np.zeros((128, 4, HALO), np.float32)
    for g, k in enumerate((2, 4, 8, 16)):
        for t in range(HALO):
            invc[:, g, t] = (1.0 / min(t + 1, k)) if half == 0 else 1.0 / k
    gvecs = np.stack([_colT(inp["norm_g"][0, 0], KD), _colT(inp["norm_g"][0, 1], KD), _colT(inp["kv_norm_g"], KD),
                      _colT(inp["pool_scale"][0], KD)], axis=1)
    return {
        "xin": xin, "condT": _colT(inp["c"][b], KD),
        "adaw00": inp["ada_w"][0, 0], "adab00T": _colT(inp["ada_b"][0, 0], 3 * KD),
        "adaw01": inp["ada_w"][0, 1], "adab01T": _colT(inp["ada_b"][0, 1], 3 * KD),
        "kvadaw": inp["kv_ada_w"], "kvadabT": _colT(inp["kv_ada_b"], 2 * KD),
        "gvecs": np.ascontiguousarray(gvecs), "kgT": np.asarray(inp["k_norm_g"], np.float32).reshape(128, 1),
        "poolw": inp["pool_w"][0], "w1": inp["ffn_w1"][0], "w3": inp["ffn_w3"][0], "w2": inp["ffn_w2"][0],
        "wk": inp["w_k"], "wv": inp["w_v"],
        "ident": np.eye(128, dtype=np.float32), "onesbf": np.ones((128, 128), ml_dtypes.bfloat16),
        "hmask": hmask, "invc": invc,
    }


def phase_bias_setup(cx, rb_d, oh_d, gq_row_d, gk_row_d, rbrow_d, extrep, NEGC, ones32):
    nc, cfg = cx.nc, cx.cfg
    H = cfg.H
    with ExitStack() as es:
        RB = cx.sb(es, "rb", [32, 3, H], F32)
        OH = cx.sb(es, "oh", [32, 3, 129], F32)
        EXT = cx.sb(es, "ext", [H, 3, 384], F32)
        ROW = cx.sb(es, "row", [1, 3 * 32 * H + 256], F32)
        SC = cx.sb(es, "sc", [1, 16], F32)
        cs, ds = cx.sem(es, "bc"), cx.sem(es, "bd")
        P = Prog()
        NR = 3 * 32 * H
        P.dma("sp", RB[:, :, :], rb_d, inc=ds)
        P.dma("sp", OH[:, :, :], oh_d, inc=ds)
        P.dma("sp", ROW[0:1, 0:NR], rbrow_d, inc=ds)
        P.dma("sp", ROW[0:1, NR:NR + 128], gq_row_d, inc=ds)
        P.dma("sp", ROW[0:1, NR + 128:NR + 256], gk_row_d, inc=ds)
        ch = Chain(P, cs, ds)
        ch.last = (ds, ds.n)
        ch.op("dve", lambda e: e.memset(EXT[:, :, :], NEG))

        def mm(e):
            ins = None
            for g in range(3):
                ins = e.matmul(cx.ps[g][0:H, 0:129], lhsT=RB[:, g, :], rhs=OH[:, g, :], start=True, stop=True)
            return ins
        ch.op("pe", mm)

        def cp(e):
            ins = None
            for g in range(3):
                ins = e.tensor_copy(out=EXT[:, g, 127:256], in_=cx.ps[g][0:H, 0:129])
            return ins
        ch.op("dve", cp)
        for g in range(3):
            ch.dma("sp", extrep[g], EXT[:, g, :].unsqueeze(1).broadcast_to([H, 128, 384]))
        ch.op("dve", lambda e: e.tensor_tensor(out=ROW[0:1, :], in0=ROW[0:1, :], in1=ROW[0:1, :], op=ALU.mult))

        def red(e):
            e.tensor_reduce(out=SC[0:1, 0:1], in_=ROW[0:1, NR:NR + 128], axis=mybir.AxisListType.X, op=ALU.max)
            e.tensor_reduce(out=SC[0:1, 1:2], in_=ROW[0:1, NR + 128:NR + 256], axis=mybir.AxisListType.X, op=ALU.max)
            return e.tensor_reduce(out=SC[0:1, 2:3], in_=ROW[0:1, 0:NR], axis=mybir.AxisListType.X, op=ALU.max)
        ch.op("dve", red)
        ch.op("dve", lambda e: e.tensor_tensor(out=SC[0:1, 3:4], in0=SC[0:1, 0:1], in1=SC[0:1, 1:2], op=ALU.mult))
        ch.op("act", lambda e: e.activation(out=SC[0:1, 4:6], in_=SC[0:1, 2:4], func=AF.Sqrt))
        ch.op("dve", lambda e: e.tensor_scalar(out=SC[0:1, 6:7], in0=SC[0:1, 5:6], scalar1=float(-np.sqrt(128.0) * 1.001),
                                               scalar2=None, op0=ALU.mult))
        ch.op("dve", lambda e: e.tensor_tensor(out=SC[0:1, 7:8], in0=SC[0:1, 6:7], in1=SC[0:1, 4:5], op=ALU.subtract))
        ch.op("pe", lambda e: e.matmul(cx.ps[4][:, 0:1], lhsT=ones32[0:1, :], rhs=SC[0:1, 7:8], start=True, stop=True))
        ch.op("dve", lambda e: e.tensor_copy(out=NEGC[:, 0:1], in_=cx.ps[4][:, 0:1]))
        emit(nc, P)


def phase_attention(cx, qnT, kn_prev, kn_own, v_prev, v_own, extrep, oT, NEGC, cmask, onesbf):
    nc, cfg = cx.nc, cx.cfg
    T, H, D = cfg.T, cfg.H, cfg.D
    NBW = 2 * T // 128
    with ExitStack() as es:
        QN = [cx.sb(es, "aq", [128, T], BF16) for _ in range(3)]
        KW = cx.sb(es, "akw", [128, 2 * T], BF16)
        VT = [cx.sb(es, "avt", [128, 2, d, NBW // d // 2, 128], BF16) for (_, d) in PATTERNS]
        BA = [cx.sb(es, "aba", [128, 4, 256], F32) for _ in range(3)]
        BB = [cx.sb(es, "abb", [128, 4, 256], F32) for _ in range(3)]
        BM = cx.sb(es, "abm", [128, 4, 256], F32)
        TT = cx.sb(es, "att", [128, 4, 256], F32)
        PT = cx.sb(es, "apt", [128, 4, 256], BF16)
        ACC = cx.sb(es, "aacc", [128, 2, T], F32)
        RC = cx.sb(es, "arc", [128, T], F32)
        OT = cx.sb(es, "aot", [128, T], BF16)
        cs, ds = cx.sem(es, "ac"), cx.sem(es, "ad")
        P = Prog()
        ch = Chain(P, cs, ds)
        ext_t = extrep.tensor
        for h in range(H):
            for g, (_, d) in enumerate(PATTERNS):
                ch.dma("sp", QN[g][:, :], qnT[g * H + h, :, :])
                nbh = NBW // d // 2
                ch.dma("sp", VT[g][:, 0, :, :, :],
                       v_prev[:, h * 128:(h + 1) * 128].rearrange("(nb j r) e -> j r nb e", j=128, r=d))
                ch.dma("sp", VT[g][:, 1, :, :, :],
                       v_own[:, h * 128:(h + 1) * 128].rearrange("(nb j r) e -> j r nb e", j=128, r=d))
                off = ((g * H + h) * 128) * 384 + 127
                src = bass.AP(tensor=ext_t, offset=off, ap=[[383, 128], [0, 4], [1, 256]])
                ch.dma("sp", BA[g][:, :, :], src)
            ch.dma("sp", KW[:, 0:T], kn_prev[h, :, :])
            ch.dma("sp", KW[:, T:2 * T], kn_own[h, :, :])

            def mkb(e):
                ins = None
                for g in range(3):
                    e.tensor_copy(out=BB[g][:, :, 0:128], in_=BA[g][:, :, 0:128])
                    ins = e.tensor_scalar(out=BB[g][:, :, 128:256], in0=BA[g][:, :, 128:256], scalar1=cmask[:, 0:1],
                                          scalar2=None, op0=ALU.add)
                e.tensor_copy(out=BM[:, 1:4, :], in_=BA[0][:, 1:4, :])
                return ins
            ch.op("dve", mkb)
            ch.op("dve", lambda e: e.tensor_copy(out=BM[:, 0:1, :], in_=BB[0][:, 0:1, :]))
            for g, (_, d) in enumerate(PATTERNS):
                nl_n = T // (128 * d)
                units = [(r, nl) for nl in range(nl_n) for r in range(d)]
                qv = QN[g][:, :].rearrange("p (m r) -> p r m", r=d)
                kv = KW[:, :].rearrange("p (m r) -> p r m", r=d)
                av = ACC[:, :, :].rearrange("p a (m r) -> p a r m", r=d)
                for b0 in range(0, len(units), 4):
                    ub = units[b0:b0 + 4]
                    if all(nl == 0 for (_, nl) in ub):
                        bias = BB[g]
                    elif any(nl == 0 for (_, nl) in ub):
                        assert g == 0 and b0 == 0
                        bias = BM
                    else:
                        bias = BA[g]

                    def s1(e, ub=ub, qv=qv, kv=kv, d=d):
                        ins = None
                        for u, (r, nl) in enumerate(ub):
                            m0 = T // d + 128 * nl
                            for blk in range(2):
                                ins = e.matmul(cx.ps[u // 2][:, (u % 2) * 256 + blk * 128:(u % 2) * 256 + blk * 128 + 128],
                                               lhsT=kv[:, r, m0 - blk * 128:m0 - blk * 128 + 128],
                                               rhs=qv[:, r, 128 * nl:128 * nl + 128], start=True, stop=True)
                        return ins
                    ch.op("pe", s1)

                    def s2(e, bias=bias):
                        ins = None
                        for bk in range(2):
                            ins = e.tensor_tensor(out=TT[:, 2 * bk:2 * bk + 2, :],
                                                  in0=cx.ps[bk][:, :].rearrange("p (u c) -> p u c", u=2),
                                                  in1=bias[:, 2 * bk:2 * bk + 2, :], op=ALU.add)
                        return ins
                    ch.op("dve", s2)
                    ch.op("act", lambda e: e.activation(out=PT[:, :, :], in_=TT[:, :, :], func=AF.Exp,
                                                        bias=NEGC[:, 0:1], scale=1.0))

                    def s4(e, ub=ub, g=g, d=d):
                        ins = None
                        for u, (r, nl) in enumerate(ub):
                            nbc = T // (128 * d) + nl
                            ob = cx.ps[2 + u // 2]
                            o0 = (u % 2) * 256
                            nbh = T // (128 * d)
                            for blk in range(2):
                                nbw = nbc - blk
                                e.matmul(ob[:, o0:o0 + 128], lhsT=VT[g][:, nbw // nbh, r, nbw % nbh, :],
                                         rhs=PT[:, u, blk * 128:(blk + 1) * 128], start=(blk == 0), stop=(blk == 1))
                            for blk in range(2):
                                ins = e.matmul(ob[:, o0 + 128:o0 + 256], lhsT=onesbf[:, :],
                                               rhs=PT[:, u, blk * 128:(blk + 1) * 128], start=(blk == 0), stop=(blk == 1))
                        return ins
                    ch.op("pe", s4)

                    def s5(e, ub=ub, g=g, av=av):
                        ins = None
                        for u, (r, nl) in enumerate(ub):
                            src = cx.ps[2 + u // 2][:, (u % 2) * 256:(u % 2) * 256 + 256].rearrange(
                                "p (a q) -> p a q", a=2)
                            dst = av[:, :, r, 128 * nl:128 * nl + 128]
                            if g == 0:
                                ins = e.tensor_copy(out=dst, in_=src)
                            else:
                                ins = e.tensor_tensor(out=dst, in0=dst, in1=src, op=ALU.add)
                        return ins
                    ch.op("dve", s5)
            ch.op("dve", lambda e: e.reciprocal(out=RC[:, :], in_=ACC[:, 1, :]))
            ch.op("dve", lambda e: e.tensor_tensor(out=OT[:, :], in0=ACC[:, 0, :], in1=RC[:, :], op=ALU.mult))
            ch.dma("sp", oT[h, :, :], OT[:, :])
        ch.op("dve", lambda e: e.memset(RC[0:1, 0:1], 0.0))
        emit(nc, P)


def build_l1(cfg, stage=99):
    nc = bass.Bass("TRN2", target_bir_lowering=False)
    D, KD, T, H, NE, DFE = cfg.D, cfg.KD, cfg.T, cfg.H, cfg.NE, cfg.DFE
    xT_in = _din(nc, "xT_in", [KD, 128, T])
    knTw = _din(nc, "knTw", [H, 128, 2 * T], BF16)
    vw = _din(nc, "vw", [2 * T, D], BF16)
    condT_d = _din(nc, "condT", [128, KD])
    adaw10 = _din(nc, "adaw10", [D, 3 * D])
    adab10 = _din(nc, "adab10T", [128, 3 * KD])
    adaw11 = _din(nc, "adaw11", [D, 3 * D])
    adab11 = _din(nc, "adab11T", [128, 3 * KD])
    gvecs_d = _din(nc, "gvecs", [128, 2, KD])
    qg_d = _din(nc, "qgT", [128, 1])
    wq = _din(nc, "wq", [D, 3 * D])
    wo = _din(nc, "wo", [D, D])
    rb_d = _din(nc, "relb", [32, 3, H])
    rbrow_d = _din(nc, "relbrow", [1, 3 * 32 * H])
    gqrow_d = _din(nc, "gqrow", [1, 128])
    gkrow_d = _din(nc, "gkrow", [1, 128])
    oh_d = _din(nc, "oh", [32, 3, 129])
    cmask_d = _din(nc, "cmask", [128, 1])
    rw_d = _din(nc, "routerw", [D, NE])
    mw1 = _din(nc, "mw1", [NE, D, DFE])
    mw3 = _din(nc, "mw3", [NE, D, DFE])
    mw2 = _din(nc, "mw2", [NE * DFE, D])
    ident_d = _din(nc, "ident", [128, 128])
    ones32_d = _din(nc, "ones32", [128, 128])
    onesbf_d = _din(nc, "onesbf", [128, 128], BF16)
    out = _dout(nc, "out", [T, D])
    xT = _dint(nc, "xT2", [KD, 128, T])
    qraw = _dint(nc, "qraw", [3 * H, 128, T])
    qnT = _dint(nc, "qnT", [3 * H, 128, T], BF16)
    oT = _dint(nc, "oT", [H, 128, T], BF16)
    extrep = _dint(nc, "extrep", [3, H, 128, 384])
    comb = _dint(nc, "comb", [NE, 128, T])
    NFC = NE * DFE // 128
    aT = _dint(nc, "aT2", [NFC, 128, T], BF16)

    with ExitStack() as es:
        cx = Ctx(nc, cfg, es)
        sb = lambda name, shape, dt=F32: es.enter_context(nc.sbuf_tensor("s_" + name, shape, dt))
        ident = sb("ident", [128, 128])
        ones32 = sb("ones32", [128, 128])
        onesbf = sb("onesbf", [128, 128], BF16)
        condS = sb("condS", [128, KD])
        b10, b11 = sb("b10", [128, 3 * KD]), sb("b11", [128, 3 * KD])
        M10, M11 = sb("M10", [128, 3 * KD]), sb("M11", [128, 3 * KD])
        gv = sb("gv", [128, 2, KD])
        qg = sb("qg", [128, 1])
        cmask = sb("cmask", [128, 1])
        NEGC = sb("NEGC", [128, 1])
        rw = sb("rw", [128, KD, NE])
        A10, A11 = sb("A10", [128, KD]), sb("A11", [128, KD])

        def post(ch):
            ch.op("dve", lambda e: e.memset(cx.epsD[:, :], float(D * EPS)))
            ch.op("dve", lambda e: e.memset(cx.epsE[:, :], float(128 * EPS)))
            ch.op("act", lambda e: e.activation(out=condS[:, :], in_=condS[:, :], func=AF.Silu))
        loads = [(ident[:, :], ident_d), (ones32[:, :], ones32_d), (onesbf[:, :], onesbf_d), (condS[:, :], condT_d),
                 (b10[:, :], adab10), (b11[:, :], adab11), (gv[:, :, :], gvecs_d), (qg[:, :], qg_d),
                 (cmask[:, :], cmask_d), (rw[:, :, :], rw_d.rearrange("(c p) e -> p c e", p=128))]
        loads += [(xT[c, :, :], xT_in[c, :, :]) for c in range(KD)]
        phase_loads(cx, loads, post)
        phase_ada(cx, adaw10, b10, condS, M10, 3 * KD)
        phase_ada(cx, adaw11, b11, condS, M11, 3 * KD)

        def derive(ch):
            _derive_AB(ch, A10, M10, gv[:, 0, :], KD, D)
            _derive_AB(ch, A11, M11, gv[:, 1, :], KD, D)
        phase_loads(cx, [], derive)
        if stage >= 1:
            with ExitStack() as seg:
                cx.alloc_in(seg)
                phase_norm(cx, xT, 0, A10, M10[:, 0:KD], onesbf)
                tiles = [{"ap": wq[:, cb * 256:(cb + 1) * 256], "jobs": [cb * 2, cb * 2 + 1]}
                         for cb in range(3 * D // 256)]
                phase_gemm(cx, "qproj", KD, 0, tiles,
                           EpiRaw(lambda ch_, tb: qraw[ch_, :, tb * 512:(tb + 1) * 512], F32, 512))
            phase_headnorm(cx, qraw, qnT, 3 * H, qg, onesbf)
        if stage >= 2:
            phase_bias_setup(cx, rb_d, oh_d, gqrow_d, gkrow_d, rbrow_d, extrep, NEGC, ones32)
            phase_attention(cx, qnT, knTw[:, :, 0:T], knTw[:, :, T:2 * T], vw[0:T, :], vw[T:2 * T, :], extrep, oT,
                            NEGC, cmask, onesbf)
        seg2 = ExitStack()
        cx.alloc_in(seg2)
        if stage >= 3:
            tiles = [{"ap": wo[:, cb * 256:(cb + 1) * 256], "jobs": [cb * 2, cb * 2 + 1]} for cb in range(D // 256)]
            phase_gemm(cx, "oproj", KD, 0, tiles, EpiResid(xT, 0, M10[:, 2 * KD:3 * KD]), in_src=oT)
        if stage >= 4:
            phase_norm(cx, xT, 0, A11, M11[:, 0:KD], onesbf, mode="router",
                       router={"rw": rw, "ident": ident, "ones32": ones32, "comb": comb})
            tiles = []
            for ex in range(NE):
                for j in range(DFE // 128):
                    fc = ex * (DFE // 128) + j
                    tiles.append({"ap": mw1[ex, :, j * 128:(j + 1) * 128], "jobs": [(0, fc, ex)]})
                    tiles.append({"ap": mw3[ex, :, j * 128:(j + 1) * 128], "jobs": [(1, fc, ex)]})
            phase_gemm(cx, "moe1", KD, 0, tiles, EpiSwiglu(aT, comb), WCOLS=128)
            for c0 in range(0, NFC, 32):
                kc = min(32, NFC - c0)
                tiles = [{"ap": mw2[c0 * 128:(c0 + kc) * 128, cb * 256:(cb + 1) * 256], "jobs": [cb * 2, cb * 2 + 1]}
                         for cb in range(D // 256)]
                phase_gemm(cx, "moe2", kc, 0, tiles, EpiResid(xT, 0, M11[:, 2 * KD:3 * KD]),
                           in_src=aT[c0:c0 + kc, :, :])
        seg2.close()
        phase_transpose_out(cx, xT, 0, out, ident)
    return nc


def _t5_bucket_np(n):
    n = np.asarray(n, np.int64)
    max_exact = 16
    nf = np.maximum(n, 1).astype(np.float32)
    large = max_exact + (np.log(nf / np.float32(max_exact)) / np.float32(np.log(2048 / max_exact))
                         * np.float32(32 - max_exact)).astype(np.int32)
    return np.where(n < max_exact, n, np.minimum(large, 31))


def prep_l1(inp, cfg, core, xT1, knT_all, v_all):
    b, half = divmod(core, 2)
    D, KD, T, H = cfg.D, cfg.KD, cfg.T, cfg.H
    knTw = np.zeros((H, 128, 2 * T), ml_dtypes.bfloat16)
    vw = np.zeros((2 * T, D), ml_dtypes.bfloat16)
    knTw[:, :, T:] = knT_all[core]
    vw[T:] = v_all[core]
    if half == 1:
        knTw[:, :, :T] = knT_all[core - 1]
        vw[:T] = v_all[core - 1]
    oh = np.zeros((32, 3, 129), np.float32)
    for g, (_, d) in enumerate(PATTERNS):
        bk = _t5_bucket_np(np.arange(129) * d)
        oh[bk, g, np.arange(129)] = 1.0
    gvecs = np.stack([_colT(inp["norm_g"][1, 0], KD), _colT(inp["norm_g"][1, 1], KD)], axis=1)
    cm = np.full((128, 1), NEG if half == 0 else 0.0, np.float32)
    NE, DFE = cfg.NE, cfg.DFE
    return {
        "xT_in": xT1, "knTw": knTw, "vw": vw, "condT": _colT(inp["c"][b], KD),
        "adaw10": inp["ada_w"][1, 0], "adab10T": _colT(inp["ada_b"][1, 0], 3 * KD),
        "adaw11": inp["ada_w"][1, 1], "adab11T": _colT(inp["ada_b"][1, 1], 3 * KD),
        "gvecs": np.ascontiguousarray(gvecs), "qgT": np.asarray(inp["q_norm_g"][0], np.float32).reshape(128, 1),
        "wq": inp["w_q"][0], "wo": inp["w_o"][0], "relb": np.ascontiguousarray(inp["rel_bias"]),
        "relbrow": np.ascontiguousarray(inp["rel_bias"]).reshape(1, -1),
        "gqrow": np.asarray(inp["q_norm_g"][0], np.float32).reshape(1, 128),
        "gkrow": np.asarray(inp["k_norm_g"], np.float32).reshape(1, 128),
        "oh": oh, "cmask": cm, "routerw": inp["router_w"][0],
        "mw1": inp["moe_w1"][0], "mw3": inp["moe_w3"][0], "mw2": inp["moe_w2"][0].reshape(NE * DFE, D),
        "ident": np.eye(128, dtype=np.float32), "ones32": np.ones((128, 128), np.float32),
        "onesbf": np.ones((128, 128), ml_dtypes.bfloat16),
    }


def _l0_pass(cx, cfg, tag, xin, xT, knT, v_o, aT, kraw, w, sbt):
    nc = cx.nc
    D, KD, T, H, G, DFF = cfg.D, cfg.KD, cfg.T, cfg.H, cfg.G, cfg.DFF
    KG = KD // 4
    phase_transpose_in(cx, xin, xT, sbt["ident"], HALO + T)
    with ExitStack() as seg:
        cx.alloc_in(seg)
        phase_norm(cx, xT, HALO, sbt["A00"], sbt["M00"][:, 0:KD], sbt["onesbf"], mode="pool",
                   pool={"hmask": sbt["hmask" + tag], "invc": sbt["invc" + tag]})
        for g in range(4):
            tiles = []
            for cb in range(G // 256):
                tiles.append({"ap": w["poolw"][g, :, cb * 256:(cb + 1) * 256],
                              "jobs": [g * KG + cb * 2, g * KG + cb * 2 + 1]})
            phase_gemm(cx, f"pool{g}", KG, g * KG, tiles, EpiResid(xT, HALO, sbt["GT0"]))
        phase_norm(cx, xT, HALO, sbt["A01"], sbt["M01"][:, 0:KD], sbt["onesbf"])
        tiles = []
        for j in range(DFF // 128):
            tiles.append({"ap": w["w1"][:, j * 128:(j + 1) * 128], "jobs": [(0, j, 0)]})
            tiles.append({"ap": w["w3"][:, j * 128:(j + 1) * 128], "jobs": [(1, j, 0)]})
        phase_gemm(cx, "ffn1", KD, 0, tiles, EpiSwiglu(aT), WCOLS=128)
        nfc = DFF // 128
        for c0 in range(0, nfc, 32):
            kc = min(32, nfc - c0)
            tiles = [{"ap": w["w2"][c0 * 128:(c0 + kc) * 128, cb * 256:(cb + 1) * 256], "jobs": [cb * 2, cb * 2 + 1]}
                     for cb in range(D // 256)]
            phase_gemm(cx, "ffn2", kc, 0, tiles, EpiResid(xT, HALO, sbt["M01"][:, 2 * KD:3 * KD]),
                       in_src=aT[c0:c0 + kc, :, :])
        phase_norm(cx, xT, HALO, sbt["AKV"], sbt["MKV"][:, 0:KD], sbt["onesbf"])
        tiles = [{"ap": w["wk"][:, cb * 256:(cb + 1) * 256], "jobs": [cb * 2, cb * 2 + 1]} for cb in range(D // 256)]
        phase_gemm(cx, "kproj", KD, 0, tiles,
                   EpiRaw(lambda ch_, tb: kraw[ch_, :, tb * 512:(tb + 1) * 512], F32, 512))
        phase_headnorm(cx, kraw, knT, H, sbt["kg"], sbt["onesbf"])
        tiles = [{"ap": w["wv"][:, cb * 256:(cb + 1) * 256], "jobs": [cb]} for cb in range(D // 256)]
        phase_gemm(cx, "vproj", KD, 0, tiles,
                   EpiRaw(lambda cb, tt: v_o[tt * 128:(tt + 1) * 128, cb * 256:(cb + 1) * 256], BF16, 256),
                   mode="tm")


def build_fused(cfg):
    nc = bass.Bass("TRN2", target_bir_lowering=False)
    D, KD, T, H, G, DFF, NE, DFE = cfg.D, cfg.KD, cfg.T, cfg.H, cfg.G, cfg.DFF, cfg.NE, cfg.DFE
    xinA = _din(nc, "xinA", [HALO + T, D])
    xinB = _din(nc, "xinB", [HALO + T, D])
    condT_d = _din(nc, "condT", [128, KD])
    adaw = {k: _din(nc, "adaw" + k, [D, 3 * D]) for k in ("00", "01", "10", "11")}
    adab = {k: _din(nc, "adab" + k + "T", [128, 3 * KD]) for k in ("00", "01", "10", "11")}
    kvadaw = _din(nc, "kvadaw", [D, 2 * D])
    kvadab = _din(nc, "kvadabT", [128, 2 * KD])
    gvecs_d = _din(nc, "gvecs", [128, 6, KD])
    kg_d = _din(nc, "kgT", [128, 1])
    qg_d = _din(nc, "qgT", [128, 1])
    w = {"poolw": _din(nc, "poolw", [4, G, G]), "w1": _din(nc, "w1", [D, DFF]), "w3": _din(nc, "w3", [D, DFF]),
         "w2": _din(nc, "w2", [DFF, D]), "wk": _din(nc, "wk", [D, D]), "wv": _din(nc, "wv", [D, D])}
    wq = _din(nc, "wq", [D, 3 * D])
    wo = _din(nc, "wo", [D, D])
    rb_d = _din(nc, "relb", [32, 3, H])
    rbrow_d = _din(nc, "relbrow", [1, 3 * 32 * H])
    gqrow_d = _din(nc, "gqrow", [1, 128])
    gkrow_d = _din(nc, "gkrow", [1, 128])
    oh_d = _din(nc, "oh", [32, 3, 129])
    cmask_d = _din(nc, "cmask", [128, 1])
    rw_d = _din(nc, "routerw", [D, NE])
    mw1 = _din(nc, "mw1", [NE, D, DFE])
    mw3 = _din(nc, "mw3", [NE, D, DFE])
    mw2 = _din(nc, "mw2", [NE * DFE, D])
    ident_d = _din(nc, "ident", [128, 128])
    ones32_d = _din(nc, "ones32", [128, 128])
    onesbf_d = _din(nc, "onesbf", [128, 128], BF16)
    hmA_d, hmB_d = _din(nc, "hmaskA", [128, 2]), _din(nc, "hmaskB", [128, 2])
    icA_d, icB_d = _din(nc, "invcA", [128, 4, HALO]), _din(nc, "invcB", [128, 4, HALO])
    out = _dout(nc, "out", [T, D])
    xTA = _dint(nc, "xTA", [KD, 128, HALO + T])
    xTB = _dint(nc, "xTB", [KD, 128, HALO + T])
    knA, knB = _dint(nc, "knA", [H, 128, T], BF16), _dint(nc, "knB", [H, 128, T], BF16)
    vA, vB = _dint(nc, "vA", [T, D], BF16), _dint(nc, "vB", [T, D], BF16)
    NFC = NE * DFE // 128
    aT = _dint(nc, "aT", [max(DFF // 128, NFC), 128, T], BF16)
    kraw = _dint(nc, "qkraw", [3 * H, 128, T])
    qnT = _dint(nc, "qnT", [3 * H, 128, T], BF16)
    oT = _dint(nc, "oT", [H, 128, T], BF16)
    extrep = _dint(nc, "extrep", [3, H, 128, 384])
    comb = _dint(nc, "comb", [NE, 128, T])

    with ExitStack() as es:
        cx = Ctx(nc, cfg, es)
        sb = lambda name, shape, dt=F32: es.enter_context(nc.sbuf_tensor("s_" + name, shape, dt))
        t = {}
        t["ident"], t["ones32"] = sb("ident", [128, 128]), sb("ones32", [128, 128])
        t["onesbf"] = sb("onesbf", [128, 128], BF16)
        condS = sb("condS", [128, KD])
        bT = {k: sb("b" + k, [128, 3 * KD]) for k in ("00", "01", "10", "11")}
        bkv = sb("bkv", [128, 2 * KD])
        for k in ("00", "01", "10", "11"):
            t["M" + k] = sb("M" + k, [128, 3 * KD])
            t["A" + k] = sb("A" + k, [128, KD])
        t["MKV"], t["AKV"], t["GT0"] = sb("MKV", [128, 2 * KD]), sb("AKV", [128, KD]), sb("GT0", [128, KD])
        gv = sb("gv", [128, 6, KD])
        t["kg"], qg = sb("kg", [128, 1]), sb("qg", [128, 1])
        t["hmaskA"], t["hmaskB"] = sb("hmaskA", [128, 2]), sb("hmaskB", [128, 2])
        t["invcA"], t["invcB"] = sb("invcA", [128, 4, HALO]), sb("invcB", [128, 4, HALO])
        cmask, NEGC = sb("cmask", [128, 1]), sb("NEGC", [128, 1])
        rw = sb("rw", [128, KD, NE])

        def post(ch):
            ch.op("dve", lambda e: e.memset(cx.epsD[:, :], float(D * EPS)))
            ch.op("dve", lambda e: e.memset(cx.epsE[:, :], float(128 * EPS)))
            ch.op("act", lambda e: e.activation(out=condS[:, :], in_=condS[:, :], func=AF.Silu))
            ch.op("dve", lambda e: e.tensor_scalar(out=t["kg"][:, :], in0=t["kg"][:, :], scalar1=float(np.sqrt(128.0)),
                                                   scalar2=None, op0=ALU.mult))
        loads = [(t["ident"][:, :], ident_d), (t["ones32"][:, :], ones32_d), (t["onesbf"][:, :], onesbf_d),
                 (condS[:, :], condT_d), (bkv[:, :], kvadab), (gv[:, :, :], gvecs_d), (t["kg"][:, :], kg_d),
                 (qg[:, :], qg_d), (t["hmaskA"][:, :], hmA_d), (t["hmaskB"][:, :], hmB_d),
                 (t["invcA"][:, :, :], icA_d), (t["invcB"][:, :, :], icB_d), (cmask[:, :], cmask_d),
                 (rw[:, :, :], rw_d.rearrange("(c p) e -> p c e", p=128))]
        loads += [(bT[k][:, :], adab[k]) for k in ("00", "01", "10", "11")]
        phase_loads(cx, loads, post)
        for k in ("00", "01", "10", "11"):
            phase_ada(cx, adaw[k], bT[k], condS, t["M" + k], 3 * KD)
        phase_ada(cx, kvadaw, bkv, condS, t["MKV"], 2 * KD)

        def derive(ch):
            _derive_AB(ch, t["A00"], t["M00"], gv[:, 0, :], KD, D)
            _derive_AB(ch, t["A01"], t["M01"], gv[:, 1, :], KD, D)
            _derive_AB(ch, t["AKV"], t["MKV"], gv[:, 2, :], KD, D)
            _derive_AB(ch, t["A10"], t["M10"], gv[:, 4, :], KD, D)
            _derive_AB(ch, t["A11"], t["M11"], gv[:, 5, :], KD, D)
            ch.op("dve", lambda e: e.tensor_tensor(out=t["GT0"][:, :], in0=t["M00"][:, 2 * KD:3 * KD], in1=gv[:, 3, :],
                                                   op=ALU.mult))
        phase_loads(cx, [], derive)
        _l0_pass(cx, cfg, "A", xinA, xTA, knA, vA, aT, kraw, w, t)
        _l0_pass(cx, cfg, "B", xinB, xTB, knB, vB, aT, kraw, w, t)
        xT = xTB
        with ExitStack() as seg:
            cx.alloc_in(seg)
            phase_norm(cx, xT, HALO, t["A10"], t["M10"][:, 0:KD], t["onesbf"])
            tiles = [{"ap": wq[:, cb * 256:(cb + 1) * 256], "jobs": [cb * 2, cb * 2 + 1]} for cb in range(3 * D // 256)]
            phase_gemm(cx, "qproj", KD, 0, tiles,
                       EpiRaw(lambda ch_, tb: kraw[ch_, :, tb * 512:(tb + 1) * 512], F32, 512))
        phase_headnorm(cx, kraw, qnT, 3 * H, qg, t["onesbf"])
        phase_bias_setup(cx, rb_d, oh_d, gqrow_d, gkrow_d, rbrow_d, extrep, NEGC, t["ones32"])
        phase_attention(cx, qnT, knA, knB, vA, vB, extrep, oT, NEGC, cmask, t["onesbf"])
        with ExitStack() as seg:
            cx.alloc_in(seg)
            tiles = [{"ap": wo[:, cb * 256:(cb + 1) * 256], "jobs": [cb * 2, cb * 2 + 1]} for cb in range(D // 256)]
            phase_gemm(cx, "oproj", KD, 0, tiles, EpiResid(xT, HALO, t["M10"][:, 2 * KD:3 * KD]), in_src=oT)
            phase_norm(cx, xT, HALO, t["A11"], t["M11"][:, 0:KD], t["onesbf"], mode="router",
                       router={"rw": rw, "ident": t["ident"], "ones32": t["ones32"], "comb": comb})
            tiles = []
            for ex in range(NE):
                for j in range(DFE // 128):
                    fc = ex * (DFE // 128) + j
                    tiles.append({"ap": mw1[ex, :, j * 128:(j + 1) * 128], "jobs": [(0, fc, ex)]})
                    tiles.append({"ap": mw3[ex, :, j * 128:(j + 1) * 128], "jobs": [(1, fc, ex)]})
            phase_gemm(cx, "moe1", KD, 0, tiles, EpiSwiglu(aT, comb), WCOLS=128)
            for c0 in range(0, NFC, 32):
                kc = min(32, NFC - c0)
                tiles = [{"ap": mw2[c0 * 128:(c0 + kc) * 128, cb * 256:(cb + 1) * 256], "jobs": [cb * 2, cb * 2 + 1]}
                         for cb in range(D // 256)]
                phase_gemm(cx, "moe2", kc, 0, tiles, EpiResid(xT, HALO, t["M11"][:, 2 * KD:3 * KD]),
                           in_src=aT[c0:c0 + kc, :, :])
        phase_transpose_out(cx, xT, HALO, out, t["ident"])
    return nc


def prep_fused(inp, cfg, core):
    b, half = divmod(core, 2)
    D, KD, T, H, NE, DFE = cfg.D, cfg.KD, cfg.T, cfg.H, cfg.NE, cfg.DFE
    x = inp["x"]
    xinB = np.zeros((HALO + T, D), np.float32)
    xinB[HALO:] = x[b, half * T:(half + 1) * T]
    xinA = np.zeros((HALO + T, D), np.float32)
    if half == 1:
        xinB[:HALO] = x[b, T - HALO:T]
        xinA[HALO:] = x[b, 0:T]
    invc_start = np.zeros((128, 4, HALO), np.float32)
    invc_mid = np.zeros((128, 4, HALO), np.float32)
    for g, k in enumerate((2, 4, 8, 16)):
        for t_ in range(HALO):
            invc_start[:, g, t_] = 1.0 / min(t_ + 1, k)
            invc_mid[:, g, t_] = 1.0 / k
    oh = np.zeros((32, 3, 129), np.float32)
    for g, (_, d) in enumerate(PATTERNS):
        bk = _t5_bucket_np(np.arange(129) * d)
        oh[bk, g, np.arange(129)] = 1.0
    gvecs = np.stack([_colT(inp["norm_g"][0, 0], KD), _colT(inp["norm_g"][0, 1], KD), _colT(inp["kv_norm_g"], KD),
                      _colT(inp["pool_scale"][0], KD), _colT(inp["norm_g"][1, 0], KD), _colT(inp["norm_g"][1, 1], KD)],
                     axis=1)
    m = {
        "xinA": xinA, "xinB": xinB, "condT": _colT(inp["c"][b], KD),
        "kvadaw": inp["kv_ada_w"], "kvadabT": _colT(inp["kv_ada_b"], 2 * KD),
        "gvecs": np.ascontiguousarray(gvecs),
        "kgT": np.asarray(inp["k_norm_g"], np.float32).reshape(128, 1),
        "qgT": np.asarray(inp["q_norm_g"][0], np.float32).reshape(128, 1),
        "poolw": inp["pool_w"][0], "w1": inp["ffn_w1"][0], "w3": inp["ffn_w3"][0], "w2": inp["ffn_w2"][0],
        "wk": inp["w_k"], "wv": inp["w_v"], "wq": inp["w_q"][0], "wo": inp["w_o"][0],
        "relb": np.ascontiguousarray(inp["rel_bias"]),
        "relbrow": np.ascontiguousarray(inp["rel_bias"]).reshape(1, -1),
        "gqrow": np.asarray(inp["q_norm_g"][0], np.float32).reshape(1, 128),
        "gkrow": np.asarray(inp["k_norm_g"], np.float32).reshape(1, 128),
        "oh": oh, "cmask": np.full((128, 1), NEG if half == 0 else 0.0, np.float32),
        "routerw": inp["router_w"][0],
        "mw1": inp["moe_w1"][0], "mw3": inp["moe_w3"][0], "mw2": inp["moe_w2"][0].reshape(NE * DFE, D),
        "ident": np.eye(128, dtype=np.float32), "ones32": np.ones((128, 128), np.float32),
        "onesbf": np.ones((128, 128), ml_dtypes.bfloat16),
        "hmaskA": np.zeros((128, 2), np.float32), "hmaskB": np.full((128, 2), float(half), np.float32),
        "invcA": invc_start, "invcB": invc_start if half == 0 else invc_mid,
    }
    for i in range(2):
        for j in range(2):
            m[f"adaw{i}{j}"] = inp["ada_w"][i, j]
            m[f"adab{i}{j}T"] = _colT(inp["ada_b"][i, j], 3 * KD)
    return m


_CFG = Cfg()


def kernel(**inputs):
    cfg = _CFG
    inp = {k: np.asarray(v) for k, v in inputs.items()}
    n = 8
    nc = build_fused(cfg)
    maps = [prep_fused(inp, cfg, c) for c in range(n)]
    res = run_bass_kernel_spmd(nc, maps, core_ids=list(range(n))).results
    out = np.empty((cfg.B, cfg.SEQ, cfg.D), np.float32)
    for c in range(n):
        b, half = divmod(c, 2)
        out[b, half * cfg.T:(half + 1) * cfg.T] = res[c]["out"]
    return out
```

```python
import numpy as np
import ml_dtypes
from contextlib import ExitStack
import concourse.bass as bass
import concourse.mybir as mybir
from concourse.bass_utils import run_bass_kernel_spmd

F32 = mybir.dt.float32
BF16 = mybir.dt.bfloat16
AF = mybir.ActivationFunctionType
ALU = mybir.AluOpType
NEG = -1.0e30
EPS = 1e-6
HALO = 16
PATTERNS = ((128, 1), (512, 4), (2048, 16))


class Cfg:
    def __init__(self, D=4096, DFF=11008, DFE=3584, NE=8, SEQ=4096, B=4):
        self.D = D
        self.KD = D // 128
        self.H = D // 128
        self.DFF = DFF
        self.DFE = DFE
        self.NE = NE
        self.SEQ = SEQ
        self.B = B
        self.T = SEQ // 2
        self.G = D // 4


class Sem:
    def __init__(self, h):
        self.h = h
        self.n = 0


class Prog:
    def __init__(self):
        self.q = {k: [] for k in ("sp", "act", "dve", "pool", "pe")}

    key = None

    def op(self, eng, fn, waits=(), inc=None, k=1):
        if inc is not None:
            inc.n += k
        self.q[eng].append((tuple(waits), fn, inc, k, self.key))

    def merged(self, eng):
        q = self.q[eng]
        if any(o[4] is not None for o in q):
            assert all(o[4] is not None for o in q)
            q = sorted(q, key=lambda o: o[4])
        return q

    def dma(self, eng, out, in_, waits=(), inc=None):
        self.op(eng, lambda e, o=out, i=in_: e.dma_start(out=o, in_=i), waits, inc, 16)


class Chain:
    def __init__(self, P, cs, ds, cid=None):
        self.P, self.cs, self.ds = P, cs, ds
        self.last = None
        self.cid = cid
        self.step = 0

    def _key(self):
        if self.cid is not None:
            self.P.key = (self.step, self.cid)
            self.step += 1

    def op(self, eng, fn, extra=()):
        w = list(extra)
        if self.last:
            w.append(self.last)
        self._key()
        self.P.op(eng, fn, w, self.cs, 1)
        self.P.key = None
        self.last = (self.cs, self.cs.n)

    def dma(self, eng, out, in_, extra=()):
        w = list(extra)
        if self.last:
            w.append(self.last)
        self._key()
        self.P.dma(eng, out, in_, w, self.ds)
        self.P.key = None
        self.last = (self.ds, self.ds.n)


def _run(e, ops):
    seen = {}
    for waits, fn, inc, k, _key in ops:
        for (s, v) in waits:
            if v <= 0:
                continue
            key = id(s)
            if seen.get(key, 0) >= v:
                continue
            seen[key] = v
            e.wait_ge(s.h, v)
        ins = fn(e)
        if inc is not None:
            ins.then_inc(inc.h, k)


def emit(nc, P):
    with nc.Block() as block:
        if P.q["sp"]:
            @block.sync
            def _(e):
                _run(e, P.merged("sp"))
        if P.q["act"]:
            @block.scalar
            def _(e):
                _run(e, P.merged("act"))
        if P.q["dve"]:
            @block.vector
            def _(e):
                _run(e, P.merged("dve"))
        if P.q["pool"]:
            @block.gpsimd
            def _(e):
                _run(e, P.merged("pool"))
        if P.q["pe"]:
            @block.tensor
            def _(e):
                _run(e, P.merged("pe"))


class Ctx:
    def __init__(self, nc, cfg, es):
        self.nc = nc
        self.cfg = cfg
        self.ps = [es.enter_context(nc.psum_tensor(f"psb{i}", [128, 512], F32)) for i in range(8)]
        self.IN = None
        self.scr = es.enter_context(nc.sbuf_tensor("scr", [128, 16], F32))
        self.epsD = es.enter_context(nc.sbuf_tensor("epsD", [128, 1], F32))
        self.epsE = es.enter_context(nc.sbuf_tensor("epsE", [128, 1], F32))
        self.uid = 0
        self._phase_sems = {}

    def alloc_in(self, es):
        self.uid += 1
        self.IN = es.enter_context(self.nc.sbuf_tensor(f"IN_{self.uid}", [128, 32, self.cfg.T], BF16))

    def sb(self, es, name, shape, dt):
        self.uid += 1
        return es.enter_context(self.nc.sbuf_tensor(f"{name}_{self.uid}", shape, dt))

    def sem(self, es, name):
        self.uid += 1
        h = self.nc.alloc_semaphore(name=f"{name}_{self.uid}")
        lst = getattr(es, "_sem_list", None)
        if lst is None:
            lst = []
            es._sem_list = lst

            def cleanup(lst=lst):
                self.nc.clear_and_free_semaphores(lst)
                self.nc.all_engine_barrier()
            es.callback(cleanup)
        lst.append(h)
        return Sem(h)


def phase_loads(cx, pairs, post=None):
    with ExitStack() as es:
        cs, ds = cx.sem(es, "lc"), cx.sem(es, "ld")
        P = Prog()
        for (o, i) in pairs:
            P.dma("sp", o, i, inc=ds)
        ch = Chain(P, cs, ds)
        ch.last = (ds, ds.n)
        if post is not None:
            post(ch)
        else:
            ch.op("dve", lambda e: e.memset(cx.scr[0:1, 0:1], 0.0))
        emit(cx.nc, P)


def phase_ada(cx, w_ap, bT_sb, condS, mod_sb, NJ):
    nc, cfg = cx.nc, cx.cfg
    KD = cfg.KD
    with ExitStack() as es:
        NBUF = 2
        AW = [cx.sb(es, "aw", [128, KD, 256], F32) for i in range(NBUF)]
        ld = [cx.sem(es, "awld") for i in range(NBUF)]
        s_mm = cx.sem(es, "awmm")
        s_fin = cx.sem(es, "awfin")
        P = Prog()
        wv = w_ap.rearrange("(kc p) n -> p kc n", p=128)
        ntile = NJ // 2
        ps = cx.ps[0]
        for t in range(ntile):
            b = t % NBUF
            P.dma("sp", AW[b][:], wv[:, :, t * 256:(t + 1) * 256],
                  waits=[(s_mm, t - NBUF + 1)], inc=ld[b])
            for jj in range(2):
                j = t * 2 + jj
                for kc in range(KD):
                    last = (jj == 1 and kc == KD - 1)
                    P.op("pe",
                         lambda e, b=b, jj=jj, kc=kc, j=j: e.matmul(
                             ps[:, j:j + 1], lhsT=AW[b][:, kc, jj * 128:(jj + 1) * 128],
                             rhs=condS[:, kc:kc + 1], start=(kc == 0), stop=(kc == KD - 1)),
                         waits=[(ld[b], 16 * (t // NBUF + 1))],
                         inc=s_mm if last else None)
        P.op("dve", lambda e: e.tensor_tensor(out=mod_sb[:, 0:NJ], in0=ps[:, 0:NJ], in1=bT_sb[:, 0:NJ], op=ALU.add),
             waits=[(s_mm, ntile)], inc=s_fin)
        emit(nc, P)


def phase_transpose_in(cx, x_ap, xT_ap, ident, ntok_total):
    nc, cfg = cx.nc, cx.cfg
    KD, D = cfg.KD, cfg.D
    xTv = xT_ap.rearrange("c p t -> p c t")
    tiles = []
    t0 = 0
    rem = ntok_total % 128
    if rem:
        tiles.append((0, rem))
        t0 = rem
    while t0 < ntok_total:
        tiles.append((t0, 128))
        t0 += 128
    with ExitStack() as es:
        XI = cx.sb(es, "xi", [128, D], F32)
        XO = cx.sb(es, "xo", [128, KD, 128], F32)
        cs, ds = cx.sem(es, "tc"), cx.sem(es, "td")
        P = Prog()
        ch = Chain(P, cs, ds)
        for (t0, n) in tiles:
            ch.dma("sp", XI[0:n, :], x_ap[t0:t0 + n, :])
            for g0 in range(0, KD, 32):
                gcs = list(range(g0, min(KD, g0 + 32)))

                def tr(e, gcs=gcs, g0=g0, n=n):
                    ins = None
                    for c in gcs:
                        bank = cx.ps[((c - g0) // 4) % 8]
                        cc = (c - g0) % 4
                        ins = e.transpose(bank[:, cc * 128:cc * 128 + n], XI[0:n, c * 128:(c + 1) * 128],
                                          ident[0:n, 0:n])
                    return ins
                ch.op("pe", tr)
                nb = (len(gcs) + 3) // 4
                for bk in range(nb):
                    c0 = g0 + bk * 4
                    ncz = min(4, KD - c0)
                    src = cx.ps[bk][:, 0:ncz * 128].rearrange("p (c t) -> p c t", t=128)[:, :, 0:n]
                    dst = XO[:, c0:c0 + ncz, 0:n]
                    if bk % 2 == 0:
                        ch.op("act", lambda e, s=src, d=dst: e.copy(out=d, in_=s))
                    else:
                        ch.op("dve", lambda e, s=src, d=dst: e.tensor_copy(out=d, in_=s))
            ch.dma("sp", xTv[:, :, t0:t0 + n], XO[:, :, 0:n])
        ch.op("dve", lambda e: e.memset(XO[0:1, 0, 0:1], 0.0))
        emit(nc, P)


def phase_transpose_out(cx, xT_ap, col0, out_ap, ident):
    nc, cfg = cx.nc, cx.cfg
    KD, D, T = cfg.KD, cfg.D, cfg.T
    xTv = xT_ap.rearrange("c p t -> p c t")
    with ExitStack() as es:
        XI = cx.sb(es, "yi", [128, KD, 128], F32)
        XO = cx.sb(es, "yo", [128, D], F32)
        cs, ds = cx.sem(es, "tc"), cx.sem(es, "td")
        P = Prog()
        ch = Chain(P, cs, ds)
        for t0 in range(0, T, 128):
            ch.dma("sp", XI[:, :, :], xTv[:, :, col0 + t0:col0 + t0 + 128])
            for g0 in range(0, KD, 32):
                gcs = list(range(g0, min(KD, g0 + 32)))

                def tr(e, gcs=gcs, g0=g0):
                    ins = None
                    for c in gcs:
                        bank = cx.ps[((c - g0) // 4) % 8]
                        cc = (c - g0) % 4
                        ins = e.transpose(bank[:, cc * 128:(cc + 1) * 128], XI[:, c, :], ident[:, :])
                    return ins
                ch.op("pe", tr)
                nb = (len(gcs) + 3) // 4
                for bk in range(nb):
                    c0 = g0 + bk * 4
                    ncz = min(4, KD - c0)
                    src = cx.ps[bk][:, 0:ncz * 128]
                    dst = XO[:, c0 * 128:(c0 + ncz) * 128]
                    if bk % 2 == 0:
                        ch.op("act", lambda e, s=src, d=dst: e.copy(out=d, in_=s))
                    else:
                        ch.op("dve", lambda e, s=src, d=dst: e.tensor_copy(out=d, in_=s))
            ch.dma("sp", out_ap[t0:t0 + 128, :], XO[:, :])
        ch.op("dve", lambda e: e.memset(XO[0:1, 0:1], 0.0))
        emit(nc, P)


def phase_norm(cx, xT_ap, col0, A_sb, B_sb, ones_bf, mode="plain", pool=None, router=None):
    nc, cfg = cx.nc, cx.cfg
    KD, D, T = cfg.KD, cfg.D, cfg.T
    xTv = xT_ap.rearrange("c p t -> p c t")
    IN = cx.IN
    blocks = [(t, 128) for t in range(0, T, 128)]
    if mode == "pool":
        blocks = [(-HALO, HALO)] + blocks
    NCH = 2 if mode == "plain" else 1
    with ExitStack() as es:
        P = Prog()
        if mode == "pool":
            HB = cx.sb(es, "nhb", [128, KD, 128 + HALO], F32)
            S1 = cx.sb(es, "ns1", [128, KD // 4, 128 + HALO], F32)
            S2 = cx.sb(es, "ns2", [128, KD // 4, 128 + HALO], F32)
        if mode == "router":
            NE = cfg.NE
            HF = cx.sb(es, "nhf", [128, KD, 128], F32)
            LG = cx.sb(es, "nlg", [128, 16], F32)
            M8 = cx.sb(es, "nm8", [128, 16], F32)
            CB = cx.sb(es, "ncb", [128, 16], F32)
            DG = cx.sb(es, "ndg", [128, NE, 128], F32)
            CO = cx.sb(es, "nco", [128, NE, 128], F32)
            combv = router["comb"].rearrange("e p t -> p e t")
        for cid in range(NCH):
            XB = cx.sb(es, "nxb", [128, KD, 128], F32)
            SQ = cx.sb(es, "nsq", [128, KD, 128], BF16)
            RS = cx.sb(es, "nrs", [128, 128], F32)
            cs, ds = cx.sem(es, "nc"), cx.sem(es, "nd")
            ch = Chain(P, cs, ds, cid if NCH > 1 else None)
            psS = cx.ps[cid]
            for (t0, n) in blocks[cid::NCH]:
                ch.dma("sp", XB[:, :, 0:n], xTv[:, :, col0 + t0:col0 + t0 + n])
                ch.op("act", lambda e, n=n, SQ=SQ, XB=XB: e.activation(out=SQ[:, :, 0:n], in_=XB[:, :, 0:n],
                                                                       func=AF.Square))

                def ssmm(e, n=n, SQ=SQ, psS=psS):
                    ins = None
                    for c in range(KD):
                        ins = e.matmul(psS[:, 0:n], lhsT=ones_bf[:, :], rhs=SQ[:, c, 0:n],
                                       start=(c == 0), stop=(c == KD - 1))
                    return ins
                ch.op("pe", ssmm)
                ch.op("act", lambda e, n=n, RS=RS, psS=psS: e.activation(out=RS[:, 0:n], in_=psS[:, 0:n], func=AF.Sqrt,
                                                                         bias=cx.epsD[:, 0:1], scale=1.0))
                ch.op("dve", lambda e, n=n, RS=RS: e.reciprocal(out=RS[:, 0:n], in_=RS[:, 0:n]))

                def nrm(e, n=n, XB=XB, RS=RS):
                    ins = None
                    for c in range(KD):
                        ins = e.scalar_tensor_tensor(out=XB[:, c, 0:n], in0=XB[:, c, 0:n], scalar=A_sb[:, c:c + 1],
                                                     in1=RS[:, 0:n], op0=ALU.mult, op1=ALU.mult)
                    return ins
                ch.op("dve", nrm)

                def shf(e, n=n, t0=t0, XB=XB):
                    ins = None
                    for c in range(KD):
                        if mode == "pool":
                            dst = HB[:, c, HALO:HALO + n] if t0 >= 0 else HB[:, c, 0:HALO]
                        elif mode == "router":
                            dst = HF[:, c, 0:n]
                        else:
                            dst = IN[:, c, t0:t0 + n]
                        ins = e.activation(out=dst, in_=XB[:, c, 0:n], func=AF.Identity, bias=B_sb[:, c:c + 1])
                    return ins
                ch.op("act", shf)
                if mode == "pool":
                    _pool_block(ch, cx, pool, HB, S1, S2, t0)
                if mode == "router":
                    _router_block(ch, cx, router, HF, LG, M8, CB, DG, CO, combv, t0)
            ch.op("dve", lambda e, RS=RS: e.memset(RS[0:1, 0:1], 0.0))
        emit(nc, P)


def _pool_block(ch, cx, pool, HB, S1, S2, t0):
    cfg = cx.cfg
    KG = cfg.KD // 4
    IN = cx.IN
    W = 128 + HALO
    if t0 < 0:
        ch.op("dve", lambda e: e.tensor_scalar(out=HB[:, :, 0:HALO], in0=HB[:, :, 0:HALO],
                                               scalar1=pool["hmask"][:, 0:1], scalar2=None, op0=ALU.mult))
        return
    first = (t0 == 0)
    for g, k in enumerate((2, 4, 8, 16)):
        cs_ = slice(g * KG, (g + 1) * KG)
        cur, cur_cs, lo, step, it = HB, cs_, 0, 1, 0
        bufs = [S1, S2]
        while step < k:
            dst = bufs[it % 2]
            nlo = lo + step
            ch.op("dve", lambda e, dst=dst, cur=cur, cur_cs=cur_cs, nlo=nlo, step=step: e.tensor_tensor(
                out=dst[:, :, nlo:W], in0=cur[:, cur_cs, nlo:W], in1=cur[:, cur_cs, nlo - step:W - step], op=ALU.add))
            cur, cur_cs, lo = dst, slice(0, KG), nlo
            step *= 2
            it += 1
        ch.op("dve", lambda e, cur=cur, cs_=cs_, k=k: e.scalar_tensor_tensor(
            out=IN[:, cs_, t0:t0 + 128], in0=cur[:, :, HALO:W], scalar=1.0 / k, in1=HB[:, cs_, HALO:W],
            op0=ALU.mult, op1=ALU.subtract))
        if first:
            def fix(e, cur=cur, g=g):
                ins = None
                for c in range(KG):
                    ins = e.tensor_tensor(out=cur[:, c, HALO:2 * HALO], in0=cur[:, c, HALO:2 * HALO],
                                          in1=pool["invc"][:, g, :], op=ALU.mult)
                return ins
            ch.op("dve", fix)
            ch.op("dve", lambda e, cur=cur, cs_=cs_: e.tensor_tensor(
                out=IN[:, cs_, 0:HALO], in0=cur[:, :, HALO:2 * HALO], in1=HB[:, cs_, HALO:2 * HALO], op=ALU.subtract))
    ch.op("dve", lambda e: e.tensor_copy(out=HB[:, :, 0:HALO], in_=HB[:, :, 128:W]))


def _router_block(ch, cx, R, HF, LG, M8, CB, DG, CO, combv, t0):
    cfg = cx.cfg
    KD, NE = cfg.KD, cfg.NE
    IN = cx.IN
    psR = cx.ps[1]
    ch.op("pool", lambda e: e.tensor_copy(out=IN[:, 0:KD, t0:t0 + 128], in_=HF[:, :, :]))

    def rmm(e):
        ins = None
        for c in range(KD):
            ins = e.matmul(psR[:, 0:NE], lhsT=HF[:, c, :], rhs=R["rw"][:, c, :], start=(c == 0), stop=(c == KD - 1))
        return ins
    ch.op("pe", rmm)

    ch.op("dve", lambda e: e.tensor_copy(out=LG[:, 0:NE], in_=psR[:, 0:NE]))
    ch.op("dve", lambda e: e.max(out=M8[:, 0:8], in_=LG[:, 0:NE]))
    ch.op("dve", lambda e: e.tensor_tensor(out=M8[:, 8:9], in0=M8[:, 1:2], in1=M8[:, 0:1], op=ALU.subtract))
    ch.op("act", lambda e: e.activation(out=M8[:, 9:10], in_=M8[:, 8:9], func=AF.Sigmoid))

    ch.op("dve", lambda e: e.tensor_scalar(out=M8[:, 10:11], in0=M8[:, 9:10], scalar1=-1.0, scalar2=1.0,
                                           op0=ALU.mult, op1=ALU.add))

    def cm2(e):
        e.tensor_scalar(out=CB[:, 0:NE], in0=LG[:, 0:NE], scalar1=M8[:, 0:1], scalar2=M8[:, 10:11],
                        op0=ALU.is_equal, op1=ALU.mult)
        return e.tensor_scalar(out=CB[:, NE:2 * NE], in0=LG[:, 0:NE], scalar1=M8[:, 1:2], scalar2=M8[:, 9:10],
                               op0=ALU.is_equal, op1=ALU.mult)
    ch.op("dve", cm2)
    ch.op("dve", lambda e: e.tensor_tensor(out=CB[:, 0:NE], in0=CB[:, 0:NE], in1=CB[:, NE:2 * NE], op=ALU.add))

    def cmb(e):
        ins = None
        for ex in range(NE):
            ins = e.tensor_scalar(out=DG[:, ex, :], in0=R["ident"][:, :], scalar1=CB[:, ex:ex + 1], scalar2=None,
                                  op0=ALU.mult)
        return ins
    ch.op("dve", cmb)

    def bmm(e):
        ins = None
        for ex in range(NE):
            bank = cx.ps[2 + ex // 4]
            ins = e.matmul(bank[:, (ex % 4) * 128:(ex % 4 + 1) * 128], lhsT=R["ones32"][:, :], rhs=DG[:, ex, :],
                           start=True, stop=True)
        return ins
    ch.op("pe", bmm)
    for hb in range(NE // 4):
        ch.op("act", lambda e, hb=hb: e.copy(out=CO[:, hb * 4:(hb + 1) * 4, :],
                                             in_=cx.ps[2 + hb][:, :].rearrange("p (a t) -> p a t", t=128)))
    ch.dma("sp", combv[:, :, t0:t0 + 128], CO[:, :, :])


def phase_gemm(cx, name, KC, kc0, tiles, epi, in_src=None, mode="fm", WCOLS=256):
    nc, cfg = cx.nc, cx.cfg
    T = cfg.T
    IN = cx.IN
    NB = T // 512
    with ExitStack() as es:
        NBUF = 3
        WT = [cx.sb(es, "wt", [128, KC, WCOLS], BF16) for _ in range(NBUF)]
        wld = [cx.sem(es, "wld") for _ in range(NBUF)]
        s_mm = cx.sem(es, "gmm")
        s_free = cx.sem(es, "gfree")
        s_in = cx.sem(es, "gin")
        P = Prog()
        epi.setup(cx, es, P, s_mm, s_free)
        in_wait = []
        if in_src is not None:
            for kc in range(KC):
                P.dma("sp", IN[:, kc0 + kc, :], in_src[kc, :, :], inc=s_in)
            in_wait = [(s_in, 16 * KC)]
        G = 0
        tile_lastG = []
        for ti, tl in enumerate(tiles):
            b = ti % NBUF
            wv = tl["ap"].rearrange("(kc p) n -> p kc n", p=128)
            wfree = [(s_mm, tile_lastG[ti - NBUF])] if ti >= NBUF else []
            P.dma("pool", WT[b][:, :, :], wv, waits=wfree, inc=wld[b])
            wready = [(wld[b], 16 * (ti // NBUF + 1))]
            if mode == "fm":
                for ji, meta in enumerate(tl["jobs"]):
                    for tb in range(NB):
                        bank = cx.ps[G % 8]
                        for kc in range(KC):
                            P.op("pe", lambda e, bank=bank, b=b, kc=kc, ji=ji, tb=tb: e.matmul(
                                bank[:, :], lhsT=WT[b][:, kc, ji * 128:(ji + 1) * 128],
                                rhs=IN[:, kc0 + kc, tb * 512:(tb + 1) * 512],
                                start=(kc == 0), stop=(kc == KC - 1)),
                                waits=wready + in_wait + [(s_free, epi.need_free(G))],
                                inc=s_mm if kc == KC - 1 else None)
                        epi.group(G, meta, tb, bank)
                        G += 1
            else:
                for tt in range(T // 128):
                    bank = cx.ps[G % 8]
                    for kc in range(KC):
                        P.op("pe", lambda e, bank=bank, b=b, kc=kc, tt=tt: e.matmul(
                            bank[:, 0:WCOLS], lhsT=IN[:, kc0 + kc, tt * 128:(tt + 1) * 128],
                            rhs=WT[b][:, kc, :], start=(kc == 0), stop=(kc == KC - 1)),
                            waits=wready + in_wait + [(s_free, epi.need_free(G))],
                            inc=s_mm if kc == KC - 1 else None)
                    epi.group(G, tl["jobs"][0], tt, bank)
                    G += 1
            tile_lastG.append(G)
        epi.finish()
        emit(nc, P)


class EpiResid:
    NS = 4

    def __init__(self, xT_ap, col0, gate_sb):
        self.xT, self.col0, self.gate = xT_ap, col0, gate_sb

    def setup(self, cx, es, P, s_mm, s_free):
        self.cx, self.P, self.s_mm, self.s_free = cx, P, s_mm, s_free
        self.XR = [cx.sb(es, "xr", [128, 512], F32) for _ in range(self.NS)]
        self.ld = [cx.sem(es, "xrld") for _ in range(self.NS)]
        self.st = [cx.sem(es, "xrst") for _ in range(self.NS)]

    def need_free(self, G):
        return G - 7

    def group(self, G, meta, tb, bank):
        P, s = self.P, G % self.NS
        r = G // self.NS
        chunk = meta
        c0 = self.col0 + tb * 512
        XR = self.XR[s]
        P.dma("sp", XR[:, :], self.xT[chunk, :, c0:c0 + 512], waits=[(self.st[s], 16 * r)], inc=self.ld[s])
        P.op("dve", lambda e: e.scalar_tensor_tensor(out=XR[:, :], in0=bank[:, :], scalar=self.gate[:, chunk:chunk + 1],
                                                     in1=XR[:, :], op0=ALU.mult, op1=ALU.add),
             waits=[(self.s_mm, G + 1), (self.ld[s], 16 * (r + 1))], inc=self.s_free)
        P.dma("act", self.xT[chunk, :, c0:c0 + 512], XR[:, :], waits=[(self.s_free, G + 1)], inc=self.st[s])

    def finish(self):
        self.P.op("dve", lambda e: e.memset(self.cx.scr[0:1, 0:1], 0.0), waits=[(s, s.n) for s in self.st])


class EpiSwiglu:
    NS = 2

    def __init__(self, aT_ap, comb_ap=None):
        self.aT, self.comb = aT_ap, comb_ap

    def setup(self, cx, es, P, s_mm, s_free):
        self.cx, self.P, self.s_mm, self.s_free = cx, P, s_mm, s_free
        self.SG = [cx.sb(es, "sg", [128, 512], F32) for _ in range(self.NS)]
        self.AO = [cx.sb(es, "ao", [128, 512], BF16) for _ in range(self.NS)]
        self.st = [cx.sem(es, "aost") for _ in range(self.NS)]
        self.s_sg = cx.sem(es, "sgs")
        self.s_d1 = cx.sem(es, "sd1")
        self.E = 0
        self.bank1 = {}
        self.cur_e = -1
        self.last_E_of = {}
        if self.comb is not None:
            self.CBT = [cx.sb(es, "cbt", [128, cx.cfg.T], F32) for _ in range(2)]
            self.cbld = [cx.sem(es, "cbld") for _ in range(2)]

    def need_free(self, G):
        Gp = G - 8
        if Gp < 0:
            return 0
        m, r = divmod(Gp, 8)
        return 4 * m + (r % 4) + 1

    def group(self, G, meta, tb, bank):
        which, fchunk, ex = meta
        if which == 0:
            self.bank1[tb] = bank
            return
        P = self.P
        E = self.E
        self.E += 1
        s = E % self.NS
        r = E // self.NS
        b1, b3 = self.bank1[tb], bank
        SG, AO = self.SG[s], self.AO[s]
        P.op("act", lambda e: e.activation(out=SG[:, :], in_=b1[:, :], func=AF.Silu),
             waits=[(self.s_mm, G + 1), (self.s_free, E - self.NS + 1)], inc=self.s_sg)
        if self.comb is None:
            P.op("dve", lambda e: e.tensor_tensor(out=AO[:, :], in0=SG[:, :], in1=b3[:, :], op=ALU.mult),
                 waits=[(self.s_sg, E + 1), (self.st[s], 16 * r)], inc=self.s_free)
        else:
            cb = ex % 2
            if ex != self.cur_e:
                P.dma("sp", self.CBT[cb][:, :], self.comb[ex, :, :],
                      waits=[(self.s_free, self.last_E_of.get(ex - 2, 0))], inc=self.cbld[cb])
                self.cur_e = ex
            CBT = self.CBT[cb]
            P.op("dve", lambda e: e.tensor_tensor(out=SG[:, :], in0=SG[:, :], in1=b3[:, :], op=ALU.mult),
                 waits=[(self.s_sg, E + 1), (self.st[s], 16 * r)], inc=self.s_d1)
            P.op("dve", lambda e: e.tensor_tensor(out=AO[:, :], in0=SG[:, :], in1=CBT[:, tb * 512:(tb + 1) * 512],
                                                  op=ALU.mult),
                 waits=[(self.cbld[cb], 16 * (ex // 2 + 1)), (self.s_d1, E + 1)], inc=self.s_free)
            self.last_E_of[ex] = E + 1
        P.dma("sp", self.aT[fchunk, :, tb * 512:(tb + 1) * 512], AO[:, :], waits=[(self.s_free, E + 1)],
              inc=self.st[s])

    def finish(self):
        self.P.op("dve", lambda e: e.memset(self.cx.scr[0:1, 0:1], 0.0), waits=[(s, s.n) for s in self.st])


class EpiRaw:
    NS = 4

    def __init__(self, dst_fn, dt, width=512):
        self.dst_fn, self.dt, self.width = dst_fn, dt, width

    def setup(self, cx, es, P, s_mm, s_free):
        self.cx, self.P, self.s_mm, self.s_free = cx, P, s_mm, s_free
        self.RO = [cx.sb(es, "ro", [128, self.width], self.dt) for _ in range(self.NS)]
        self.st = [cx.sem(es, "rost") for _ in range(self.NS)]

    def need_free(self, G):
        return G - 7

    def group(self, G, meta, tb, bank):
        P, s = self.P, G % self.NS
        r = G // self.NS
        RO = self.RO[s]
        w = self.width
        if G % 2 == 0:
            P.op("act", lambda e: e.copy(out=RO[:, :], in_=bank[:, 0:w]),
                 waits=[(self.s_mm, G + 1), (self.st[s], 16 * r), (self.s_free, G)], inc=self.s_free)
        else:
            P.op("dve", lambda e: e.tensor_copy(out=RO[:, :], in_=bank[:, 0:w]),
                 waits=[(self.s_mm, G + 1), (self.st[s], 16 * r), (self.s_free, G)], inc=self.s_free)
        P.dma("sp", self.dst_fn(meta, tb), RO[:, :], waits=[(self.s_free, G + 1)], inc=self.st[s])

    def finish(self):
        self.P.op("dve", lambda e: e.memset(self.cx.scr[0:1, 0:1], 0.0), waits=[(s, s.n) for s in self.st])


def phase_headnorm(cx, raw_ap, out_ap, nchunk, gvec_sb, ones_bf):
    nc, cfg = cx.nc, cx.cfg
    T = cfg.T
    NB = T // 512
    NCH = 2
    with ExitStack() as es:
        P = Prog()
        for cid in range(NCH):
            RW = cx.sb(es, "hraw", [128, T], F32)
            SQ = cx.sb(es, "hsq", [128, T], BF16)
            RS = cx.sb(es, "hrs", [128, T], F32)
            QN = cx.sb(es, "hqn", [128, T], BF16)
            cs, ds = cx.sem(es, "hc"), cx.sem(es, "hd")
            ch = Chain(P, cs, ds, cid)
            banks = cx.ps[4 * cid:4 * cid + 4]
            for ci in range(cid, nchunk, NCH):
                ch.dma("sp", RW[:, :], raw_ap[ci, :, :])
                ch.op("act", lambda e, SQ=SQ, RW=RW: e.activation(out=SQ[:, :], in_=RW[:, :], func=AF.Square))

                def mm(e, SQ=SQ, banks=banks):
                    ins = None
                    for tb in range(NB):
                        ins = e.matmul(banks[tb][:, :], lhsT=ones_bf[:, :], rhs=SQ[:, tb * 512:(tb + 1) * 512],
                                       start=True, stop=True)
                    return ins
                ch.op("pe", mm)

                def sq(e, RS=RS, banks=banks):
                    ins = None
                    for tb in range(NB):
                        ins = e.activation(out=RS[:, tb * 512:(tb + 1) * 512], in_=banks[tb][:, :], func=AF.Sqrt,
                                           bias=cx.epsE[:, 0:1], scale=1.0)
                    return ins
                ch.op("act", sq)
                ch.op("dve", lambda e, RS=RS: e.reciprocal(out=RS[:, :], in_=RS[:, :]))
                ch.op("dve", lambda e, QN=QN, RW=RW, RS=RS: e.scalar_tensor_tensor(
                    out=QN[:, :], in0=RW[:, :], scalar=gvec_sb[:, 0:1], in1=RS[:, :], op0=ALU.mult, op1=ALU.mult))
                ch.dma("sp", out_ap[ci, :, :], QN[:, :])
            ch.op("dve", lambda e, RS=RS: e.memset(RS[0:1, 0:1], 0.0))
        emit(nc, P)


def _din(nc, name, shape, dt=F32):
    return nc.dram_tensor(name, list(shape), dt, kind="ExternalInput").ap()


def _dout(nc, name, shape, dt=F32):
    return nc.dram_tensor(name, list(shape), dt, kind="ExternalOutput").ap()


def _dint(nc, name, shape, dt=F32):
    return nc.dram_tensor(name, list(shape), dt, kind="Internal").ap()


def _derive_AB(ch, A, MOD, g_sb, KD, D):
    ch.op("dve", lambda e: e.scalar_tensor_tensor(out=A[:, 0:KD], in0=MOD[:, KD:2 * KD], scalar=1.0, in1=g_sb[:, 0:KD],
                                                  op0=ALU.add, op1=ALU.mult))
    ch.op("dve", lambda e: e.tensor_scalar(out=A[:, 0:KD], in0=A[:, 0:KD], scalar1=float(np.sqrt(D)), scalar2=None,
                                           op0=ALU.mult))


def build_l0(cfg, stage=99):
    nc = bass.Bass("TRN2", target_bir_lowering=False)
    D, KD, T, H, G, DFF = cfg.D, cfg.KD, cfg.T, cfg.H, cfg.G, cfg.DFF
    KG = KD // 4
    xin = _din(nc, "xin", [HALO + T, D])
    condT_d = _din(nc, "condT", [128, KD])
    adaw00 = _din(nc, "adaw00", [D, 3 * D])
    adab00 = _din(nc, "adab00T", [128, 3 * KD])
    adaw01 = _din(nc, "adaw01", [D, 3 * D])
    adab01 = _din(nc, "adab01T", [128, 3 * KD])
    kvadaw = _din(nc, "kvadaw", [D, 2 * D])
    kvadab = _din(nc, "kvadabT", [128, 2 * KD])
    gvecs_d = _din(nc, "gvecs", [128, 4, KD])
    kg_d = _din(nc, "kgT", [128, 1])
    poolw = _din(nc, "poolw", [4, G, G])
    w1 = _din(nc, "w1", [D, DFF])
    w3 = _din(nc, "w3", [D, DFF])
    w2 = _din(nc, "w2", [DFF, D])
    wk = _din(nc, "wk", [D, D])
    wv = _din(nc, "wv", [D, D])
    ident_d = _din(nc, "ident", [128, 128])
    onesbf_d = _din(nc, "onesbf", [128, 128], BF16)
    hmask_d = _din(nc, "hmask", [128, 2])
    invc_d = _din(nc, "invc", [128, 4, HALO])
    xT = _dout(nc, "xT", [KD, 128, HALO + T])
    knT = _dout(nc, "knT", [H, 128, T], BF16)
    v_o = _dout(nc, "v", [T, D], BF16)
    aT = _dint(nc, "aT", [DFF // 128, 128, T], BF16)
    kraw = _dint(nc, "kraw", [H, 128, T])

    with ExitStack() as es:
        cx = Ctx(nc, cfg, es)
        sb = lambda name, shape, dt=F32: es.enter_context(nc.sbuf_tensor("s_" + name, shape, dt))
        ident = sb("ident", [128, 128])
        onesbf = sb("onesbf", [128, 128], BF16)
        condS = sb("condS", [128, KD])
        b00, b01, bkv = sb("b00", [128, 3 * KD]), sb("b01", [128, 3 * KD]), sb("bkv", [128, 2 * KD])
        M00, M01, MKV = sb("M00", [128, 3 * KD]), sb("M01", [128, 3 * KD]), sb("MKV", [128, 2 * KD])
        gv = sb("gv", [128, 4, KD])
        kg = sb("kg", [128, 1])
        hmask = sb("hmask", [128, 2])
        invc = sb("invc", [128, 4, HALO])
        A00, A01, AKV, GT0 = sb("A00", [128, KD]), sb("A01", [128, KD]), sb("AKV", [128, KD]), sb("GT0", [128, KD])

        def post(ch):
            ch.op("dve", lambda e: e.memset(cx.epsD[:, :], float(D * EPS)))
            ch.op("dve", lambda e: e.memset(cx.epsE[:, :], float(128 * EPS)))
            ch.op("act", lambda e: e.activation(out=condS[:, :], in_=condS[:, :], func=AF.Silu))
            ch.op("dve", lambda e: e.tensor_scalar(out=kg[:, :], in0=kg[:, :], scalar1=float(np.sqrt(128.0)),
                                                   scalar2=None, op0=ALU.mult))
        phase_loads(cx, [(ident[:, :], ident_d), (onesbf[:, :], onesbf_d), (condS[:, :], condT_d),
                         (b00[:, :], adab00), (b01[:, :], adab01), (bkv[:, :], kvadab), (gv[:, :, :], gvecs_d),
                         (kg[:, :], kg_d), (hmask[:, :], hmask_d), (invc[:, :, :], invc_d)], post)
        phase_ada(cx, adaw00, b00, condS, M00, 3 * KD)
        phase_ada(cx, adaw01, b01, condS, M01, 3 * KD)
        phase_ada(cx, kvadaw, bkv, condS, MKV, 2 * KD)

        def derive(ch):
            _derive_AB(ch, A00, M00, gv[:, 0, :], KD, D)
            _derive_AB(ch, A01, M01, gv[:, 1, :], KD, D)
            _derive_AB(ch, AKV, MKV, gv[:, 2, :], KD, D)
            ch.op("dve", lambda e: e.tensor_tensor(out=GT0[:, :], in0=M00[:, 2 * KD:3 * KD], in1=gv[:, 3, :],
                                                   op=ALU.mult))
        phase_loads(cx, [], derive)
        phase_transpose_in(cx, xin, xT, ident, HALO + T)
        seg = ExitStack()
        cx.alloc_in(seg)
        if stage >= 1:
            phase_norm(cx, xT, HALO, A00, M00[:, 0:KD], onesbf, mode="pool", pool={"hmask": hmask, "invc": invc})
        if stage >= 2:
            for g in range(4):
                tiles = []
                for cb in range(G // 256):
                    tiles.append({"ap": poolw[g, :, cb * 256:(cb + 1) * 256],
                                  "jobs": [g * KG + cb * 2, g * KG + cb * 2 + 1]})
                phase_gemm(cx, f"pool{g}", KG, g * KG, tiles, EpiResid(xT, HALO, GT0))
        if stage >= 3:
            phase_norm(cx, xT, HALO, A01, M01[:, 0:KD], onesbf)
            tiles = []
            for j in range(DFF // 128):
                tiles.append({"ap": w1[:, j * 128:(j + 1) * 128], "jobs": [(0, j, 0)]})
                tiles.append({"ap": w3[:, j * 128:(j + 1) * 128], "jobs": [(1, j, 0)]})
            phase_gemm(cx, "ffn1", KD, 0, tiles, EpiSwiglu(aT), WCOLS=128)
        if stage >= 4:
            nfc = DFF // 128
            for c0 in range(0, nfc, 32):
                kc = min(32, nfc - c0)
                tiles = [{"ap": w2[c0 * 128:(c0 + kc) * 128, cb * 256:(cb + 1) * 256], "jobs": [cb * 2, cb * 2 + 1]}
                         for cb in range(D // 256)]
                phase_gemm(cx, "ffn2", kc, 0, tiles, EpiResid(xT, HALO, M01[:, 2 * KD:3 * KD]),
                           in_src=aT[c0:c0 + kc, :, :])
        if stage >= 5:
            phase_norm(cx, xT, HALO, AKV, MKV[:, 0:KD], onesbf)
            tiles = [{"ap": wk[:, cb * 256:(cb + 1) * 256], "jobs": [cb * 2, cb * 2 + 1]} for cb in range(D // 256)]
            phase_gemm(cx, "kproj", KD, 0, tiles,
                       EpiRaw(lambda ch_, tb: kraw[ch_, :, tb * 512:(tb + 1) * 512], F32, 512))
            phase_headnorm(cx, kraw, knT, H, kg, onesbf)
            tiles = [{"ap": wv[:, cb * 256:(cb + 1) * 256], "jobs": [cb]} for cb in range(D // 256)]
            phase_gemm(cx, "vproj", KD, 0, tiles,
                       EpiRaw(lambda cb, tt: v_o[tt * 128:(tt + 1) * 128, cb * 256:(cb + 1) * 256], BF16, 256),
                       mode="tm")
        seg.close()
    return nc


def _colT(vec, KD):
    return np.ascontiguousarray(np.asarray(vec, np.float32).reshape(KD, 128).T)


def prep_l0(inp, cfg, core):
    b, half = divmod(core, 2)
    D, KD, T = cfg.D, cfg.KD, cfg.T
    t0 = half * T
    x = inp["x"]
    xin = np.zeros((HALO + T, D), np.float32)
    xin[HALO:] = x[b, t0:t0 + T]
    if half == 1:
        xin[:HALO] = x[b, t0 - HALO:t0]
    hmask = np.full((128, 2), float(half), np.float32)
    invc = np.zeros((128, 4, HALO), np.float32)
    for g, k in enumerate((2, 4, 8, 16)):
        for t in range(HALO):
            invc[:, g, t] = (1.0 / min(t + 1, k)) if half == 0 else 1.0 / k
    gvecs = np.stack([_colT(inp["norm_g"][0, 0], KD), _colT(inp["norm_g"][0, 1], KD), _colT(inp["kv_norm_g"], KD),
                      _colT(inp["pool_scale"][0], KD)], axis=1)
    return {
        "xin": xin, "condT": _colT(inp["c"][b], KD),
        "adaw00": inp["ada_w"][0, 0], "adab00T": _colT(inp["ada_b"][0, 0], 3 * KD),
        "adaw01": inp["ada_w"][0, 1], "adab01T": _colT(inp["ada_b"][0, 1], 3 * KD),
        "kvadaw": inp["kv_ada_w"], "kvadabT": _colT(inp["kv_ada_b"], 2 * KD),
        "gvecs": np.ascontiguousarray(gvecs), "kgT": np.asarray(inp["k_norm_g"], np.float32).reshape(128, 1),
        "poolw": inp["pool_w"][0], "w1": inp["ffn_w1"][0], "w3": inp["ffn_w3"][0], "w2": inp["ffn_w2"][0],
        "wk": inp["w_k"], "wv": inp["w_v"],
        "ident": np.eye(128, dtype=np.float32), "onesbf": np.ones((128, 128), ml_dtypes.bfloat16),
        "hmask": hmask, "invc": invc,
    }


def phase_bias_setup(cx, rb_d, oh_d, gq_row_d, gk_row_d, rbrow_d, extrep, NEGC, ones32):
    nc, cfg = cx.nc, cx.cfg
    H = cfg.H
    with ExitStack() as es:
        RB = cx.sb(es, "rb", [32, 3, H], F32)
        OH = cx.sb(es, "oh", [32, 3, 129], F32)
        EXT = cx.sb(es, "ext", [H, 3, 384], F32)
        ROW = cx.sb(es, "row", [1, 3 * 32 * H + 256], F32)
        SC = cx.sb(es, "sc", [1, 16], F32)
        cs, ds = cx.sem(es, "bc"), cx.sem(es, "bd")
        P = Prog()
        NR = 3 * 32 * H
        P.dma("sp", RB[:, :, :], rb_d, inc=ds)
        P.dma("sp", OH[:, :, :], oh_d, inc=ds)
        P.dma("sp", ROW[0:1, 0:NR], rbrow_d, inc=ds)
        P.dma("sp", ROW[0:1, NR:NR + 128], gq_row_d, inc=ds)
        P.dma("sp", ROW[0:1, NR + 128:NR + 256], gk_row_d, inc=ds)
        ch = Chain(P, cs, ds)
        ch.last = (ds, ds.n)
        ch.op("dve", lambda e: e.memset(EXT[:, :, :], NEG))

        def mm(e):
            ins = None
            for g in range(3):
                ins = e.matmul(cx.ps[g][0:H, 0:129], lhsT=RB[:, g, :], rhs=OH[:, g, :], start=True, stop=True)
            return ins
        ch.op("pe", mm)

        def cp(e):
            ins = None
            for g in range(3):
                ins = e.tensor_copy(out=EXT[:, g, 127:256], in_=cx.ps[g][0:H, 0:129])
            return ins
        ch.op("dve", cp)
        for g in range(3):
            ch.dma("sp", extrep[g], EXT[:, g, :].unsqueeze(1).broadcast_to([H, 128, 384]))
        ch.op("dve", lambda e: e.tensor_tensor(out=ROW[0:1, :], in0=ROW[0:1, :], in1=ROW[0:1, :], op=ALU.mult))

        def red(e):
            e.tensor_reduce(out=SC[0:1, 0:1], in_=ROW[0:1, NR:NR + 128], axis=mybir.AxisListType.X, op=ALU.max)
            e.tensor_reduce(out=SC[0:1, 1:2], in_=ROW[0:1, NR + 128:NR + 256], axis=mybir.AxisListType.X, op=ALU.max)
            return e.tensor_reduce(out=SC[0:1, 2:3], in_=ROW[0:1, 0:NR], axis=mybir.AxisListType.X, op=ALU.max)
        ch.op("dve", red)
        ch.op("dve", lambda e: e.tensor_tensor(out=SC[0:1, 3:4], in0=SC[0:1, 0:1], in1=SC[0:1, 1:2], op=ALU.mult))
        ch.op("act", lambda e: e.activation(out=SC[0:1, 4:6], in_=SC[0:1, 2:4], func=AF.Sqrt))
        ch.op("dve", lambda e: e.tensor_scalar(out=SC[0:1, 6:7], in0=SC[0:1, 5:6], scalar1=float(-np.sqrt(128.0) * 1.001),
                                               scalar2=None, op0=ALU.mult))
        ch.op("dve", lambda e: e.tensor_tensor(out=SC[0:1, 7:8], in0=SC[0:1, 6:7], in1=SC[0:1, 4:5], op=ALU.subtract))
        ch.op("pe", lambda e: e.matmul(cx.ps[4][:, 0:1], lhsT=ones32[0:1, :], rhs=SC[0:1, 7:8], start=True, stop=True))
        ch.op("dve", lambda e: e.tensor_copy(out=NEGC[:, 0:1], in_=cx.ps[4][:, 0:1]))
        emit(nc, P)


def phase_attention(cx, qnT, kn_prev, kn_own, v_prev, v_own, extrep, oT, NEGC, cmask, onesbf):
    nc, cfg = cx.nc, cx.cfg
    T, H, D = cfg.T, cfg.H, cfg.D
    NBW = 2 * T // 128
    NCH = 2
    ext_t = extrep.tensor
    with ExitStack() as es:
        P = Prog()
        for cid in range(NCH):
            QNb = cx.sb(es, "aq", [128, T], BF16)
            KW = cx.sb(es, "akw", [128, 2 * T], BF16)
            VTb = cx.sb(es, "avt", [128, NBW * 128], BF16)
            BA = [cx.sb(es, "aba", [128, 4, 256], F32) for _ in range(3)]
            BB = [cx.sb(es, "abb", [128, 4, 256], F32) for _ in range(3)]
            BM = cx.sb(es, "abm", [128, 4, 256], F32)
            TT = cx.sb(es, "att", [128, 4, 256], F32)
            PT = cx.sb(es, "apt", [128, 4, 256], BF16)
            ACC = cx.sb(es, "aacc", [128, 2, T], F32)
            OT = cx.sb(es, "aot", [128, T], BF16)
            cs, ds = cx.sem(es, "ac"), cx.sem(es, "ad")
            ch = Chain(P, cs, ds, cid)
            pb = cx.ps[4 * cid:4 * cid + 4]
            for h in range(cid, H, NCH):
                for g in range(3):
                    off = ((g * H + h) * 128) * 384 + 127
                    src_ = bass.AP(tensor=ext_t, offset=off, ap=[[383, 128], [0, 4], [1, 256]])
                    ch.dma("sp", BA[g][:, :, :], src_)
                ch.dma("sp", KW[:, 0:T], kn_prev[h, :, :])
                ch.dma("sp", KW[:, T:2 * T], kn_own[h, :, :])

                def mkb(e, BA=BA, BB=BB, BM=BM):
                    ins = None
                    for g in range(3):
                        e.tensor_copy(out=BB[g][:, :, 0:128], in_=BA[g][:, :, 0:128])
                        ins = e.tensor_scalar(out=BB[g][:, :, 128:256], in0=BA[g][:, :, 128:256],
                                              scalar1=cmask[:, 0:1], scalar2=None, op0=ALU.add)
                    e.tensor_copy(out=BM[:, 1:4, :], in_=BA[0][:, 1:4, :])
                    return ins
                ch.op("dve", mkb)
                ch.op("dve", lambda e, BM=BM, BB=BB: e.tensor_copy(out=BM[:, 0:1, :], in_=BB[0][:, 0:1, :]))
                for g, (_, d) in enumerate(PATTERNS):
                    nbh = T // (128 * d)
                    VT = VTb[:, :].rearrange("p (s r nb e) -> p s r nb e", s=2, r=d, nb=nbh)
                    ch.dma("sp", QNb[:, :], qnT[g * H + h, :, :])
                    ch.dma("sp", VT[:, 0, :, :, :],
                           v_prev[:, h * 128:(h + 1) * 128].rearrange("(nb j r) e -> j r nb e", j=128, r=d))
                    ch.dma("sp", VT[:, 1, :, :, :],
                           v_own[:, h * 128:(h + 1) * 128].rearrange("(nb j r) e -> j r nb e", j=128, r=d))
                    units = [(r, nl) for nl in range(nbh) for r in range(d)]
                    qv = QNb[:, :].rearrange("p (m r) -> p r m", r=d)
                    kv = KW[:, :].rearrange("p (m r) -> p r m", r=d)
                    av = ACC[:, :, :].rearrange("p a (m r) -> p a r m", r=d)
                    for b0 in range(0, len(units), 4):
                        ub = units[b0:b0 + 4]
                        if all(nl == 0 for (_, nl) in ub):
                            bias = BB[g]
                        elif any(nl == 0 for (_, nl) in ub):
                            assert g == 0 and b0 == 0
                            bias = BM
                        else:
                            bias = BA[g]

                        def s1(e, ub=ub, qv=qv, kv=kv, d=d, pb=pb):
                            ins = None
                            for u, (r, nl) in enumerate(ub):
                                m0 = T // d + 128 * nl
                                for blk in range(2):
                                    c0 = (u % 2) * 256 + blk * 128
                                    ins = e.matmul(pb[u // 2][:, c0:c0 + 128],
                                                   lhsT=kv[:, r, m0 - blk * 128:m0 - blk * 128 + 128],
                                                   rhs=qv[:, r, 128 * nl:128 * nl + 128], start=True, stop=True)
                            return ins
                        ch.op("pe", s1)

                        def s2(e, bias=bias, pb=pb, TT=TT):
                            ins = None
                            for bk in range(2):
                                ins = e.tensor_tensor(out=TT[:, 2 * bk:2 * bk + 2, :],
                                                      in0=pb[bk][:, :].rearrange("p (u c) -> p u c", u=2),
                                                      in1=bias[:, 2 * bk:2 * bk + 2, :], op=ALU.add)
                            return ins
                        ch.op("dve", s2)
                        ch.op("act", lambda e, PT=PT, TT=TT: e.activation(out=PT[:, :, :], in_=TT[:, :, :], func=AF.Exp,
                                                                          bias=NEGC[:, 0:1], scale=1.0))

                        def s4(e, ub=ub, VT=VT, nbh=nbh, pb=pb, PT=PT):
                            ins = None
                            for u, (r, nl) in enumerate(ub):
                                nbc = nbh + nl
                                ob = pb[2 + u // 2]
                                o0 = (u % 2) * 256
                                for blk in range(2):
                                    nbw = nbc - blk
                                    e.matmul(ob[:, o0:o0 + 128], lhsT=VT[:, nbw // nbh, r, nbw % nbh, :],
                                             rhs=PT[:, u, blk * 128:(blk + 1) * 128], start=(blk == 0), stop=(blk == 1))
                                for blk in range(2):
                                    ins = e.matmul(ob[:, o0 + 128:o0 + 256], lhsT=onesbf[:, :],
                                                   rhs=PT[:, u, blk * 128:(blk + 1) * 128],
                                                   start=(blk == 0), stop=(blk == 1))
                            return ins
                        ch.op("pe", s4)

                        def s5(e, ub=ub, g=g, av=av, pb=pb):
                            ins = None
                            for u, (r, nl) in enumerate(ub):
                                sr = pb[2 + u // 2][:, (u % 2) * 256:(u % 2) * 256 + 256].rearrange(
                                    "p (a q) -> p a q", a=2)
                                dst = av[:, :, r, 128 * nl:128 * nl + 128]
                                if g == 0:
                                    ins = e.tensor_copy(out=dst, in_=sr)
                                else:
                                    ins = e.tensor_tensor(out=dst, in0=dst, in1=sr, op=ALU.add)
                            return ins
                        ch.op("dve", s5)
                ch.op("dve", lambda e, ACC=ACC: e.reciprocal(out=ACC[:, 1, :], in_=ACC[:, 1, :]))
                ch.op("dve", lambda e, OT=OT, ACC=ACC: e.tensor_tensor(out=OT[:, :], in0=ACC[:, 0, :], in1=ACC[:, 1, :],
                                                                       op=ALU.mult))
                ch.dma("sp", oT[h, :, :], OT[:, :])
            ch.op("dve", lambda e, TT=TT: e.memset(TT[0:1, 0, 0:1], 0.0))
        emit(nc, P)


def build_l1(cfg, stage=99):
    nc = bass.Bass("TRN2", target_bir_lowering=False)
    D, KD, T, H, NE, DFE = cfg.D, cfg.KD, cfg.T, cfg.H, cfg.NE, cfg.DFE
    xT_in = _din(nc, "xT_in", [KD, 128, T])
    knTw = _din(nc, "knTw", [H, 128, 2 * T], BF16)
    vw = _din(nc, "vw", [2 * T, D], BF16)
    condT_d = _din(nc, "condT", [128, KD])
    adaw10 = _din(nc, "adaw10", [D, 3 * D])
    adab10 = _din(nc, "adab10T", [128, 3 * KD])
    adaw11 = _din(nc, "adaw11", [D, 3 * D])
    adab11 = _din(nc, "adab11T", [128, 3 * KD])
    gvecs_d = _din(nc, "gvecs", [128, 2, KD])
    qg_d = _din(nc, "qgT", [128, 1])
    wq = _din(nc, "wq", [D, 3 * D])
    wo = _din(nc, "wo", [D, D])
    rb_d = _din(nc, "relb", [32, 3, H])
    rbrow_d = _din(nc, "relbrow", [1, 3 * 32 * H])
    gqrow_d = _din(nc, "gqrow", [1, 128])
    gkrow_d = _din(nc, "gkrow", [1, 128])
    oh_d = _din(nc, "oh", [32, 3, 129])
    cmask_d = _din(nc, "cmask", [128, 1])
    rw_d = _din(nc, "routerw", [D, NE])
    mw1 = _din(nc, "mw1", [NE, D, DFE])
    mw3 = _din(nc, "mw3", [NE, D, DFE])
    mw2 = _din(nc, "mw2", [NE * DFE, D])
    ident_d = _din(nc, "ident", [128, 128])
    ones32_d = _din(nc, "ones32", [128, 128])
    onesbf_d = _din(nc, "onesbf", [128, 128], BF16)
    out = _dout(nc, "out", [T, D])
    xT = _dint(nc, "xT2", [KD, 128, T])
    qraw = _dint(nc, "qraw", [3 * H, 128, T])
    qnT = _dint(nc, "qnT", [3 * H, 128, T], BF16)
    oT = _dint(nc, "oT", [H, 128, T], BF16)
    extrep = _dint(nc, "extrep", [3, H, 128, 384])
    comb = _dint(nc, "comb", [NE, 128, T])
    NFC = NE * DFE // 128
    aT = _dint(nc, "aT2", [NFC, 128, T], BF16)

    with ExitStack() as es:
        cx = Ctx(nc, cfg, es)
        sb = lambda name, shape, dt=F32: es.enter_context(nc.sbuf_tensor("s_" + name, shape, dt))
        ident = sb("ident", [128, 128])
        ones32 = sb("ones32", [128, 128])
        onesbf = sb("onesbf", [128, 128], BF16)
        condS = sb("condS", [128, KD])
        b10, b11 = sb("b10", [128, 3 * KD]), sb("b11", [128, 3 * KD])
        M10, M11 = sb("M10", [128, 3 * KD]), sb("M11", [128, 3 * KD])
        gv = sb("gv", [128, 2, KD])
        qg = sb("qg", [128, 1])
        cmask = sb("cmask", [128, 1])
        NEGC = sb("NEGC", [128, 1])
        rw = sb("rw", [128, KD, NE])
        A10, A11 = sb("A10", [128, KD]), sb("A11", [128, KD])

        def post(ch):
            ch.op("dve", lambda e: e.memset(cx.epsD[:, :], float(D * EPS)))
            ch.op("dve", lambda e: e.memset(cx.epsE[:, :], float(128 * EPS)))
            ch.op("act", lambda e: e.activation(out=condS[:, :], in_=condS[:, :], func=AF.Silu))
        loads = [(ident[:, :], ident_d), (ones32[:, :], ones32_d), (onesbf[:, :], onesbf_d), (condS[:, :], condT_d),
                 (b10[:, :], adab10), (b11[:, :], adab11), (gv[:, :, :], gvecs_d), (qg[:, :], qg_d),
                 (cmask[:, :], cmask_d), (rw[:, :, :], rw_d.rearrange("(c p) e -> p c e", p=128))]
        loads += [(xT[c, :, :], xT_in[c, :, :]) for c in range(KD)]
        phase_loads(cx, loads, post)
        phase_ada(cx, adaw10, b10, condS, M10, 3 * KD)
        phase_ada(cx, adaw11, b11, condS, M11, 3 * KD)

        def derive(ch):
            _derive_AB(ch, A10, M10, gv[:, 0, :], KD, D)
            _derive_AB(ch, A11, M11, gv[:, 1, :], KD, D)
        phase_loads(cx, [], derive)
        if stage >= 1:
            with ExitStack() as seg:
                cx.alloc_in(seg)
                phase_norm(cx, xT, 0, A10, M10[:, 0:KD], onesbf)
                tiles = [{"ap": wq[:, cb * 256:(cb + 1) * 256], "jobs": [cb * 2, cb * 2 + 1]}
                         for cb in range(3 * D // 256)]
                phase_gemm(cx, "qproj", KD, 0, tiles,
                           EpiRaw(lambda ch_, tb: qraw[ch_, :, tb * 512:(tb + 1) * 512], F32, 512))
            phase_headnorm(cx, qraw, qnT, 3 * H, qg, onesbf)
        if stage >= 2:
            phase_bias_setup(cx, rb_d, oh_d, gqrow_d, gkrow_d, rbrow_d, extrep, NEGC, ones32)
            phase_attention(cx, qnT, knTw[:, :, 0:T], knTw[:, :, T:2 * T], vw[0:T, :], vw[T:2 * T, :], extrep, oT,
                            NEGC, cmask, onesbf)
        seg2 = ExitStack()
        cx.alloc_in(seg2)
        if stage >= 3:
            tiles = [{"ap": wo[:, cb * 256:(cb + 1) * 256], "jobs": [cb * 2, cb * 2 + 1]} for cb in range(D // 256)]
            phase_gemm(cx, "oproj", KD, 0, tiles, EpiResid(xT, 0, M10[:, 2 * KD:3 * KD]), in_src=oT)
        if stage >= 4:
            phase_norm(cx, xT, 0, A11, M11[:, 0:KD], onesbf, mode="router",
                       router={"rw": rw, "ident": ident, "ones32": ones32, "comb": comb})
            tiles = []
            for ex in range(NE):
                for j in range(DFE // 128):
                    fc = ex * (DFE // 128) + j
                    tiles.append({"ap": mw1[ex, :, j * 128:(j + 1) * 128], "jobs": [(0, fc, ex)]})
                    tiles.append({"ap": mw3[ex, :, j * 128:(j + 1) * 128], "jobs": [(1, fc, ex)]})
            phase_gemm(cx, "moe1", KD, 0, tiles, EpiSwiglu(aT, comb), WCOLS=128)
            for c0 in range(0, NFC, 32):
                kc = min(32, NFC - c0)
                tiles = [{"ap": mw2[c0 * 128:(c0 + kc) * 128, cb * 256:(cb + 1) * 256], "jobs": [cb * 2, cb * 2 + 1]}
                         for cb in range(D // 256)]
                phase_gemm(cx, "moe2", kc, 0, tiles, EpiResid(xT, 0, M11[:, 2 * KD:3 * KD]),
                           in_src=aT[c0:c0 + kc, :, :])
        seg2.close()
        phase_transpose_out(cx, xT, 0, out, ident)
    return nc


def _t5_bucket_np(n):
    n = np.asarray(n, np.int64)
    max_exact = 16
    nf = np.maximum(n, 1).astype(np.float32)
    large = max_exact + (np.log(nf / np.float32(max_exact)) / np.float32(np.log(2048 / max_exact))
                         * np.float32(32 - max_exact)).astype(np.int32)
    return np.where(n < max_exact, n, np.minimum(large, 31))


def prep_l1(inp, cfg, core, xT1, knT_all, v_all):
    b, half = divmod(core, 2)
    D, KD, T, H = cfg.D, cfg.KD, cfg.T, cfg.H
    knTw = np.zeros((H, 128, 2 * T), ml_dtypes.bfloat16)
    vw = np.zeros((2 * T, D), ml_dtypes.bfloat16)
    knTw[:, :, T:] = knT_all[core]
    vw[T:] = v_all[core]
    if half == 1:
        knTw[:, :, :T] = knT_all[core - 1]
        vw[:T] = v_all[core - 1]
    oh = np.zeros((32, 3, 129), np.float32)
    for g, (_, d) in enumerate(PATTERNS):
        bk = _t5_bucket_np(np.arange(129) * d)
        oh[bk, g, np.arange(129)] = 1.0
    gvecs = np.stack([_colT(inp["norm_g"][1, 0], KD), _colT(inp["norm_g"][1, 1], KD)], axis=1)
    cm = np.full((128, 1), NEG if half == 0 else 0.0, np.float32)
    NE, DFE = cfg.NE, cfg.DFE
    return {
        "xT_in": xT1, "knTw": knTw, "vw": vw, "condT": _colT(inp["c"][b], KD),
        "adaw10": inp["ada_w"][1, 0], "adab10T": _colT(inp["ada_b"][1, 0], 3 * KD),
        "adaw11": inp["ada_w"][1, 1], "adab11T": _colT(inp["ada_b"][1, 1], 3 * KD),
        "gvecs": np.ascontiguousarray(gvecs), "qgT": np.asarray(inp["q_norm_g"][0], np.float32).reshape(128, 1),
        "wq": inp["w_q"][0], "wo": inp["w_o"][0], "relb": np.ascontiguousarray(inp["rel_bias"]),
        "relbrow": np.ascontiguousarray(inp["rel_bias"]).reshape(1, -1),
        "gqrow": np.asarray(inp["q_norm_g"][0], np.float32).reshape(1, 128),
        "gkrow": np.asarray(inp["k_norm_g"], np.float32).reshape(1, 128),
        "oh": oh, "cmask": cm, "routerw": inp["router_w"][0],
        "mw1": inp["moe_w1"][0], "mw3": inp["moe_w3"][0], "mw2": inp["moe_w2"][0].reshape(NE * DFE, D),
        "ident": np.eye(128, dtype=np.float32), "ones32": np.ones((128, 128), np.float32),
        "onesbf": np.ones((128, 128), ml_dtypes.bfloat16),
    }


def _l0_pass(cx, cfg, tag, xin, xT, knT, v_o, aT, kraw, w, sbt):
    nc = cx.nc
    D, KD, T, H, G, DFF = cfg.D, cfg.KD, cfg.T, cfg.H, cfg.G, cfg.DFF
    KG = KD // 4
    phase_transpose_in(cx, xin, xT, sbt["ident"], HALO + T)
    with ExitStack() as seg:
        cx.alloc_in(seg)
        phase_norm(cx, xT, HALO, sbt["A00"], sbt["M00"][:, 0:KD], sbt["onesbf"], mode="pool",
                   pool={"hmask": sbt["hmask" + tag], "invc": sbt["invc" + tag]})
        for g in range(4):
            tiles = []
            for cb in range(G // 256):
                tiles.append({"ap": w["poolw"][g, :, cb * 256:(cb + 1) * 256],
                              "jobs": [g * KG + cb * 2, g * KG + cb * 2 + 1]})
            phase_gemm(cx, f"pool{g}", KG, g * KG, tiles, EpiResid(xT, HALO, sbt["GT0"]))
        phase_norm(cx, xT, HALO, sbt["A01"], sbt["M01"][:, 0:KD], sbt["onesbf"])
        tiles = []
        for j in range(DFF // 128):
            tiles.append({"ap": w["w1"][:, j * 128:(j + 1) * 128], "jobs": [(0, j, 0)]})
            tiles.append({"ap": w["w3"][:, j * 128:(j + 1) * 128], "jobs": [(1, j, 0)]})
        phase_gemm(cx, "ffn1", KD, 0, tiles, EpiSwiglu(aT), WCOLS=128)
        nfc = DFF // 128
        for c0 in range(0, nfc, 32):
            kc = min(32, nfc - c0)
            tiles = [{"ap": w["w2"][c0 * 128:(c0 + kc) * 128, cb * 256:(cb + 1) * 256], "jobs": [cb * 2, cb * 2 + 1]}
                     for cb in range(D // 256)]
            phase_gemm(cx, "ffn2", kc, 0, tiles, EpiResid(xT, HALO, sbt["M01"][:, 2 * KD:3 * KD]),
                       in_src=aT[c0:c0 + kc, :, :])
        phase_norm(cx, xT, HALO, sbt["AKV"], sbt["MKV"][:, 0:KD], sbt["onesbf"])
        tiles = [{"ap": w["wk"][:, cb * 256:(cb + 1) * 256], "jobs": [cb * 2, cb * 2 + 1]} for cb in range(D // 256)]
        phase_gemm(cx, "kproj", KD, 0, tiles,
                   EpiRaw(lambda ch_, tb: kraw[ch_, :, tb * 512:(tb + 1) * 512], F32, 512))
        phase_headnorm(cx, kraw, knT, H, sbt["kg"], sbt["onesbf"])
        tiles = [{"ap": w["wv"][:, cb * 256:(cb + 1) * 256], "jobs": [cb]} for cb in range(D // 256)]
        phase_gemm(cx, "vproj", KD, 0, tiles,
                   EpiRaw(lambda cb, tt: v_o[tt * 128:(tt + 1) * 128, cb * 256:(cb + 1) * 256], BF16, 256),
                   mode="tm")


def build_fused(cfg):
    nc = bass.Bass("TRN2", target_bir_lowering=False)
    D, KD, T, H, G, DFF, NE, DFE = cfg.D, cfg.KD, cfg.T, cfg.H, cfg.G, cfg.DFF, cfg.NE, cfg.DFE
    xinA = _din(nc, "xinA", [HALO + T, D])
    xinB = _din(nc, "xinB", [HALO + T, D])
    condT_d = _din(nc, "condT", [128, KD])
    adaw = {k: _din(nc, "adaw" + k, [D, 3 * D]) for k in ("00", "01", "10", "11")}
    adab = {k: _din(nc, "adab" + k + "T", [128, 3 * KD]) for k in ("00", "01", "10", "11")}
    kvadaw = _din(nc, "kvadaw", [D, 2 * D])
    kvadab = _din(nc, "kvadabT", [128, 2 * KD])
    gvecs_d = _din(nc, "gvecs", [128, 6, KD])
    kg_d = _din(nc, "kgT", [128, 1])
    qg_d = _din(nc, "qgT", [128, 1])
    w = {"poolw": _din(nc, "poolw", [4, G, G]), "w1": _din(nc, "w1", [D, DFF]), "w3": _din(nc, "w3", [D, DFF]),
         "w2": _din(nc, "w2", [DFF, D]), "wk": _din(nc, "wk", [D, D]), "wv": _din(nc, "wv", [D, D])}
    wq = _din(nc, "wq", [D, 3 * D])
    wo = _din(nc, "wo", [D, D])
    rb_d = _din(nc, "relb", [32, 3, H])
    rbrow_d = _din(nc, "relbrow", [1, 3 * 32 * H])
    gqrow_d = _din(nc, "gqrow", [1, 128])
    gkrow_d = _din(nc, "gkrow", [1, 128])
    oh_d = _din(nc, "oh", [32, 3, 129])
    cmask_d = _din(nc, "cmask", [128, 1])
    rw_d = _din(nc, "routerw", [D, NE])
    mw1 = _din(nc, "mw1", [NE, D, DFE])
    mw3 = _din(nc, "mw3", [NE, D, DFE])
    mw2 = _din(nc, "mw2", [NE * DFE, D])
    ident_d = _din(nc, "ident", [128, 128])
    ones32_d = _din(nc, "ones32", [128, 128])
    onesbf_d = _din(nc, "onesbf", [128, 128], BF16)
    hmA_d, hmB_d = _din(nc, "hmaskA", [128, 2]), _din(nc, "hmaskB", [128, 2])
    icA_d, icB_d = _din(nc, "invcA", [128, 4, HALO]), _din(nc, "invcB", [128, 4, HALO])
    out = _dout(nc, "out", [T, D])
    xTA = _dint(nc, "xTA", [KD, 128, HALO + T])
    xTB = _dint(nc, "xTB", [KD, 128, HALO + T])
    knA, knB = _dint(nc, "knA", [H, 128, T], BF16), _dint(nc, "knB", [H, 128, T], BF16)
    vA, vB = _dint(nc, "vA", [T, D], BF16), _dint(nc, "vB", [T, D], BF16)
    NFC = NE * DFE // 128
    aT = _dint(nc, "aT", [max(DFF // 128, NFC), 128, T], BF16)
    kraw = _dint(nc, "qkraw", [3 * H, 128, T])
    qnT = _dint(nc, "qnT", [3 * H, 128, T], BF16)
    oT = _dint(nc, "oT", [H, 128, T], BF16)
    extrep = _dint(nc, "extrep", [3, H, 128, 384])
    comb = _dint(nc, "comb", [NE, 128, T])

    with ExitStack() as es:
        cx = Ctx(nc, cfg, es)
        sb = lambda name, shape, dt=F32: es.enter_context(nc.sbuf_tensor("s_" + name, shape, dt))
        t = {}
        t["ident"], t["ones32"] = sb("ident", [128, 128]), sb("ones32", [128, 128])
        t["onesbf"] = sb("onesbf", [128, 128], BF16)
        condS = sb("condS", [128, KD])
        bT = {k: sb("b" + k, [128, 3 * KD]) for k in ("00", "01", "10", "11")}
        bkv = sb("bkv", [128, 2 * KD])
        for k in ("00", "01", "10", "11"):
            t["M" + k] = sb("M" + k, [128, 3 * KD])
            t["A" + k] = sb("A" + k, [128, KD])
        t["MKV"], t["AKV"], t["GT0"] = sb("MKV", [128, 2 * KD]), sb("AKV", [128, KD]), sb("GT0", [128, KD])
        gv = sb("gv", [128, 6, KD])
        t["kg"], qg = sb("kg", [128, 1]), sb("qg", [128, 1])
        t["hmaskA"], t["hmaskB"] = sb("hmaskA", [128, 2]), sb("hmaskB", [128, 2])
        t["invcA"], t["invcB"] = sb("invcA", [128, 4, HALO]), sb("invcB", [128, 4, HALO])
        cmask, NEGC = sb("cmask", [128, 1]), sb("NEGC", [128, 1])
        rw = sb("rw", [128, KD, NE])

        def post(ch):
            ch.op("dve", lambda e: e.memset(cx.epsD[:, :], float(D * EPS)))
            ch.op("dve", lambda e: e.memset(cx.epsE[:, :], float(128 * EPS)))
            ch.op("act", lambda e: e.activation(out=condS[:, :], in_=condS[:, :], func=AF.Silu))
            ch.op("dve", lambda e: e.tensor_scalar(out=t["kg"][:, :], in0=t["kg"][:, :], scalar1=float(np.sqrt(128.0)),
                                                   scalar2=None, op0=ALU.mult))
        loads = [(t["ident"][:, :], ident_d), (t["ones32"][:, :], ones32_d), (t["onesbf"][:, :], onesbf_d),
                 (condS[:, :], condT_d), (bkv[:, :], kvadab), (gv[:, :, :], gvecs_d), (t["kg"][:, :], kg_d),
                 (qg[:, :], qg_d), (t["hmaskA"][:, :], hmA_d), (t["hmaskB"][:, :], hmB_d),
                 (t["invcA"][:, :, :], icA_d), (t["invcB"][:, :, :], icB_d), (cmask[:, :], cmask_d),
                 (rw[:, :, :], rw_d.rearrange("(c p) e -> p c e", p=128))]
        loads += [(bT[k][:, :], adab[k]) for k in ("00", "01", "10", "11")]
        phase_loads(cx, loads, post)
        for k in ("00", "01", "10", "11"):
            phase_ada(cx, adaw[k], bT[k], condS, t["M" + k], 3 * KD)
        phase_ada(cx, kvadaw, bkv, condS, t["MKV"], 2 * KD)

        def derive(ch):
            _derive_AB(ch, t["A00"], t["M00"], gv[:, 0, :], KD, D)
            _derive_AB(ch, t["A01"], t["M01"], gv[:, 1, :], KD, D)
            _derive_AB(ch, t["AKV"], t["MKV"], gv[:, 2, :], KD, D)
            _derive_AB(ch, t["A10"], t["M10"], gv[:, 4, :], KD, D)
            _derive_AB(ch, t["A11"], t["M11"], gv[:, 5, :], KD, D)
            ch.op("dve", lambda e: e.tensor_tensor(out=t["GT0"][:, :], in0=t["M00"][:, 2 * KD:3 * KD], in1=gv[:, 3, :],
                                                   op=ALU.mult))
        phase_loads(cx, [], derive)
        _l0_pass(cx, cfg, "A", xinA, xTA, knA, vA, aT, kraw, w, t)
        _l0_pass(cx, cfg, "B", xinB, xTB, knB, vB, aT, kraw, w, t)
        xT = xTB
        with ExitStack() as seg:
            cx.alloc_in(seg)
            phase_norm(cx, xT, HALO, t["A10"], t["M10"][:, 0:KD], t["onesbf"])
            tiles = [{"ap": wq[:, cb * 256:(cb + 1) * 256], "jobs": [cb * 2, cb * 2 + 1]} for cb in range(3 * D // 256)]
            phase_gemm(cx, "qproj", KD, 0, tiles,
                       EpiRaw(lambda ch_, tb: kraw[ch_, :, tb * 512:(tb + 1) * 512], F32, 512))
        phase_headnorm(cx, kraw, qnT, 3 * H, qg, t["onesbf"])
        phase_bias_setup(cx, rb_d, oh_d, gqrow_d, gkrow_d, rbrow_d, extrep, NEGC, t["ones32"])
        phase_attention(cx, qnT, knA, knB, vA, vB, extrep, oT, NEGC, cmask, t["onesbf"])
        with ExitStack() as seg:
            cx.alloc_in(seg)
            tiles = [{"ap": wo[:, cb * 256:(cb + 1) * 256], "jobs": [cb * 2, cb * 2 + 1]} for cb in range(D // 256)]
            phase_gemm(cx, "oproj", KD, 0, tiles, EpiResid(xT, HALO, t["M10"][:, 2 * KD:3 * KD]), in_src=oT)
            phase_norm(cx, xT, HALO, t["A11"], t["M11"][:, 0:KD], t["onesbf"], mode="router",
                       router={"rw": rw, "ident": t["ident"], "ones32": t["ones32"], "comb": comb})
            tiles = []
            for ex in range(NE):
                for j in range(DFE // 128):
                    fc = ex * (DFE // 128) + j
                    tiles.append({"ap": mw1[ex, :, j * 128:(j + 1) * 128], "jobs": [(0, fc, ex)]})
                    tiles.append({"ap": mw3[ex, :, j * 128:(j + 1) * 128], "jobs": [(1, fc, ex)]})
            phase_gemm(cx, "moe1", KD, 0, tiles, EpiSwiglu(aT, comb), WCOLS=128)
            for c0 in range(0, NFC, 32):
                kc = min(32, NFC - c0)
                tiles = [{"ap": mw2[c0 * 128:(c0 + kc) * 128, cb * 256:(cb + 1) * 256], "jobs": [cb * 2, cb * 2 + 1]}
                         for cb in range(D // 256)]
                phase_gemm(cx, "moe2", kc, 0, tiles, EpiResid(xT, HALO, t["M11"][:, 2 * KD:3 * KD]),
                           in_src=aT[c0:c0 + kc, :, :])
        phase_transpose_out(cx, xT, HALO, out, t["ident"])
    return nc


def prep_fused(inp, cfg, core):
    b, half = divmod(core, 2)
    D, KD, T, H, NE, DFE = cfg.D, cfg.KD, cfg.T, cfg.H, cfg.NE, cfg.DFE
    x = inp["x"]
    xinB = np.zeros((HALO + T, D), np.float32)
    xinB[HALO:] = x[b, half * T:(half + 1) * T]
    xinA = np.zeros((HALO + T, D), np.float32)
    if half == 1:
        xinB[:HALO] = x[b, T - HALO:T]
        xinA[HALO:] = x[b, 0:T]
    invc_start = np.zeros((128, 4, HALO), np.float32)
    invc_mid = np.zeros((128, 4, HALO), np.float32)
    for g, k in enumerate((2, 4, 8, 16)):
        for t_ in range(HALO):
            invc_start[:, g, t_] = 1.0 / min(t_ + 1, k)
            invc_mid[:, g, t_] = 1.0 / k
    oh = np.zeros((32, 3, 129), np.float32)
    for g, (_, d) in enumerate(PATTERNS):
        bk = _t5_bucket_np(np.arange(129) * d)
        oh[bk, g, np.arange(129)] = 1.0
    gvecs = np.stack([_colT(inp["norm_g"][0, 0], KD), _colT(inp["norm_g"][0, 1], KD), _colT(inp["kv_norm_g"], KD),
                      _colT(inp["pool_scale"][0], KD), _colT(inp["norm_g"][1, 0], KD), _colT(inp["norm_g"][1, 1], KD)],
                     axis=1)
    m = {
        "xinA": xinA, "xinB": xinB, "condT": _colT(inp["c"][b], KD),
        "kvadaw": inp["kv_ada_w"], "kvadabT": _colT(inp["kv_ada_b"], 2 * KD),
        "gvecs": np.ascontiguousarray(gvecs),
        "kgT": np.asarray(inp["k_norm_g"], np.float32).reshape(128, 1),
        "qgT": np.asarray(inp["q_norm_g"][0], np.float32).reshape(128, 1),
        "poolw": inp["pool_w"][0], "w1": inp["ffn_w1"][0], "w3": inp["ffn_w3"][0], "w2": inp["ffn_w2"][0],
        "wk": inp["w_k"], "wv": inp["w_v"], "wq": inp["w_q"][0], "wo": inp["w_o"][0],
        "relb": np.ascontiguousarray(inp["rel_bias"]),
        "relbrow": np.ascontiguousarray(inp["rel_bias"]).reshape(1, -1),
        "gqrow": np.asarray(inp["q_norm_g"][0], np.float32).reshape(1, 128),
        "gkrow": np.asarray(inp["k_norm_g"], np.float32).reshape(1, 128),
        "oh": oh, "cmask": np.full((128, 1), NEG if half == 0 else 0.0, np.float32),
        "routerw": inp["router_w"][0],
        "mw1": inp["moe_w1"][0], "mw3": inp["moe_w3"][0], "mw2": inp["moe_w2"][0].reshape(NE * DFE, D),
        "ident": np.eye(128, dtype=np.float32), "ones32": np.ones((128, 128), np.float32),
        "onesbf": np.ones((128, 128), ml_dtypes.bfloat16),
        "hmaskA": np.zeros((128, 2), np.float32), "hmaskB": np.full((128, 2), float(half), np.float32),
        "invcA": invc_start, "invcB": invc_start if half == 0 else invc_mid,
    }
    for i in range(2):
        for j in range(2):
            m[f"adaw{i}{j}"] = inp["ada_w"][i, j]
            m[f"adab{i}{j}T"] = _colT(inp["ada_b"][i, j], 3 * KD)
    return m


_CFG = Cfg()


def kernel(**inputs):
    cfg = _CFG
    inp = {k: np.asarray(v) for k, v in inputs.items()}
    n = 8
    nc = build_fused(cfg)
    maps = [prep_fused(inp, cfg, c) for c in range(n)]
    res = run_bass_kernel_spmd(nc, maps, core_ids=list(range(n))).results
    out = np.empty((cfg.B, cfg.SEQ, cfg.D), np.float32)
    for c in range(n):
        b, half = divmod(c, 2)
        out[b, half * cfg.T:(half + 1) * cfg.T] = res[c]["out"]
    return out
```

```python
import numpy as np
import ml_dtypes
from contextlib import ExitStack
import concourse.bass as bass
import concourse.mybir as mybir
from concourse.bass_utils import run_bass_kernel_spmd

F32 = mybir.dt.float32
BF16 = mybir.dt.bfloat16
AF = mybir.ActivationFunctionType
ALU = mybir.AluOpType
NEG = -1.0e30
EPS = 1e-6
HALO = 16
PATTERNS = ((128, 1), (512, 4), (2048, 16))


class Cfg:
    def __init__(self, D=4096, DFF=11008, DFE=3584, NE=8, SEQ=4096, B=4):
        self.D = D
        self.KD = D // 128
        self.H = D // 128
        self.DFF = DFF
        self.DFE = DFE
        self.NE = NE
        self.SEQ = SEQ
        self.B = B
        self.T = SEQ // 2
        self.G = D // 4


class Sem:
    def __init__(self, h):
        self.h = h
        self.n = 0


class Prog:
    def __init__(self):
        self.q = {k: [] for k in ("sp", "act", "dve", "pool", "pe")}

    key = None

    def op(self, eng, fn, waits=(), inc=None, k=1):
        if inc is not None:
            inc.n += k
        self.q[eng].append((tuple(waits), fn, inc, k, self.key))

    def merged(self, eng):
        q = self.q[eng]
        if any(o[4] is not None for o in q):
            assert all(o[4] is not None for o in q)
            q = sorted(q, key=lambda o: o[4])
        return q

    def dma(self, eng, out, in_, waits=(), inc=None):
        self.op(eng, lambda e, o=out, i=in_: e.dma_start(out=o, in_=i), waits, inc, 16)


class Chain:
    def __init__(self, P, cs, ds, cid=None):
        self.P, self.cs, self.ds = P, cs, ds
        self.last = None
        self.cid = cid
        self.step = 0

    def _key(self):
        if self.cid is not None:
            self.P.key = (self.step, self.cid)
            self.step += 1

    def op(self, eng, fn, extra=()):
        w = list(extra)
        if self.last:
            w.append(self.last)
        self._key()
        self.P.op(eng, fn, w, self.cs, 1)
        self.P.key = None
        self.last = (self.cs, self.cs.n)

    def dma(self, eng, out, in_, extra=()):
        w = list(extra)
        if self.last:
            w.append(self.last)
        self._key()
        self.P.dma(eng, out, in_, w, self.ds)
        self.P.key = None
        self.last = (self.ds, self.ds.n)


def _run(e, ops):
    seen = {}
    for waits, fn, inc, k, _key in ops:
        for (s, v) in waits:
            if v <= 0:
                continue
            key = id(s)
            if seen.get(key, 0) >= v:
                continue
            seen[key] = v
            e.wait_ge(s.h, v)
        ins = fn(e)
        if inc is not None:
            ins.then_inc(inc.h, k)


def emit(nc, P):
    with nc.Block() as block:
        if P.q["sp"]:
            @block.sync
            def _(e):
                _run(e, P.merged("sp"))
        if P.q["act"]:
            @block.scalar
            def _(e):
                _run(e, P.merged("act"))
        if P.q["dve"]:
            @block.vector
            def _(e):
                _run(e, P.merged("dve"))
        if P.q["pool"]:
            @block.gpsimd
            def _(e):
                _run(e, P.merged("pool"))
        if P.q["pe"]:
            @block.tensor
            def _(e):
                _run(e, P.merged("pe"))


class Ctx:
    def __init__(self, nc, cfg, es):
        self.nc = nc
        self.cfg = cfg
        self.ps = [es.enter_context(nc.psum_tensor(f"psb{i}", [128, 512], F32)) for i in range(8)]
        self.IN = None
        self.scr = es.enter_context(nc.sbuf_tensor("scr", [128, 16], F32))
        self.epsD = es.enter_context(nc.sbuf_tensor("epsD", [128, 1], F32))
        self.epsE = es.enter_context(nc.sbuf_tensor("epsE", [128, 1], F32))
        self.uid = 0
        self._phase_sems = {}

    def alloc_in(self, es):
        self.uid += 1
        self.IN = es.enter_context(self.nc.sbuf_tensor(f"IN_{self.uid}", [128, 32, self.cfg.T], BF16))

    def sb(self, es, name, shape, dt):
        self.uid += 1
        return es.enter_context(self.nc.sbuf_tensor(f"{name}_{self.uid}", shape, dt))

    def sem(self, es, name):
        self.uid += 1
        h = self.nc.alloc_semaphore(name=f"{name}_{self.uid}")
        lst = getattr(es, "_sem_list", None)
        if lst is None:
            lst = []
            es._sem_list = lst

            def cleanup(lst=lst):
                self.nc.clear_and_free_semaphores(lst)
                self.nc.all_engine_barrier()
            es.callback(cleanup)
        lst.append(h)
        return Sem(h)


def phase_loads(cx, pairs, post=None):
    with ExitStack() as es:
        cs, ds = cx.sem(es, "lc"), cx.sem(es, "ld")
        P = Prog()
        for (o, i) in pairs:
            P.dma("sp", o, i, inc=ds)
        ch = Chain(P, cs, ds)
        ch.last = (ds, ds.n)
        if post is not None:
            post(ch)
        else:
            ch.op("dve", lambda e: e.memset(cx.scr[0:1, 0:1], 0.0))
        emit(cx.nc, P)


def phase_ada(cx, w_ap, bT_sb, condS, mod_sb, NJ):
    nc, cfg = cx.nc, cx.cfg
    KD = cfg.KD
    with ExitStack() as es:
        NBUF = 2
        AW = [cx.sb(es, "aw", [128, KD, 256], F32) for i in range(NBUF)]
        ld = [cx.sem(es, "awld") for i in range(NBUF)]
        s_mm = cx.sem(es, "awmm")
        s_fin = cx.sem(es, "awfin")
        P = Prog()
        wv = w_ap.rearrange("(kc p) n -> p kc n", p=128)
        ntile = NJ // 2
        ps = cx.ps[0]
        for t in range(ntile):
            b = t % NBUF
            P.dma("sp", AW[b][:], wv[:, :, t * 256:(t + 1) * 256],
                  waits=[(s_mm, t - NBUF + 1)], inc=ld[b])
            for jj in range(2):
                j = t * 2 + jj
                for kc in range(KD):
                    last = (jj == 1 and kc == KD - 1)
                    P.op("pe",
                         lambda e, b=b, jj=jj, kc=kc, j=j: e.matmul(
                             ps[:, j:j + 1], lhsT=AW[b][:, kc, jj * 128:(jj + 1) * 128],
                             rhs=condS[:, kc:kc + 1], start=(kc == 0), stop=(kc == KD - 1)),
                         waits=[(ld[b], 16 * (t // NBUF + 1))],
                         inc=s_mm if last else None)
        P.op("dve", lambda e: e.tensor_tensor(out=mod_sb[:, 0:NJ], in0=ps[:, 0:NJ], in1=bT_sb[:, 0:NJ], op=ALU.add),
             waits=[(s_mm, ntile)], inc=s_fin)
        emit(nc, P)


def _transpose_steps(ch, banks, KD, pe_fn, src_fn, dst_fn):
    for g0 in range(0, KD, 16):
        gcs = list(range(g0, min(KD, g0 + 16)))

        def tr(e, gcs=gcs, g0=g0):
            ins = None
            for c in gcs:
                ins = pe_fn(e, banks[(c - g0) // 4], (c - g0) % 4, c)
            return ins
        ch.op("pe", tr)
        nb = (len(gcs) + 3) // 4

        def ev(e, par, g0=g0, nb=nb):
            ins = None
            for bk in range(par, nb, 2):
                c0 = g0 + bk * 4
                ncz = min(4, KD - c0)
                s, d = src_fn(banks[bk], ncz), dst_fn(c0, ncz)
                ins = e.copy(out=d, in_=s) if par == 0 else e.tensor_copy(out=d, in_=s)
            return ins
        ch.op("act", lambda e, ev=ev: ev(e, 0))
        if nb > 1:
            ch.op("dve", lambda e, ev=ev: ev(e, 1))


def phase_transpose_in(cx, x_ap, xT_ap, ident, ntok_total):
    nc, cfg = cx.nc, cx.cfg
    KD, D = cfg.KD, cfg.D
    xTv = xT_ap.rearrange("c p t -> p c t")
    tiles = []
    t0 = 0
    rem = ntok_total % 128
    if rem:
        tiles.append((0, rem))
        t0 = rem
    while t0 < ntok_total:
        tiles.append((t0, 128))
        t0 += 128
    NCH = 2
    with ExitStack() as es:
        P = Prog()
        for cid in range(NCH):
            XI = cx.sb(es, "xi", [128, D], F32)
            XO = cx.sb(es, "xo", [128, KD, 128], F32)
            cs, ds = cx.sem(es, "tc"), cx.sem(es, "td")
            ch = Chain(P, cs, ds, cid)
            banks = cx.ps[4 * cid:4 * cid + 4]
            for (t0, n) in tiles[cid::NCH]:
                ch.dma("sp", XI[0:n, :], x_ap[t0:t0 + n, :])
                _transpose_steps(
                    ch, banks, KD,
                    lambda e, bank, cc, c, n=n, XI=XI: e.transpose(bank[:, cc * 128:cc * 128 + n],
                                                                   XI[0:n, c * 128:(c + 1) * 128], ident[0:n, 0:n]),
                    lambda bank, ncz, n=n: bank[:, 0:ncz * 128].rearrange("p (c t) -> p c t", t=128)[:, :, 0:n],
                    lambda c0, ncz, n=n, XO=XO: XO[:, c0:c0 + ncz, 0:n])
                ch.dma("sp", xTv[:, :, t0:t0 + n], XO[:, :, 0:n])
            ch.op("dve", lambda e, XO=XO: e.memset(XO[0:1, 0, 0:1], 0.0))
        emit(nc, P)


def phase_transpose_out(cx, xT_ap, col0, out_ap, ident):
    nc, cfg = cx.nc, cx.cfg
    KD, D, T = cfg.KD, cfg.D, cfg.T
    xTv = xT_ap.rearrange("c p t -> p c t")
    NCH = 2
    tiles = list(range(0, T, 128))
    with ExitStack() as es:
        P = Prog()
        for cid in range(NCH):
            XI = cx.sb(es, "yi", [128, KD, 128], F32)
            XO = cx.sb(es, "yo", [128, D], F32)
            cs, ds = cx.sem(es, "tc"), cx.sem(es, "td")
            ch = Chain(P, cs, ds, cid)
            banks = cx.ps[4 * cid:4 * cid + 4]
            for t0 in tiles[cid::NCH]:
                ch.dma("sp", XI[:, :, :], xTv[:, :, col0 + t0:col0 + t0 + 128])
                _transpose_steps(
                    ch, banks, KD,
                    lambda e, bank, cc, c, XI=XI: e.transpose(bank[:, cc * 128:(cc + 1) * 128], XI[:, c, :],
                                                              ident[:, :]),
                    lambda bank, ncz: bank[:, 0:ncz * 128],
                    lambda c0, ncz, XO=XO: XO[:, c0 * 128:(c0 + ncz) * 128])
                ch.dma("sp", out_ap[t0:t0 + 128, :], XO[:, :])
            ch.op("dve", lambda e, XO=XO: e.memset(XO[0:1, 0:1], 0.0))
        emit(nc, P)


def phase_norm(cx, xT_ap, col0, A_sb, B_sb, ones_bf, mode="plain", pool=None, router=None):
    nc, cfg = cx.nc, cx.cfg
    KD, D, T = cfg.KD, cfg.D, cfg.T
    xTv = xT_ap.rearrange("c p t -> p c t")
    IN = cx.IN
    blocks = [(t, 128) for t in range(0, T, 128)]
    if mode == "pool":
        blocks = [(-HALO, HALO)] + blocks
    NCH = 2 if mode == "plain" else 1
    with ExitStack() as es:
        P = Prog()
        if mode == "pool":
            HB = cx.sb(es, "nhb", [128, KD, 128 + HALO], F32)
            S1 = cx.sb(es, "ns1", [128, KD // 4, 128 + HALO], F32)
            S2 = cx.sb(es, "ns2", [128, KD // 4, 128 + HALO], F32)
        if mode == "router":
            NE = cfg.NE
            HF = cx.sb(es, "nhf", [128, KD, 128], F32)
            LG = cx.sb(es, "nlg", [128, 16], F32)
            M8 = cx.sb(es, "nm8", [128, 16], F32)
            CB = cx.sb(es, "ncb", [128, 16], F32)
            DG = cx.sb(es, "ndg", [128, NE, 128], F32)
            CO = cx.sb(es, "nco", [128, NE, 128], F32)
            combv = router["comb"].rearrange("e p t -> p e t")
        for cid in range(NCH):
            XB = cx.sb(es, "nxb", [128, KD, 128], F32)
            SQ = cx.sb(es, "nsq", [128, KD, 128], BF16)
            RS = cx.sb(es, "nrs", [128, 128], F32)
            cs, ds = cx.sem(es, "nc"), cx.sem(es, "nd")
            ch = Chain(P, cs, ds, cid if NCH > 1 else None)
            psS = cx.ps[cid]
            for (t0, n) in blocks[cid::NCH]:
                ch.dma("sp", XB[:, :, 0:n], xTv[:, :, col0 + t0:col0 + t0 + n])
                ch.op("act", lambda e, n=n, SQ=SQ, XB=XB: e.activation(out=SQ[:, :, 0:n], in_=XB[:, :, 0:n],
                                                                       func=AF.Square))

                def ssmm(e, n=n, SQ=SQ, psS=psS):
                    ins = None
                    for c in range(KD):
                        ins = e.matmul(psS[:, 0:n], lhsT=ones_bf[:, :], rhs=SQ[:, c, 0:n],
                                       start=(c == 0), stop=(c == KD - 1))
                    return ins
                ch.op("pe", ssmm)
                ch.op("act", lambda e, n=n, RS=RS, psS=psS: e.activation(out=RS[:, 0:n], in_=psS[:, 0:n], func=AF.Sqrt,
                                                                         bias=cx.epsD[:, 0:1], scale=1.0))
                ch.op("dve", lambda e, n=n, RS=RS: e.reciprocal(out=RS[:, 0:n], in_=RS[:, 0:n]))

                def nrm(e, n=n, XB=XB, RS=RS):
                    ins = None
                    for c in range(KD):
                        ins = e.scalar_tensor_tensor(out=XB[:, c, 0:n], in0=XB[:, c, 0:n], scalar=A_sb[:, c:c + 1],
                                                     in1=RS[:, 0:n], op0=ALU.mult, op1=ALU.mult)
                    return ins
                ch.op("dve", nrm)

                def shf(e, n=n, t0=t0, XB=XB):
                    ins = None
                    for c in range(KD):
                        if mode == "pool":
                            dst = HB[:, c, HALO:HALO + n] if t0 >= 0 else HB[:, c, 0:HALO]
                        elif mode == "router":
                            dst = HF[:, c, 0:n]
                        else:
                            dst = IN[:, c, t0:t0 + n]
                        ins = e.activation(out=dst, in_=XB[:, c, 0:n], func=AF.Identity, bias=B_sb[:, c:c + 1])
                    return ins
                ch.op("act", shf)
                if mode == "pool":
                    _pool_block(ch, cx, pool, HB, S1, S2, t0)
                if mode == "router":
                    _router_block(ch, cx, router, HF, LG, M8, CB, DG, CO, combv, t0)
            ch.op("dve", lambda e, RS=RS: e.memset(RS[0:1, 0:1], 0.0))
        emit(nc, P)


def _pool_block(ch, cx, pool, HB, S1, S2, t0):
    cfg = cx.cfg
    KG = cfg.KD // 4
    IN = cx.IN
    W = 128 + HALO
    if t0 < 0:
        ch.op("dve", lambda e: e.tensor_scalar(out=HB[:, :, 0:HALO], in0=HB[:, :, 0:HALO],
                                               scalar1=pool["hmask"][:, 0:1], scalar2=None, op0=ALU.mult))
        return
    first = (t0 == 0)
    for g, k in enumerate((2, 4, 8, 16)):
        cs_ = slice(g * KG, (g + 1) * KG)
        cur, cur_cs, lo, step, it = HB, cs_, 0, 1, 0
        bufs = [S1, S2]
        while step < k:
            dst = bufs[it % 2]
            nlo = lo + step
            ch.op("dve", lambda e, dst=dst, cur=cur, cur_cs=cur_cs, nlo=nlo, step=step: e.tensor_tensor(
                out=dst[:, :, nlo:W], in0=cur[:, cur_cs, nlo:W], in1=cur[:, cur_cs, nlo - step:W - step], op=ALU.add))
            cur, cur_cs, lo = dst, slice(0, KG), nlo
            step *= 2
            it += 1
        ch.op("dve", lambda e, cur=cur, cs_=cs_, k=k: e.scalar_tensor_tensor(
            out=IN[:, cs_, t0:t0 + 128], in0=cur[:, :, HALO:W], scalar=1.0 / k, in1=HB[:, cs_, HALO:W],
            op0=ALU.mult, op1=ALU.subtract))
        if first:
            def fix(e, cur=cur, g=g):
                ins = None
                for c in range(KG):
                    ins = e.tensor_tensor(out=cur[:, c, HALO:2 * HALO], in0=cur[:, c, HALO:2 * HALO],
                                          in1=pool["invc"][:, g, :], op=ALU.mult)
                return ins
            ch.op("dve", fix)
            ch.op("dve", lambda e, cur=cur, cs_=cs_: e.tensor_tensor(
                out=IN[:, cs_, 0:HALO], in0=cur[:, :, HALO:2 * HALO], in1=HB[:, cs_, HALO:2 * HALO], op=ALU.subtract))
    ch.op("dve", lambda e: e.tensor_copy(out=HB[:, :, 0:HALO], in_=HB[:, :, 128:W]))


def _router_block(ch, cx, R, HF, LG, M8, CB, DG, CO, combv, t0):
    cfg = cx.cfg
    KD, NE = cfg.KD, cfg.NE
    IN = cx.IN
    psR = cx.ps[1]
    ch.op("pool", lambda e: e.tensor_copy(out=IN[:, 0:KD, t0:t0 + 128], in_=HF[:, :, :]))

    def rmm(e):
        ins = None
        for c in range(KD):
            ins = e.matmul(psR[:, 0:NE], lhsT=HF[:, c, :], rhs=R["rw"][:, c, :], start=(c == 0), stop=(c == KD - 1))
        return ins
    ch.op("pe", rmm)

    ch.op("dve", lambda e: e.tensor_copy(out=LG[:, 0:NE], in_=psR[:, 0:NE]))
    ch.op("dve", lambda e: e.max(out=M8[:, 0:8], in_=LG[:, 0:NE]))
    ch.op("dve", lambda e: e.tensor_tensor(out=M8[:, 8:9], in0=M8[:, 1:2], in1=M8[:, 0:1], op=ALU.subtract))
    ch.op("act", lambda e: e.activation(out=M8[:, 9:10], in_=M8[:, 8:9], func=AF.Sigmoid))

    ch.op("dve", lambda e: e.tensor_scalar(out=M8[:, 10:11], in0=M8[:, 9:10], scalar1=-1.0, scalar2=1.0,
                                           op0=ALU.mult, op1=ALU.add))

    def cm2(e):
        e.tensor_scalar(out=CB[:, 0:NE], in0=LG[:, 0:NE], scalar1=M8[:, 0:1], scalar2=M8[:, 10:11],
                        op0=ALU.is_equal, op1=ALU.mult)
        return e.tensor_scalar(out=CB[:, NE:2 * NE], in0=LG[:, 0:NE], scalar1=M8[:, 1:2], scalar2=M8[:, 9:10],
                               op0=ALU.is_equal, op1=ALU.mult)
    ch.op("dve", cm2)
    ch.op("dve", lambda e: e.tensor_tensor(out=CB[:, 0:NE], in0=CB[:, 0:NE], in1=CB[:, NE:2 * NE], op=ALU.add))

    def cmb(e):
        ins = None
        for ex in range(NE):
            ins = e.tensor_scalar(out=DG[:, ex, :], in0=R["ident"][:, :], scalar1=CB[:, ex:ex + 1], scalar2=None,
                                  op0=ALU.mult)
        return ins
    ch.op("dve", cmb)

    def bmm(e):
        ins = None
        for ex in range(NE):
            bank = cx.ps[2 + ex // 4]
            ins = e.matmul(bank[:, (ex % 4) * 128:(ex % 4 + 1) * 128], lhsT=R["ones32"][:, :], rhs=DG[:, ex, :],
                           start=True, stop=True)
        return ins
    ch.op("pe", bmm)
    for hb in range(NE // 4):
        ch.op("act", lambda e, hb=hb: e.copy(out=CO[:, hb * 4:(hb + 1) * 4, :],
                                             in_=cx.ps[2 + hb][:, :].rearrange("p (a t) -> p a t", t=128)))
    ch.dma("sp", combv[:, :, t0:t0 + 128], CO[:, :, :])


def phase_gemm(cx, name, KC, kc0, tiles, epi, in_src=None, mode="fm", WCOLS=256):
    nc, cfg = cx.nc, cx.cfg
    T = cfg.T
    IN = cx.IN
    NB = T // 512
    with ExitStack() as es:
        NBUF = 3
        WT = [cx.sb(es, "wt", [128, KC, WCOLS], BF16) for _ in range(NBUF)]
        wld = [cx.sem(es, "wld") for _ in range(NBUF)]
        s_mm = cx.sem(es, "gmm")
        s_free = cx.sem(es, "gfree")
        s_in = cx.sem(es, "gin")
        P = Prog()
        epi.setup(cx, es, P, s_mm, s_free)
        in_wait = []
        in_wait_kc = None
        if in_src is not None:
            NIG = 4
            s_ing = [s_in] + [cx.sem(es, "gin") for _ in range(NIG - 1)]
            per = (KC + NIG - 1) // NIG
            in_wait_kc = {}
            for kc in range(KC):
                sg = s_ing[kc // per]
                P.dma("sp", IN[:, kc0 + kc, :], in_src[kc, :, :], inc=sg)
            for kc in range(KC):
                sg = s_ing[kc // per]
                in_wait_kc[kc] = [(sg, sg.n)]
        G = 0
        tile_lastG = []
        for ti, tl in enumerate(tiles):
            b = ti % NBUF
            wv = tl["ap"].rearrange("(kc p) n -> p kc n", p=128)
            wfree = [(s_mm, tile_lastG[ti - NBUF])] if ti >= NBUF else []
            P.dma("pool", WT[b][:, :, :], wv, waits=wfree, inc=wld[b])
            wready = [(wld[b], 16 * (ti // NBUF + 1))]
            if mode == "fm":
                for ji, meta in enumerate(tl["jobs"]):
                    for tb in range(NB):
                        bank = cx.ps[G % 8]
                        for kc in range(KC):
                            P.op("pe", lambda e, bank=bank, b=b, kc=kc, ji=ji, tb=tb: e.matmul(
                                bank[:, :], lhsT=WT[b][:, kc, ji * 128:(ji + 1) * 128],
                                rhs=IN[:, kc0 + kc, tb * 512:(tb + 1) * 512],
                                start=(kc == 0), stop=(kc == KC - 1)),
                                waits=wready + (in_wait_kc[kc] if in_wait_kc else []) + [(s_free, epi.need_free(G))],
                                inc=s_mm if kc == KC - 1 else None)
                        epi.group(G, meta, tb, bank)
                        G += 1
            else:
                for tt in range(T // 128):
                    bank = cx.ps[G % 8]
                    for kc in range(KC):
                        P.op("pe", lambda e, bank=bank, b=b, kc=kc, tt=tt: e.matmul(
                            bank[:, 0:WCOLS], lhsT=IN[:, kc0 + kc, tt * 128:(tt + 1) * 128],
                            rhs=WT[b][:, kc, :], start=(kc == 0), stop=(kc == KC - 1)),
                            waits=wready + in_wait + [(s_free, epi.need_free(G))],
                            inc=s_mm if kc == KC - 1 else None)
                    epi.group(G, tl["jobs"][0], tt, bank)
                    G += 1
            tile_lastG.append(G)
        epi.finish()
        emit(nc, P)


class EpiResid:
    NS = 4

    def __init__(self, xT_ap, col0, gate_sb):
        self.xT, self.col0, self.gate = xT_ap, col0, gate_sb

    def setup(self, cx, es, P, s_mm, s_free):
        self.cx, self.P, self.s_mm, self.s_free = cx, P, s_mm, s_free
        self.XR = [cx.sb(es, "xr", [128, 512], F32) for _ in range(self.NS)]
        self.ld = [cx.sem(es, "xrld") for _ in range(self.NS)]
        self.st = [cx.sem(es, "xrst") for _ in range(self.NS)]

    def need_free(self, G):
        return G - 7

    def group(self, G, meta, tb, bank):
        P, s = self.P, G % self.NS
        r = G // self.NS
        chunk = meta
        c0 = self.col0 + tb * 512
        XR = self.XR[s]
        P.dma("sp", XR[:, :], self.xT[chunk, :, c0:c0 + 512], waits=[(self.st[s], 16 * r)], inc=self.ld[s])
        P.op("dve", lambda e: e.scalar_tensor_tensor(out=XR[:, :], in0=bank[:, :], scalar=self.gate[:, chunk:chunk + 1],
                                                     in1=XR[:, :], op0=ALU.mult, op1=ALU.add),
             waits=[(self.s_mm, G + 1), (self.ld[s], 16 * (r + 1))], inc=self.s_free)
        P.dma("act", self.xT[chunk, :, c0:c0 + 512], XR[:, :], waits=[(self.s_free, G + 1)], inc=self.st[s])

    def finish(self):
        self.P.op("dve", lambda e: e.memset(self.cx.scr[0:1, 0:1], 0.0), waits=[(s, s.n) for s in self.st])


class EpiSwiglu:
    NS = 2

    def __init__(self, aT_ap, comb_ap=None):
        self.aT, self.comb = aT_ap, comb_ap

    def setup(self, cx, es, P, s_mm, s_free):
        self.cx, self.P, self.s_mm, self.s_free = cx, P, s_mm, s_free
        self.SG = [cx.sb(es, "sg", [128, 512], F32) for _ in range(self.NS)]
        self.AO = [cx.sb(es, "ao", [128, 512], BF16) for _ in range(self.NS)]
        self.st = [cx.sem(es, "aost") for _ in range(self.NS)]
        self.s_sg = cx.sem(es, "sgs")
        self.s_d1 = cx.sem(es, "sd1")
        self.E = 0
        self.bank1 = {}
        self.cur_e = -1
        self.last_E_of = {}
        if self.comb is not None:
            self.CBT = [cx.sb(es, "cbt", [128, cx.cfg.T], F32) for _ in range(2)]
            self.cbld = [cx.sem(es, "cbld") for _ in range(2)]

    def need_free(self, G):
        Gp = G - 8
        if Gp < 0:
            return 0
        m, r = divmod(Gp, 8)
        return 4 * m + (r % 4) + 1

    def group(self, G, meta, tb, bank):
        which, fchunk, ex = meta
        if which == 0:
            self.bank1[tb] = bank
            return
        P = self.P
        E = self.E
        self.E += 1
        s = E % self.NS
        r = E // self.NS
        b1, b3 = self.bank1[tb], bank
        SG, AO = self.SG[s], self.AO[s]
        P.op("act", lambda e: e.activation(out=SG[:, :], in_=b1[:, :], func=AF.Silu),
             waits=[(self.s_mm, G + 1), (self.s_free, E - self.NS + 1)], inc=self.s_sg)
        if self.comb is None:
            P.op("dve", lambda e: e.tensor_tensor(out=AO[:, :], in0=SG[:, :], in1=b3[:, :], op=ALU.mult),
                 waits=[(self.s_sg, E + 1), (self.st[s], 16 * r)], inc=self.s_free)
        else:
            cb = ex % 2
            if ex != self.cur_e:
                P.dma("sp", self.CBT[cb][:, :], self.comb[ex, :, :],
                      waits=[(self.s_free, self.last_E_of.get(ex - 2, 0))], inc=self.cbld[cb])
                self.cur_e = ex
            CBT = self.CBT[cb]
            P.op("dve", lambda e: e.tensor_tensor(out=SG[:, :], in0=SG[:, :], in1=b3[:, :], op=ALU.mult),
                 waits=[(self.s_sg, E + 1), (self.st[s], 16 * r)], inc=self.s_d1)
            P.op("dve", lambda e: e.tensor_tensor(out=AO[:, :], in0=SG[:, :], in1=CBT[:, tb * 512:(tb + 1) * 512],
                                                  op=ALU.mult),
                 waits=[(self.cbld[cb], 16 * (ex // 2 + 1)), (self.s_d1, E + 1)], inc=self.s_free)
            self.last_E_of[ex] = E + 1
        P.dma("sp", self.aT[fchunk, :, tb * 512:(tb + 1) * 512], AO[:, :], waits=[(self.s_free, E + 1)],
              inc=self.st[s])

    def finish(self):
        self.P.op("dve", lambda e: e.memset(self.cx.scr[0:1, 0:1], 0.0), waits=[(s, s.n) for s in self.st])


class EpiRaw:
    NS = 4

    def __init__(self, dst_fn, dt, width=512):
        self.dst_fn, self.dt, self.width = dst_fn, dt, width

    def setup(self, cx, es, P, s_mm, s_free):
        self.cx, self.P, self.s_mm, self.s_free = cx, P, s_mm, s_free
        self.RO = [cx.sb(es, "ro", [128, self.width], self.dt) for _ in range(self.NS)]
        self.st = [cx.sem(es, "rost") for _ in range(self.NS)]

    def need_free(self, G):
        return G - 7

    def group(self, G, meta, tb, bank):
        P, s = self.P, G % self.NS
        r = G // self.NS
        RO = self.RO[s]
        w = self.width
        if G % 2 == 0:
            P.op("act", lambda e: e.copy(out=RO[:, :], in_=bank[:, 0:w]),
                 waits=[(self.s_mm, G + 1), (self.st[s], 16 * r), (self.s_free, G)], inc=self.s_free)
        else:
            P.op("dve", lambda e: e.tensor_copy(out=RO[:, :], in_=bank[:, 0:w]),
                 waits=[(self.s_mm, G + 1), (self.st[s], 16 * r), (self.s_free, G)], inc=self.s_free)
        P.dma("sp", self.dst_fn(meta, tb), RO[:, :], waits=[(self.s_free, G + 1)], inc=self.st[s])

    def finish(self):
        self.P.op("dve", lambda e: e.memset(self.cx.scr[0:1, 0:1], 0.0), waits=[(s, s.n) for s in self.st])


def phase_headnorm(cx, raw_ap, out_ap, nchunk, gvec_sb, ones_bf):
    nc, cfg = cx.nc, cx.cfg
    T = cfg.T
    TH = T // 2
    NB = TH // 512
    NCH = 4
    items = [(ci, hf) for ci in range(nchunk) for hf in range(2)]
    with ExitStack() as es:
        P = Prog()
        for cid in range(NCH):
            RW = cx.sb(es, "hraw", [128, TH], F32)
            SQ = cx.sb(es, "hsq", [128, TH], BF16)
            RS = cx.sb(es, "hrs", [128, TH], F32)
            QN = cx.sb(es, "hqn", [128, TH], BF16)
            cs, ds = cx.sem(es, "hc"), cx.sem(es, "hd")
            ch = Chain(P, cs, ds, cid)
            banks = cx.ps[NB * cid:NB * cid + NB]
            for (ci, hf) in items[cid::NCH]:
                ch.dma("sp", RW[:, :], raw_ap[ci, :, hf * TH:(hf + 1) * TH])
                ch.op("act", lambda e, SQ=SQ, RW=RW: e.activation(out=SQ[:, :], in_=RW[:, :], func=AF.Square))

                def mm(e, SQ=SQ, banks=banks):
                    ins = None
                    for tb in range(NB):
                        ins = e.matmul(banks[tb][:, :], lhsT=ones_bf[:, :], rhs=SQ[:, tb * 512:(tb + 1) * 512],
                                       start=True, stop=True)
                    return ins
                ch.op("pe", mm)

                def sq(e, RS=RS, banks=banks):
                    ins = None
                    for tb in range(NB):
                        ins = e.activation(out=RS[:, tb * 512:(tb + 1) * 512], in_=banks[tb][:, :], func=AF.Sqrt,
                                           bias=cx.epsE[:, 0:1], scale=1.0)
                    return ins
                ch.op("act", sq)
                ch.op("dve", lambda e, RS=RS: e.reciprocal(out=RS[:, :], in_=RS[:, :]))
                ch.op("dve", lambda e, QN=QN, RW=RW, RS=RS: e.scalar_tensor_tensor(
                    out=QN[:, :], in0=RW[:, :], scalar=gvec_sb[:, 0:1], in1=RS[:, :], op0=ALU.mult, op1=ALU.mult))
                ch.dma("sp", out_ap[ci, :, hf * TH:(hf + 1) * TH], QN[:, :])
            ch.op("dve", lambda e, RS=RS: e.memset(RS[0:1, 0:1], 0.0))
        emit(nc, P)


def _din(nc, name, shape, dt=F32):
    return nc.dram_tensor(name, list(shape), dt, kind="ExternalInput").ap()


def _dout(nc, name, shape, dt=F32):
    return nc.dram_tensor(name, list(shape), dt, kind="ExternalOutput").ap()


def _dint(nc, name, shape, dt=F32):
    return nc.dram_tensor(name, list(shape), dt, kind="Internal").ap()


def _derive_AB(ch, A, MOD, g_sb, KD, D):
    ch.op("dve", lambda e: e.scalar_tensor_tensor(out=A[:, 0:KD], in0=MOD[:, KD:2 * KD], scalar=1.0, in1=g_sb[:, 0:KD],
                                                  op0=ALU.add, op1=ALU.mult))
    ch.op("dve", lambda e: e.tensor_scalar(out=A[:, 0:KD], in0=A[:, 0:KD], scalar1=float(np.sqrt(D)), scalar2=None,
                                           op0=ALU.mult))


def build_l0(cfg, stage=99):
    nc = bass.Bass("TRN2", target_bir_lowering=False)
    D, KD, T, H, G, DFF = cfg.D, cfg.KD, cfg.T, cfg.H, cfg.G, cfg.DFF
    KG = KD // 4
    xin = _din(nc, "xin", [HALO + T, D])
    condT_d = _din(nc, "condT", [128, KD])
    adaw00 = _din(nc, "adaw00", [D, 3 * D])
    adab00 = _din(nc, "adab00T", [128, 3 * KD])
    adaw01 = _din(nc, "adaw01", [D, 3 * D])
    adab01 = _din(nc, "adab01T", [128, 3 * KD])
    kvadaw = _din(nc, "kvadaw", [D, 2 * D])
    kvadab = _din(nc, "kvadabT", [128, 2 * KD])
    gvecs_d = _din(nc, "gvecs", [128, 4, KD])
    kg_d = _din(nc, "kgT", [128, 1])
    poolw = _din(nc, "poolw", [4, G, G])
    w1 = _din(nc, "w1", [D, DFF])
    w3 = _din(nc, "w3", [D, DFF])
    w2 = _din(nc, "w2", [DFF, D])
    wk = _din(nc, "wk", [D, D])
    wv = _din(nc, "wv", [D, D])
    ident_d = _din(nc, "ident", [128, 128])
    onesbf_d = _din(nc, "onesbf", [128, 128], BF16)
    hmask_d = _din(nc, "hmask", [128, 2])
    invc_d = _din(nc, "invc", [128, 4, HALO])
    xT = _dout(nc, "xT", [KD, 128, HALO + T])
    knT = _dout(nc, "knT", [H, 128, T], BF16)
    v_o = _dout(nc, "v", [T, D], BF16)
    aT = _dint(nc, "aT", [DFF // 128, 128, T], BF16)
    kraw = _dint(nc, "kraw", [H, 128, T])

    with ExitStack() as es:
        cx = Ctx(nc, cfg, es)
        sb = lambda name, shape, dt=F32: es.enter_context(nc.sbuf_tensor("s_" + name, shape, dt))
        ident = sb("ident", [128, 128])
        onesbf = sb("onesbf", [128, 128], BF16)
        condS = sb("condS", [128, KD])
        b00, b01, bkv = sb("b00", [128, 3 * KD]), sb("b01", [128, 3 * KD]), sb("bkv", [128, 2 * KD])
        M00, M01, MKV = sb("M00", [128, 3 * KD]), sb("M01", [128, 3 * KD]), sb("MKV", [128, 2 * KD])
        gv = sb("gv", [128, 4, KD])
        kg = sb("kg", [128, 1])
        hmask = sb("hmask", [128, 2])
        invc = sb("invc", [128, 4, HALO])
        A00, A01, AKV, GT0 = sb("A00", [128, KD]), sb("A01", [128, KD]), sb("AKV", [128, KD]), sb("GT0", [128, KD])

        def post(ch):
            ch.op("dve", lambda e: e.memset(cx.epsD[:, :], float(D * EPS)))
            ch.op("dve", lambda e: e.memset(cx.epsE[:, :], float(128 * EPS)))
            ch.op("act", lambda e: e.activation(out=condS[:, :], in_=condS[:, :], func=AF.Silu))
            ch.op("dve", lambda e: e.tensor_scalar(out=kg[:, :], in0=kg[:, :], scalar1=float(np.sqrt(128.0)),
                                                   scalar2=None, op0=ALU.mult))
        phase_loads(cx, [(ident[:, :], ident_d), (onesbf[:, :], onesbf_d), (condS[:, :], condT_d),
                         (b00[:, :], adab00), (b01[:, :], adab01), (bkv[:, :], kvadab), (gv[:, :, :], gvecs_d),
                         (kg[:, :], kg_d), (hmask[:, :], hmask_d), (invc[:, :, :], invc_d)], post)
        phase_ada(cx, adaw00, b00, condS, M00, 3 * KD)
        phase_ada(cx, adaw01, b01, condS, M01, 3 * KD)
        phase_ada(cx, kvadaw, bkv, condS, MKV, 2 * KD)

        def derive(ch):
            _derive_AB(ch, A00, M00, gv[:, 0, :], KD, D)
            _derive_AB(ch, A01, M01, gv[:, 1, :], KD, D)
            _derive_AB(ch, AKV, MKV, gv[:, 2, :], KD, D)
            ch.op("dve", lambda e: e.tensor_tensor(out=GT0[:, :], in0=M00[:, 2 * KD:3 * KD], in1=gv[:, 3, :],
                                                   op=ALU.mult))
        phase_loads(cx, [], derive)
        phase_transpose_in(cx, xin, xT, ident, HALO + T)
        seg = ExitStack()
        cx.alloc_in(seg)
        if stage >= 1:
            phase_norm(cx, xT, HALO, A00, M00[:, 0:KD], onesbf, mode="pool", pool={"hmask": hmask, "invc": invc})
        if stage >= 2:
            for g in range(4):
                tiles = []
                for cb in range(G // 256):
                    tiles.append({"ap": poolw[g, :, cb * 256:(cb + 1) * 256],
                                  "jobs": [g * KG + cb * 2, g * KG + cb * 2 + 1]})
                phase_gemm(cx, f"pool{g}", KG, g * KG, tiles, EpiResid(xT, HALO, GT0))
        if stage >= 3:
            phase_norm(cx, xT, HALO, A01, M01[:, 0:KD], onesbf)
            tiles = []
            for j in range(DFF // 128):
                tiles.append({"ap": w1[:, j * 128:(j + 1) * 128], "jobs": [(0, j, 0)]})
                tiles.append({"ap": w3[:, j * 128:(j + 1) * 128], "jobs": [(1, j, 0)]})
            phase_gemm(cx, "ffn1", KD, 0, tiles, EpiSwiglu(aT), WCOLS=128)
        if stage >= 4:
            nfc = DFF // 128
            for c0 in range(0, nfc, 32):
                kc = min(32, nfc - c0)
                tiles = [{"ap": w2[c0 * 128:(c0 + kc) * 128, cb * 256:(cb + 1) * 256], "jobs": [cb * 2, cb * 2 + 1]}
                         for cb in range(D // 256)]
                phase_gemm(cx, "ffn2", kc, 0, tiles, EpiResid(xT, HALO, M01[:, 2 * KD:3 * KD]),
                           in_src=aT[c0:c0 + kc, :, :])
        if stage >= 5:
            phase_norm(cx, xT, HALO, AKV, MKV[:, 0:KD], onesbf)
            tiles = [{"ap": wk[:, cb * 256:(cb + 1) * 256], "jobs": [cb * 2, cb * 2 + 1]} for cb in range(D // 256)]
            phase_gemm(cx, "kproj", KD, 0, tiles,
                       EpiRaw(lambda ch_, tb: kraw[ch_, :, tb * 512:(tb + 1) * 512], F32, 512))
            phase_headnorm(cx, kraw, knT, H, kg, onesbf)
            tiles = [{"ap": wv[:, cb * 256:(cb + 1) * 256], "jobs": [cb]} for cb in range(D // 256)]
            phase_gemm(cx, "vproj", KD, 0, tiles,
                       EpiRaw(lambda cb, tt: v_o[tt * 128:(tt + 1) * 128, cb * 256:(cb + 1) * 256], BF16, 256),
                       mode="tm")
        seg.close()
    return nc


def _colT(vec, KD):
    return np.ascontiguousarray(np.asarray(vec, np.float32).reshape(KD, 128).T)


def prep_l0(inp, cfg, core):
    b, half = divmod(core, 2)
    D, KD, T = cfg.D, cfg.KD, cfg.T
    t0 = half * T
    x = inp["x"]
    xin = np.zeros((HALO + T, D), np.float32)
    xin[HALO:] = x[b, t0:t0 + T]
    if half == 1:
        xin[:HALO] = x[b, t0 - HALO:t0]
    hmask = np.full((128, 2), float(half), np.float32)
    invc = np.zeros((128, 4, HALO), np.float32)
    for g, k in enumerate((2, 4, 8, 16)):
        for t in range(HALO):
            invc[:, g, t] = (1.0 / min(t + 1, k)) if half == 0 else 1.0 / k
    gvecs = np.stack([_colT(inp["norm_g"][0, 0], KD), _colT(inp["norm_g"][0, 1], KD), _colT(inp["kv_norm_g"], KD),
                      _colT(inp["pool_scale"][0], KD)], axis=1)
    return {
        "xin": xin, "condT": _colT(inp["c"][b], KD),
        "adaw00": inp["ada_w"][0, 0], "adab00T": _colT(inp["ada_b"][0, 0], 3 * KD),
        "adaw01": inp["ada_w"][0, 1], "adab01T": _colT(inp["ada_b"][0, 1], 3 * KD),
        "kvadaw": inp["kv_ada_w"], "kvadabT": _colT(inp["kv_ada_b"], 2 * KD),
        "gvecs": np.ascontiguousarray(gvecs), "kgT": np.asarray(inp["k_norm_g"], np.float32).reshape(128, 1),
        "poolw": inp["pool_w"][0], "w1": inp["ffn_w1"][0], "w3": inp["ffn_w3"][0], "w2": inp["ffn_w2"][0],
        "wk": inp["w_k"], "wv": inp["w_v"],
        "ident": np.eye(128, dtype=np.float32), "onesbf": np.ones((128, 128), ml_dtypes.bfloat16),
        "hmask": hmask, "invc": invc,
    }


def phase_bias_setup(cx, rb_d, oh_d, gq_row_d, gk_row_d, rbrow_d, extrep, NEGC, ones32):
    nc, cfg = cx.nc, cx.cfg
    H = cfg.H
    with ExitStack() as es:
        RB = cx.sb(es, "rb", [32, 3, H], F32)
        OH = cx.sb(es, "oh", [32, 3, 129], F32)
        EXT = cx.sb(es, "ext", [H, 3, 384], F32)
        ROW = cx.sb(es, "row", [1, 3 * 32 * H + 256], F32)
        SC = cx.sb(es, "sc", [1, 16], F32)
        cs, ds = cx.sem(es, "bc"), cx.sem(es, "bd")
        P = Prog()
        NR = 3 * 32 * H
        P.dma("sp", RB[:, :, :], rb_d, inc=ds)
        P.dma("sp", OH[:, :, :], oh_d, inc=ds)
        P.dma("sp", ROW[0:1, 0:NR], rbrow_d, inc=ds)
        P.dma("sp", ROW[0:1, NR:NR + 128], gq_row_d, inc=ds)
        P.dma("sp", ROW[0:1, NR + 128:NR + 256], gk_row_d, inc=ds)
        ch = Chain(P, cs, ds)
        ch.last = (ds, ds.n)
        ch.op("dve", lambda e: e.memset(EXT[:, :, :], NEG))

        def mm(e):
            ins = None
            for g in range(3):
                ins = e.matmul(cx.ps[g][0:H, 0:129], lhsT=RB[:, g, :], rhs=OH[:, g, :], start=True, stop=True)
            return ins
        ch.op("pe", mm)

        def cp(e):
            ins = None
            for g in range(3):
                ins = e.tensor_copy(out=EXT[:, g, 127:256], in_=cx.ps[g][0:H, 0:129])
            return ins
        ch.op("dve", cp)
        for g in range(3):
            ch.dma("sp", extrep[g], EXT[:, g, :].unsqueeze(1).broadcast_to([H, 128, 384]))
        ch.op("dve", lambda e: e.tensor_tensor(out=ROW[0:1, :], in0=ROW[0:1, :], in1=ROW[0:1, :], op=ALU.mult))

        def red(e):
            e.tensor_reduce(out=SC[0:1, 0:1], in_=ROW[0:1, NR:NR + 128], axis=mybir.AxisListType.X, op=ALU.max)
            e.tensor_reduce(out=SC[0:1, 1:2], in_=ROW[0:1, NR + 128:NR + 256], axis=mybir.AxisListType.X, op=ALU.max)
            return e.tensor_reduce(out=SC[0:1, 2:3], in_=ROW[0:1, 0:NR], axis=mybir.AxisListType.X, op=ALU.max)
        ch.op("dve", red)
        ch.op("dve", lambda e: e.tensor_tensor(out=SC[0:1, 3:4], in0=SC[0:1, 0:1], in1=SC[0:1, 1:2], op=ALU.mult))
        ch.op("act", lambda e: e.activation(out=SC[0:1, 4:6], in_=SC[0:1, 2:4], func=AF.Sqrt))
        ch.op("dve", lambda e: e.tensor_scalar(out=SC[0:1, 6:7], in0=SC[0:1, 5:6], scalar1=float(-np.sqrt(128.0) * 1.001),
                                               scalar2=None, op0=ALU.mult))
        ch.op("dve", lambda e: e.tensor_tensor(out=SC[0:1, 7:8], in0=SC[0:1, 6:7], in1=SC[0:1, 4:5], op=ALU.subtract))
        ch.op("pe", lambda e: e.matmul(cx.ps[4][:, 0:1], lhsT=ones32[0:1, :], rhs=SC[0:1, 7:8], start=True, stop=True))
        ch.op("dve", lambda e: e.tensor_copy(out=NEGC[:, 0:1], in_=cx.ps[4][:, 0:1]))
        emit(nc, P)


def phase_attention(cx, qnT, kn_prev, kn_own, v_prev, v_own, extrep, oT, NEGC, cmask, onesbf):
    nc, cfg = cx.nc, cx.cfg
    T, H, D = cfg.T, cfg.H, cfg.D
    NBW = 2 * T // 128
    NCH = 2
    ext_t = extrep.tensor
    with ExitStack() as es:
        P = Prog()
        for cid in range(NCH):
            QNb = cx.sb(es, "aq", [128, T], BF16)
            KW = cx.sb(es, "akw", [128, 2 * T], BF16)
            VTb = cx.sb(es, "avt", [128, NBW * 128], BF16)
            BA = [cx.sb(es, "aba", [128, 4, 256], F32) for _ in range(3)]
            BB = [cx.sb(es, "abb", [128, 4, 256], F32) for _ in range(3)]
            BM = cx.sb(es, "abm", [128, 4, 256], F32)
            TT = cx.sb(es, "att", [128, 4, 256], F32)
            PT = cx.sb(es, "apt", [128, 4, 256], BF16)
            ACC = cx.sb(es, "aacc", [128, 2, T], F32)
            OT = cx.sb(es, "aot", [128, T], BF16)
            cs, ds = cx.sem(es, "ac"), cx.sem(es, "ad")
            ch = Chain(P, cs, ds, cid)
            pb = cx.ps[4 * cid:4 * cid + 4]
            for h in range(cid, H, NCH):
                for g in range(3):
                    off = ((g * H + h) * 128) * 384 + 127
                    src_ = bass.AP(tensor=ext_t, offset=off, ap=[[383, 128], [0, 4], [1, 256]])
                    ch.dma("sp", BA[g][:, :, :], src_)
                ch.dma("sp", KW[:, 0:T], kn_prev[h, :, :])
                ch.dma("sp", KW[:, T:2 * T], kn_own[h, :, :])

                def mkb(e, BA=BA, BB=BB, BM=BM):
                    ins = None
                    for g in range(3):
                        e.tensor_copy(out=BB[g][:, :, 0:128], in_=BA[g][:, :, 0:128])
                        ins = e.tensor_scalar(out=BB[g][:, :, 128:256], in0=BA[g][:, :, 128:256],
                                              scalar1=cmask[:, 0:1], scalar2=None, op0=ALU.add)
                    e.tensor_copy(out=BM[:, 1:4, :], in_=BA[0][:, 1:4, :])
                    return ins
                ch.op("dve", mkb)
                ch.op("dve", lambda e, BM=BM, BB=BB: e.tensor_copy(out=BM[:, 0:1, :], in_=BB[0][:, 0:1, :]))
                for g, (_, d) in enumerate(PATTERNS):
                    nbh = T // (128 * d)
                    VT = VTb[:, :].rearrange("p (s r nb e) -> p s r nb e", s=2, r=d, nb=nbh)
                    ch.dma("sp", QNb[:, :], qnT[g * H + h, :, :])
                    ch.dma("sp", VT[:, 0, :, :, :],
                           v_prev[:, h * 128:(h + 1) * 128].rearrange("(nb j r) e -> j r nb e", j=128, r=d))
                    ch.dma("sp", VT[:, 1, :, :, :],
                           v_own[:, h * 128:(h + 1) * 128].rearrange("(nb j r) e -> j r nb e", j=128, r=d))
                    units = [(r, nl) for nl in range(nbh) for r in range(d)]
                    qv = QNb[:, :].rearrange("p (m r) -> p r m", r=d)
                    kv = KW[:, :].rearrange("p (m r) -> p r m", r=d)
                    av = ACC[:, :, :].rearrange("p a (m r) -> p a r m", r=d)
                    for b0 in range(0, len(units), 4):
                        ub = units[b0:b0 + 4]
                        if all(nl == 0 for (_, nl) in ub):
                            bias = BB[g]
                        elif any(nl == 0 for (_, nl) in ub):
                            assert g == 0 and b0 == 0
                            bias = BM
                        else:
                            bias = BA[g]

                        def s1(e, ub=ub, qv=qv, kv=kv, d=d, pb=pb):
                            ins = None
                            for u, (r, nl) in enumerate(ub):
                                m0 = T // d + 128 * nl
                                for blk in range(2):
                                    c0 = (u % 2) * 256 + blk * 128
                                    ins = e.matmul(pb[u // 2][:, c0:c0 + 128],
                                                   lhsT=kv[:, r, m0 - blk * 128:m0 - blk * 128 + 128],
                                                   rhs=qv[:, r, 128 * nl:128 * nl + 128], start=True, stop=True)
                            return ins
                        ch.op("pe", s1)

                        def s2(e, bias=bias, pb=pb, TT=TT):
                            ins = None
                            for bk in range(2):
                                ins = e.tensor_tensor(out=TT[:, 2 * bk:2 * bk + 2, :],
                                                      in0=pb[bk][:, :].rearrange("p (u c) -> p u c", u=2),
                                                      in1=bias[:, 2 * bk:2 * bk + 2, :], op=ALU.add)
                            return ins
                        ch.op("dve", s2)
                        ch.op("act", lambda e, PT=PT, TT=TT: e.activation(out=PT[:, :, :], in_=TT[:, :, :], func=AF.Exp,
                                                                          bias=NEGC[:, 0:1], scale=1.0))

                        def s4(e, ub=ub, VT=VT, nbh=nbh, pb=pb, PT=PT):
                            ins = None
                            for u, (r, nl) in enumerate(ub):
                                nbc = nbh + nl
                                ob = pb[2 + u // 2]
                                o0 = (u % 2) * 256
                                for blk in range(2):
                                    nbw = nbc - blk
                                    e.matmul(ob[:, o0:o0 + 128], lhsT=VT[:, nbw // nbh, r, nbw % nbh, :],
                                             rhs=PT[:, u, blk * 128:(blk + 1) * 128], start=(blk == 0), stop=(blk == 1))
                                for blk in range(2):
                                    ins = e.matmul(ob[:, o0 + 128:o0 + 256], lhsT=onesbf[:, :],
                                                   rhs=PT[:, u, blk * 128:(blk + 1) * 128],
                                                   start=(blk == 0), stop=(blk == 1))
                            return ins
                        ch.op("pe", s4)

                        def s5(e, ub=ub, g=g, av=av, pb=pb):
                            ins = None
                            for u, (r, nl) in enumerate(ub):
                                sr = pb[2 + u // 2][:, (u % 2) * 256:(u % 2) * 256 + 256].rearrange(
                                    "p (a q) -> p a q", a=2)
                                dst = av[:, :, r, 128 * nl:128 * nl + 128]
                                if g == 0:
                                    ins = e.tensor_copy(out=dst, in_=sr)
                                else:
                                    ins = e.tensor_tensor(out=dst, in0=dst, in1=sr, op=ALU.add)
                            return ins
                        ch.op("dve", s5)
                ch.op("dve", lambda e, ACC=ACC: e.reciprocal(out=ACC[:, 1, :], in_=ACC[:, 1, :]))
                ch.op("dve", lambda e, OT=OT, ACC=ACC: e.tensor_tensor(out=OT[:, :], in0=ACC[:, 0, :], in1=ACC[:, 1, :],
                                                                       op=ALU.mult))
                ch.dma("sp", oT[h, :, :], OT[:, :])
            ch.op("dve", lambda e, TT=TT: e.memset(TT[0:1, 0, 0:1], 0.0))
        emit(nc, P)


def build_l1(cfg, stage=99):
    nc = bass.Bass("TRN2", target_bir_lowering=False)
    D, KD, T, H, NE, DFE = cfg.D, cfg.KD, cfg.T, cfg.H, cfg.NE, cfg.DFE
    xT_in = _din(nc, "xT_in", [KD, 128, T])
    knTw = _din(nc, "knTw", [H, 128, 2 * T], BF16)
    vw = _din(nc, "vw", [2 * T, D], BF16)
    condT_d = _din(nc, "condT", [128, KD])
    adaw10 = _din(nc, "adaw10", [D, 3 * D])
    adab10 = _din(nc, "adab10T", [128, 3 * KD])
    adaw11 = _din(nc, "adaw11", [D, 3 * D])
    adab11 = _din(nc, "adab11T", [128, 3 * KD])
    gvecs_d = _din(nc, "gvecs", [128, 2, KD])
    qg_d = _din(nc, "qgT", [128, 1])
    wq = _din(nc, "wq", [D, 3 * D])
    wo = _din(nc, "wo", [D, D])
    rb_d = _din(nc, "relb", [32, 3, H])
    rbrow_d = _din(nc, "relbrow", [1, 3 * 32 * H])
    gqrow_d = _din(nc, "gqrow", [1, 128])
    gkrow_d = _din(nc, "gkrow", [1, 128])
    oh_d = _din(nc, "oh", [32, 3, 129])
    cmask_d = _din(nc, "cmask", [128, 1])
    rw_d = _din(nc, "routerw", [D, NE])
    mw1 = _din(nc, "mw1", [NE, D, DFE])
    mw3 = _din(nc, "mw3", [NE, D, DFE])
    mw2 = _din(nc, "mw2", [NE * DFE, D])
    ident_d = _din(nc, "ident", [128, 128])
    ones32_d = _din(nc, "ones32", [128, 128])
    onesbf_d = _din(nc, "onesbf", [128, 128], BF16)
    out = _dout(nc, "out", [T, D])
    xT = _dint(nc, "xT2", [KD, 128, T])
    qraw = _dint(nc, "qraw", [3 * H, 128, T])
    qnT = _dint(nc, "qnT", [3 * H, 128, T], BF16)
    oT = _dint(nc, "oT", [H, 128, T], BF16)
    extrep = _dint(nc, "extrep", [3, H, 128, 384])
    comb = _dint(nc, "comb", [NE, 128, T])
    NFC = NE * DFE // 128
    aT = _dint(nc, "aT2", [NFC, 128, T], BF16)

    with ExitStack() as es:
        cx = Ctx(nc, cfg, es)
        sb = lambda name, shape, dt=F32: es.enter_context(nc.sbuf_tensor("s_" + name, shape, dt))
        ident = sb("ident", [128, 128])
        ones32 = sb("ones32", [128, 128])
        onesbf = sb("onesbf", [128, 128], BF16)
        condS = sb("condS", [128, KD])
        b10, b11 = sb("b10", [128, 3 * KD]), sb("b11", [128, 3 * KD])
        M10, M11 = sb("M10", [128, 3 * KD]), sb("M11", [128, 3 * KD])
        gv = sb("gv", [128, 2, KD])
        qg = sb("qg", [128, 1])
        cmask = sb("cmask", [128, 1])
        NEGC = sb("NEGC", [128, 1])
        rw = sb("rw", [128, KD, NE])
        A10, A11 = sb("A10", [128, KD]), sb("A11", [128, KD])

        def post(ch):
            ch.op("dve", lambda e: e.memset(cx.epsD[:, :], float(D * EPS)))
            ch.op("dve", lambda e: e.memset(cx.epsE[:, :], float(128 * EPS)))
            ch.op("act", lambda e: e.activation(out=condS[:, :], in_=condS[:, :], func=AF.Silu))
        loads = [(ident[:, :], ident_d), (ones32[:, :], ones32_d), (onesbf[:, :], onesbf_d), (condS[:, :], condT_d),
                 (b10[:, :], adab10), (b11[:, :], adab11), (gv[:, :, :], gvecs_d), (qg[:, :], qg_d),
                 (cmask[:, :], cmask_d), (rw[:, :, :], rw_d.rearrange("(c p) e -> p c e", p=128))]
        loads += [(xT[c, :, :], xT_in[c, :, :]) for c in range(KD)]
        phase_loads(cx, loads, post)
        phase_ada(cx, adaw10, b10, condS, M10, 3 * KD)
        phase_ada(cx, adaw11, b11, condS, M11, 3 * KD)

        def derive(ch):
            _derive_AB(ch, A10, M10, gv[:, 0, :], KD, D)
            _derive_AB(ch, A11, M11, gv[:, 1, :], KD, D)
        phase_loads(cx, [], derive)
        if stage >= 1:
            with ExitStack() as seg:
                cx.alloc_in(seg)
                phase_norm(cx, xT, 0, A10, M10[:, 0:KD], onesbf)
                tiles = [{"ap": wq[:, cb * 256:(cb + 1) * 256], "jobs": [cb * 2, cb * 2 + 1]}
                         for cb in range(3 * D // 256)]
                phase_gemm(cx, "qproj", KD, 0, tiles,
                           EpiRaw(lambda ch_, tb: qraw[ch_, :, tb * 512:(tb + 1) * 512], F32, 512))
            phase_headnorm(cx, qraw, qnT, 3 * H, qg, onesbf)
        if stage >= 2:
            phase_bias_setup(cx, rb_d, oh_d, gqrow_d, gkrow_d, rbrow_d, extrep, NEGC, ones32)
            phase_attention(cx, qnT, knTw[:, :, 0:T], knTw[:, :, T:2 * T], vw[0:T, :], vw[T:2 * T, :], extrep, oT,
                            NEGC, cmask, onesbf)
        seg2 = ExitStack()
        cx.alloc_in(seg2)
        if stage >= 3:
            tiles = [{"ap": wo[:, cb * 256:(cb + 1) * 256], "jobs": [cb * 2, cb * 2 + 1]} for cb in range(D // 256)]
            phase_gemm(cx, "oproj", KD, 0, tiles, EpiResid(xT, 0, M10[:, 2 * KD:3 * KD]), in_src=oT)
        if stage >= 4:
            phase_norm(cx, xT, 0, A11, M11[:, 0:KD], onesbf, mode="router",
                       router={"rw": rw, "ident": ident, "ones32": ones32, "comb": comb})
            tiles = []
            for ex in range(NE):
                for j in range(DFE // 128):
                    fc = ex * (DFE // 128) + j
                    tiles.append({"ap": mw1[ex, :, j * 128:(j + 1) * 128], "jobs": [(0, fc, ex)]})
                    tiles.append({"ap": mw3[ex, :, j * 128:(j + 1) * 128], "jobs": [(1, fc, ex)]})
            phase_gemm(cx, "moe1", KD, 0, tiles, EpiSwiglu(aT, comb), WCOLS=128)
            for c0 in range(0, NFC, 32):
                kc = min(32, NFC - c0)
                tiles = [{"ap": mw2[c0 * 128:(c0 + kc) * 128, cb * 256:(cb + 1) * 256], "jobs": [cb * 2, cb * 2 + 1]}
                         for cb in range(D // 256)]
                phase_gemm(cx, "moe2", kc, 0, tiles, EpiResid(xT, 0, M11[:, 2 * KD:3 * KD]),
                           in_src=aT[c0:c0 + kc, :, :])
        seg2.close()
        phase_transpose_out(cx, xT, 0, out, ident)
    return nc


def _t5_bucket_np(n):
    n = np.asarray(n, np.int64)
    max_exact = 16
    nf = np.maximum(n, 1).astype(np.float32)
    large = max_exact + (np.log(nf / np.float32(max_exact)) / np.float32(np.log(2048 / max_exact))
                         * np.float32(32 - max_exact)).astype(np.int32)
    return np.where(n < max_exact, n, np.minimum(large, 31))


def prep_l1(inp, cfg, core, xT1, knT_all, v_all):
    b, half = divmod(core, 2)
    D, KD, T, H = cfg.D, cfg.KD, cfg.T, cfg.H
    knTw = np.zeros((H, 128, 2 * T), ml_dtypes.bfloat16)
    vw = np.zeros((2 * T, D), ml_dtypes.bfloat16)
    knTw[:, :, T:] = knT_all[core]
    vw[T:] = v_all[core]
    if half == 1:
        knTw[:, :, :T] = knT_all[core - 1]
        vw[:T] = v_all[core - 1]
    oh = np.zeros((32, 3, 129), np.float32)
    for g, (_, d) in enumerate(PATTERNS):
        bk = _t5_bucket_np(np.arange(129) * d)
        oh[bk, g, np.arange(129)] = 1.0
    gvecs = np.stack([_colT(inp["norm_g"][1, 0], KD), _colT(inp["norm_g"][1, 1], KD)], axis=1)
    cm = np.full((128, 1), NEG if half == 0 else 0.0, np.float32)
    NE, DFE = cfg.NE, cfg.DFE
    return {
        "xT_in": xT1, "knTw": knTw, "vw": vw, "condT": _colT(inp["c"][b], KD),
        "adaw10": inp["ada_w"][1, 0], "adab10T": _colT(inp["ada_b"][1, 0], 3 * KD),
        "adaw11": inp["ada_w"][1, 1], "adab11T": _colT(inp["ada_b"][1, 1], 3 * KD),
        "gvecs": np.ascontiguousarray(gvecs), "qgT": np.asarray(inp["q_norm_g"][0], np.float32).reshape(128, 1),
        "wq": inp["w_q"][0], "wo": inp["w_o"][0], "relb": np.ascontiguousarray(inp["rel_bias"]),
        "relbrow": np.ascontiguousarray(inp["rel_bias"]).reshape(1, -1),
        "gqrow": np.asarray(inp["q_norm_g"][0], np.float32).reshape(1, 128),
        "gkrow": np.asarray(inp["k_norm_g"], np.float32).reshape(1, 128),
        "oh": oh, "cmask": cm, "routerw": inp["router_w"][0],
        "mw1": inp["moe_w1"][0], "mw3": inp["moe_w3"][0], "mw2": inp["moe_w2"][0].reshape(NE * DFE, D),
        "ident": np.eye(128, dtype=np.float32), "ones32": np.ones((128, 128), np.float32),
        "onesbf": np.ones((128, 128), ml_dtypes.bfloat16),
    }


def _l0_pass(cx, cfg, tag, xin, xT, knT, v_o, aT, kraw, w, sbt):
    nc = cx.nc
    D, KD, T, H, G, DFF = cfg.D, cfg.KD, cfg.T, cfg.H, cfg.G, cfg.DFF
    KG = KD // 4
    phase_transpose_in(cx, xin, xT, sbt["ident"], HALO + T)
    with ExitStack() as seg:
        cx.alloc_in(seg)
        phase_norm(cx, xT, HALO, sbt["A00"], sbt["M00"][:, 0:KD], sbt["onesbf"], mode="pool",
                   pool={"hmask": sbt["hmask" + tag], "invc": sbt["invc" + tag]})
        for g in range(4):
            tiles = []
            for cb in range(G // 256):
                tiles.append({"ap": w["poolw"][g, :, cb * 256:(cb + 1) * 256],
                              "jobs": [g * KG + cb * 2, g * KG + cb * 2 + 1]})
            phase_gemm(cx, f"pool{g}", KG, g * KG, tiles, EpiResid(xT, HALO, sbt["GT0"]))
        phase_norm(cx, xT, HALO, sbt["A01"], sbt["M01"][:, 0:KD], sbt["onesbf"])
        tiles = []
        for j in range(DFF // 128):
            tiles.append({"ap": w["w1"][:, j * 128:(j + 1) * 128], "jobs": [(0, j, 0)]})
            tiles.append({"ap": w["w3"][:, j * 128:(j + 1) * 128], "jobs": [(1, j, 0)]})
        phase_gemm(cx, "ffn1", KD, 0, tiles, EpiSwiglu(aT), WCOLS=128)
        nfc = DFF // 128
        for c0 in range(0, nfc, 32):
            kc = min(32, nfc - c0)
            tiles = [{"ap": w["w2"][c0 * 128:(c0 + kc) * 128, cb * 256:(cb + 1) * 256], "jobs": [cb * 2, cb * 2 + 1]}
                     for cb in range(D // 256)]
            phase_gemm(cx, "ffn2", kc, 0, tiles, EpiResid(xT, HALO, sbt["M01"][:, 2 * KD:3 * KD]),
                       in_src=aT[c0:c0 + kc, :, :])
        phase_norm(cx, xT, HALO, sbt["AKV"], sbt["MKV"][:, 0:KD], sbt["onesbf"])
        tiles = [{"ap": w["wk"][:, cb * 256:(cb + 1) * 256], "jobs": [cb * 2, cb * 2 + 1]} for cb in range(D // 256)]
        phase_gemm(cx, "kproj", KD, 0, tiles,
                   EpiRaw(lambda ch_, tb: kraw[ch_, :, tb * 512:(tb + 1) * 512], F32, 512))
        phase_headnorm(cx, kraw, knT, H, sbt["kg"], sbt["onesbf"])
        tiles = [{"ap": w["wv"][:, cb * 256:(cb + 1) * 256], "jobs": [cb]} for cb in range(D // 256)]
        phase_gemm(cx, "vproj", KD, 0, tiles,
                   EpiRaw(lambda cb, tt: v_o[tt * 128:(tt + 1) * 128, cb * 256:(cb + 1) * 256], BF16, 256),
                   mode="tm")


def build_fused(cfg):
    nc = bass.Bass("TRN2", target_bir_lowering=False)
    D, KD, T, H, G, DFF, NE, DFE = cfg.D, cfg.KD, cfg.T, cfg.H, cfg.G, cfg.DFF, cfg.NE, cfg.DFE
    xinA = _din(nc, "xinA", [HALO + T, D])
    xinB = _din(nc, "xinB", [HALO + T, D])
    condT_d = _din(nc, "condT", [128, KD])
    adaw = {k: _din(nc, "adaw" + k, [D, 3 * D]) for k in ("00", "01", "10", "11")}
    adab = {k: _din(nc, "adab" + k + "T", [128, 3 * KD]) for k in ("00", "01", "10", "11")}
    kvadaw = _din(nc, "kvadaw", [D, 2 * D])
    kvadab = _din(nc, "kvadabT", [128, 2 * KD])
    gvecs_d = _din(nc, "gvecs", [128, 6, KD])
    kg_d = _din(nc, "kgT", [128, 1])
    qg_d = _din(nc, "qgT", [128, 1])
    w = {"poolw": _din(nc, "poolw", [4, G, G]), "w1": _din(nc, "w1", [D, DFF]), "w3": _din(nc, "w3", [D, DFF]),
         "w2": _din(nc, "w2", [DFF, D]), "wk": _din(nc, "wk", [D, D]), "wv": _din(nc, "wv", [D, D])}
    wq = _din(nc, "wq", [D, 3 * D])
    wo = _din(nc, "wo", [D, D])
    rb_d = _din(nc, "relb", [32, 3, H])
    rbrow_d = _din(nc, "relbrow", [1, 3 * 32 * H])
    gqrow_d = _din(nc, "gqrow", [1, 128])
    gkrow_d = _din(nc, "gkrow", [1, 128])
    oh_d = _din(nc, "oh", [32, 3, 129])
    cmask_d = _din(nc, "cmask", [128, 1])
    rw_d = _din(nc, "routerw", [D, NE])
    mw1 = _din(nc, "mw1", [NE, D, DFE])
    mw3 = _din(nc, "mw3", [NE, D, DFE])
    mw2 = _din(nc, "mw2", [NE * DFE, D])
    ident_d = _din(nc, "ident", [128, 128])
    ones32_d = _din(nc, "ones32", [128, 128])
    onesbf_d = _din(nc, "onesbf", [128, 128], BF16)
    hmA_d, hmB_d = _din(nc, "hmaskA", [128, 2]), _din(nc, "hmaskB", [128, 2])
    icA_d, icB_d = _din(nc, "invcA", [128, 4, HALO]), _din(nc, "invcB", [128, 4, HALO])
    out = _dout(nc, "out", [T, D])
    xTA = _dint(nc, "xTA", [KD, 128, HALO + T])
    xTB = _dint(nc, "xTB", [KD, 128, HALO + T])
    knA, knB = _dint(nc, "knA", [H, 128, T], BF16), _dint(nc, "knB", [H, 128, T], BF16)
    vA, vB = _dint(nc, "vA", [T, D], BF16), _dint(nc, "vB", [T, D], BF16)
    NFC = NE * DFE // 128
    aT = _dint(nc, "aT", [max(DFF // 128, NFC), 128, T], BF16)
    kraw = _dint(nc, "qkraw", [3 * H, 128, T])
    qnT = _dint(nc, "qnT", [3 * H, 128, T], BF16)
    oT = _dint(nc, "oT", [H, 128, T], BF16)
    extrep = _dint(nc, "extrep", [3, H, 128, 384])
    comb = _dint(nc, "comb", [NE, 128, T])

    with ExitStack() as es:
        cx = Ctx(nc, cfg, es)
        sb = lambda name, shape, dt=F32: es.enter_context(nc.sbuf_tensor("s_" + name, shape, dt))
        t = {}
        t["ident"], t["ones32"] = sb("ident", [128, 128]), sb("ones32", [128, 128])
        t["onesbf"] = sb("onesbf", [128, 128], BF16)
        condS = sb("condS", [128, KD])
        bT = {k: sb("b" + k, [128, 3 * KD]) for k in ("00", "01", "10", "11")}
        bkv = sb("bkv", [128, 2 * KD])
        for k in ("00", "01", "10", "11"):
            t["M" + k] = sb("M" + k, [128, 3 * KD])
            t["A" + k] = sb("A" + k, [128, KD])
        t["MKV"], t["AKV"], t["GT0"] = sb("MKV", [128, 2 * KD]), sb("AKV", [128, KD]), sb("GT0", [128, KD])
        gv = sb("gv", [128, 6, KD])
        t["kg"], qg = sb("kg", [128, 1]), sb("qg", [128, 1])
        t["hmaskA"], t["hmaskB"] = sb("hmaskA", [128, 2]), sb("hmaskB", [128, 2])
        t["invcA"], t["invcB"] = sb("invcA", [128, 4, HALO]), sb("invcB", [128, 4, HALO])
        cmask, NEGC = sb("cmask", [128, 1]), sb("NEGC", [128, 1])
        rw = sb("rw", [128, KD, NE])

        def post(ch):
            ch.op("dve", lambda e: e.memset(cx.epsD[:, :], float(D * EPS)))
            ch.op("dve", lambda e: e.memset(cx.epsE[:, :], float(128 * EPS)))
            ch.op("act", lambda e: e.activation(out=condS[:, :], in_=condS[:, :], func=AF.Silu))
            ch.op("dve", lambda e: e.tensor_scalar(out=t["kg"][:, :], in0=t["kg"][:, :], scalar1=float(np.sqrt(128.0)),
                                                   scalar2=None, op0=ALU.mult))
        loads = [(t["ident"][:, :], ident_d), (t["ones32"][:, :], ones32_d), (t["onesbf"][:, :], onesbf_d),
                 (condS[:, :], condT_d), (bkv[:, :], kvadab), (gv[:, :, :], gvecs_d), (t["kg"][:, :], kg_d),
                 (qg[:, :], qg_d), (t["hmaskA"][:, :], hmA_d), (t["hmaskB"][:, :], hmB_d),
                 (t["invcA"][:, :, :], icA_d), (t["invcB"][:, :, :], icB_d), (cmask[:, :], cmask_d),
                 (rw[:, :, :], rw_d.rearrange("(c p) e -> p c e", p=128))]
        loads += [(bT[k][:, :], adab[k]) for k in ("00", "01", "10", "11")]
        phase_loads(cx, loads, post)
        for k in ("00", "01", "10", "11"):
            phase_ada(cx, adaw[k], bT[k], condS, t["M" + k], 3 * KD)
        phase_ada(cx, kvadaw, bkv, condS, t["MKV"], 2 * KD)

        def derive(ch):
            _derive_AB(ch, t["A00"], t["M00"], gv[:, 0, :], KD, D)
            _derive_AB(ch, t["A01"], t["M01"], gv[:, 1, :], KD, D)
            _derive_AB(ch, t["AKV"], t["MKV"], gv[:, 2, :], KD, D)
            _derive_AB(ch, t["A10"], t["M10"], gv[:, 4, :], KD, D)
            _derive_AB(ch, t["A11"], t["M11"], gv[:, 5, :], KD, D)
            ch.op("dve", lambda e: e.tensor_tensor(out=t["GT0"][:, :], in0=t["M00"][:, 2 * KD:3 * KD], in1=gv[:, 3, :],
                                                   op=ALU.mult))
        phase_loads(cx, [], derive)
        _l0_pass(cx, cfg, "A", xinA, xTA, knA, vA, aT, kraw, w, t)
        _l0_pass(cx, cfg, "B", xinB, xTB, knB, vB, aT, kraw, w, t)
        xT = xTB
        with ExitStack() as seg:
            cx.alloc_in(seg)
            phase_norm(cx, xT, HALO, t["A10"], t["M10"][:, 0:KD], t["onesbf"])
            tiles = [{"ap": wq[:, cb * 256:(cb + 1) * 256], "jobs": [cb * 2, cb * 2 + 1]} for cb in range(3 * D // 256)]
            phase_gemm(cx, "qproj", KD, 0, tiles,
                       EpiRaw(lambda ch_, tb: kraw[ch_, :, tb * 512:(tb + 1) * 512], F32, 512))
        phase_headnorm(cx, kraw, qnT, 3 * H, qg, t["onesbf"])
        phase_bias_setup(cx, rb_d, oh_d, gqrow_d, gkrow_d, rbrow_d, extrep, NEGC, t["ones32"])
        phase_attention(cx, qnT, knA, knB, vA, vB, extrep, oT, NEGC, cmask, t["onesbf"])
        with ExitStack() as seg:
            cx.alloc_in(seg)
            tiles = [{"ap": wo[:, cb * 256:(cb + 1) * 256], "jobs": [cb * 2, cb * 2 + 1]} for cb in range(D // 256)]
            phase_gemm(cx, "oproj", KD, 0, tiles, EpiResid(xT, HALO, t["M10"][:, 2 * KD:3 * KD]), in_src=oT)
            phase_norm(cx, xT, HALO, t["A11"], t["M11"][:, 0:KD], t["onesbf"], mode="router",
                       router={"rw": rw, "ident": t["ident"], "ones32": t["ones32"], "comb": comb})
            tiles = []
            for ex in range(NE):
                for j in range(DFE // 128):
                    fc = ex * (DFE // 128) + j
                    tiles.append({"ap": mw1[ex, :, j * 128:(j + 1) * 128], "jobs": [(0, fc, ex)]})
                    tiles.append({"ap": mw3[ex, :, j * 128:(j + 1) * 128], "jobs": [(1, fc, ex)]})
            phase_gemm(cx, "moe1", KD, 0, tiles, EpiSwiglu(aT, comb), WCOLS=128)
            for c0 in range(0, NFC, 32):
                kc = min(32, NFC - c0)
                tiles = [{"ap": mw2[c0 * 128:(c0 + kc) * 128, cb * 256:(cb + 1) * 256], "jobs": [cb * 2, cb * 2 + 1]}
                         for cb in range(D // 256)]
                phase_gemm(cx, "moe2", kc, 0, tiles, EpiResid(xT, HALO, t["M11"][:, 2 * KD:3 * KD]),
                           in_src=aT[c0:c0 + kc, :, :])
        phase_transpose_out(cx, xT, HALO, out, t["ident"])
    return nc


def prep_fused(inp, cfg, core):
    b, half = divmod(core, 2)
    D, KD, T, H, NE, DFE = cfg.D, cfg.KD, cfg.T, cfg.H, cfg.NE, cfg.DFE
    x = inp["x"]
    xinB = np.zeros((HALO + T, D), np.float32)
    xinB[HALO:] = x[b, half * T:(half + 1) * T]
    xinA = np.zeros((HALO + T, D), np.float32)
    if half == 1:
        xinB[:HALO] = x[b, T - HALO:T]
        xinA[HALO:] = x[b, 0:T]
    invc_start = np.zeros((128, 4, HALO), np.float32)
    invc_mid = np.zeros((128, 4, HALO), np.float32)
    for g, k in enumerate((2, 4, 8, 16)):
        for t_ in range(HALO):
            invc_start[:, g, t_] = 1.0 / min(t_ + 1, k)
            invc_mid[:, g, t_] = 1.0 / k
    oh = np.zeros((32, 3, 129), np.float32)
    for g, (_, d) in enumerate(PATTERNS):
        bk = _t5_bucket_np(np.arange(129) * d)
        oh[bk, g, np.arange(129)] = 1.0
    gvecs = np.stack([_colT(inp["norm_g"][0, 0], KD), _colT(inp["norm_g"][0, 1], KD), _colT(inp["kv_norm_g"], KD),
                      _colT(inp["pool_scale"][0], KD), _colT(inp["norm_g"][1, 0], KD), _colT(inp["norm_g"][1, 1], KD)],
                     axis=1)
    m = {
        "xinA": xinA, "xinB": xinB, "condT": _colT(inp["c"][b], KD),
        "kvadaw": inp["kv_ada_w"], "kvadabT": _colT(inp["kv_ada_b"], 2 * KD),
        "gvecs": np.ascontiguousarray(gvecs),
        "kgT": np.asarray(inp["k_norm_g"], np.float32).reshape(128, 1),
        "qgT": np.asarray(inp["q_norm_g"][0], np.float32).reshape(128, 1),
        "poolw": inp["pool_w"][0], "w1": inp["ffn_w1"][0], "w3": inp["ffn_w3"][0], "w2": inp["ffn_w2"][0],
        "wk": inp["w_k"], "wv": inp["w_v"], "wq": inp["w_q"][0], "wo": inp["w_o"][0],
        "relb": np.ascontiguousarray(inp["rel_bias"]),
        "relbrow": np.ascontiguousarray(inp["rel_bias"]).reshape(1, -1),
        "gqrow": np.asarray(inp["q_norm_g"][0], np.float32).reshape(1, 128),
        "gkrow": np.asarray(inp["k_norm_g"], np.float32).reshape(1, 128),
        "oh": oh, "cmask": np.full((128, 1), NEG if half == 0 else 0.0, np.float32),
        "routerw": inp["router_w"][0],
        "mw1": inp["moe_w1"][0], "mw3": inp["moe_w3"][0], "mw2": inp["moe_w2"][0].reshape(NE * DFE, D),
        "ident": np.eye(128, dtype=np.float32), "ones32": np.ones((128, 128), np.float32),
        "onesbf": np.ones((128, 128), ml_dtypes.bfloat16),
        "hmaskA": np.zeros((128, 2), np.float32), "hmaskB": np.full((128, 2), float(half), np.float32),
        "invcA": invc_start, "invcB": invc_start if half == 0 else invc_mid,
    }
    for i in range(2):
        for j in range(2):
            m[f"adaw{i}{j}"] = inp["ada_w"][i, j]
            m[f"adab{i}{j}T"] = _colT(inp["ada_b"][i, j], 3 * KD)
    return m


_CFG = Cfg()


def kernel(**inputs):
    cfg = _CFG
    inp = {k: np.asarray(v) for k, v in inputs.items()}
    n = 8
    nc = build_fused(cfg)
    maps = [prep_fused(inp, cfg, c) for c in range(n)]
    res = run_bass_kernel_spmd(nc, maps, core_ids=list(range(n))).results
    out = np.empty((cfg.B, cfg.SEQ, cfg.D), np.float32)
    for c in range(n):
        b, half = divmod(c, 2)
        out[b, half * cfg.T:(half + 1) * cfg.T] = res[c]["out"]
    return out
```
